# Optimizing a Trainium2 kernel written in Bass

```python
import jax
import jax.numpy as jnp
from jax import lax
import numpy as np

D_MODEL = 1024
BATCH = 8
SEQ = 2048
DEPTH = 2

MEM_LEN = 256
HEAD_DIM = 64
N_BRANCHES = 3
LN_EPS = 1e-5
BAND_BLOCK = 128

DIL_GROUPS = ((128, 1), (512, 4), (2048, 16))
DIL_HEADS = 4

GLA_HEADS = 4
GLA_DK = D_MODEL // 2 // GLA_HEADS
GLA_DV = D_MODEL // GLA_HEADS
GLA_RANK = 16
GLA_TAU = 16.0
GLA_CHUNK = 64

NSA_HEADS = 16
NSA_KV_GROUPS = 4
NSA_BRANCHES = 3
NSA_CMP_LEN = 32
NSA_CMP_STRIDE = 16
NSA_CMP_HIDDEN = 2 * HEAD_DIM
NSA_SEL_LEN = 64
NSA_N_SELECT = 16
NSA_WINDOW = 512
NSA_Q_BLOCK = 32

XATTN_HEADS = 4

N_EXPERTS = 32
TOP_K = 4
D_FF = D_MODEL
SWIGLU_LIMIT = 7.0
SWIGLU_ALPHA = 1.702
MOE_BLOCK = 128

DEEPNORM_ALPHA = (2 * DEPTH) ** 0.25
DEEPNORM_BETA = (8 * DEPTH) ** -0.25

A_W = len(DIL_GROUPS) * DIL_HEADS * HEAD_DIM
B_K = GLA_HEADS * GLA_DK
B_V = GLA_HEADS * GLA_DV
C_Q = NSA_HEADS * HEAD_DIM
C_KV = NSA_KV_GROUPS * HEAD_DIM
IN_WIDTHS = (A_W, A_W, A_W,
             B_K, B_K, B_V, B_V, GLA_RANK,
             C_Q, C_KV, C_KV, C_KV, C_KV, C_KV, C_KV, NSA_HEADS * NSA_BRANCHES,
             N_BRANCHES * D_MODEL)
N_IN = sum(IN_WIDTHS)

kernel_name = 'hybrid_dilated_gla_nsa_moe_deepnorm'


def layer_norm(x, g, b):
    xf = x.astype(jnp.float32)
    mu = jnp.mean(xf, axis=-1, keepdims=True)
    var = jnp.mean(jnp.square(xf - mu), axis=-1, keepdims=True)
    return ((xf - mu) * lax.rsqrt(var + LN_EPS)).astype(x.dtype) * g + b


def banded_attention(q, k, v, max_dist):
    n, seq_len, g, r, hd = q.shape
    blk = BAND_BLOCK
    n_prev = -(-max_dist // blk)
    nb = -(-seq_len // blk)
    pad = nb * blk - seq_len
    qp = jnp.pad(q, ((0, 0), (0, pad), (0, 0), (0, 0), (0, 0)))
    kv_pad = ((0, 0), (n_prev * blk, pad), (0, 0), (0, 0))
    kp = jnp.pad(k, kv_pad).reshape(n, nb + n_prev, blk, g, hd)
    vp = jnp.pad(v, kv_pad).reshape(n, nb + n_prev, blk, g, hd)
    kw = jnp.concatenate([kp[:, i:i + nb] for i in range(n_prev + 1)], axis=2)
    vw = jnp.concatenate([vp[:, i:i + nb] for i in range(n_prev + 1)], axis=2)
    width = (n_prev + 1) * blk
    qi = jnp.arange(blk)[:, None]
    kj = jnp.arange(width)[None, :]
    dist = qi + n_prev * blk - kj
    key_abs = jnp.arange(nb)[:, None, None] * blk - n_prev * blk + kj[None]
    mask = ((dist >= 0) & (dist <= max_dist))[None] & (key_abs >= 0)
    scale = hd ** -0.5

    def one_block(args):
        qb, kb, vb, mb = args
        s = jnp.einsum('nqgrd,nkgd->ngrqk', qb, kb).astype(jnp.float32) * scale
        s = jnp.where(mb, s, -jnp.inf)
        m = jnp.max(s, axis=-1, keepdims=True)
        p = jnp.exp(s - m)
        den = jnp.sum(p, axis=-1, keepdims=True)
        o = jnp.einsum('ngrqk,nkgd->nqgrd', (p / den).astype(vb.dtype), vb)
        lse = (m + jnp.log(den))[..., 0]
        return o, jnp.transpose(lse, (0, 3, 1, 2))

    qb = jnp.moveaxis(qp.reshape(n, nb, blk, g, r, hd), 1, 0)
    o, lse = lax.map(one_block, (qb, jnp.moveaxis(kw, 1, 0), jnp.moveaxis(vw, 1, 0), mask))
    o = jnp.moveaxis(o, 0, 1).reshape(n, nb * blk, g, r, hd)[:, :seq_len]
    lse = jnp.moveaxis(lse, 0, 1).reshape(n, nb * blk, g, r)[:, :seq_len]
    return o, lse


def dilated_attention(q, k, v):
    bsz, seq, _, nh, hd = q.shape
    outs, lses = [], []
    for gi, (window, dil) in enumerate(DIL_GROUPS):
        sub_len = seq // dil

        def to_sub(t):
            return t.reshape(bsz, sub_len, dil, nh, hd).transpose(0, 2, 1, 3, 4).reshape(bsz * dil, sub_len, nh, hd)

        o, lse = banded_attention(to_sub(q[:, :, gi])[:, :, :, None], to_sub(k[:, :, gi]), to_sub(v[:, :, gi]), window // dil)
        outs.append(o[:, :, :, 0].reshape(bsz, dil, sub_len, nh, hd).transpose(0, 2, 1, 3, 4).reshape(bsz, seq, nh, hd))
        lses.append(lse[..., 0].reshape(bsz, dil, sub_len, nh).transpose(0, 2, 1, 3).reshape(bsz, seq, nh))
    w = jax.nn.softmax(jnp.stack(lses), axis=0)
    return jnp.einsum('gbsh,gbshd->bshd', w.astype(q.dtype), jnp.stack(outs))


def gla_mixer(q, k, v, r, lr, w_alpha2, b_alpha, norm_g):
    bsz, seq, _ = q.shape
    nh, dk, dv, c = GLA_HEADS, GLA_DK, GLA_DV, GLA_CHUNK
    dtype = q.dtype
    log_a = jax.nn.log_sigmoid((lr @ w_alpha2 + b_alpha).astype(jnp.float32)) / GLA_TAU
    n_chunks = seq // c

    def chunks(t, width):
        return t.reshape(bsz, n_chunks, c, nh, width).transpose(1, 0, 3, 2, 4).astype(jnp.float32)

    qc = chunks(q, dk) * (dk ** -0.5)
    kc = chunks(k, dk)
    vc = chunks(v, dv)
    bc = jnp.cumsum(chunks(log_a, dk), axis=3)
    causal = jnp.tril(jnp.ones((c, c), dtype=bool))

    def step(state, inp):
        qi, ki, vi, bi = inp
        diff = bi[:, :, :, None, :] - bi[:, :, None, :, :]
        decay = jnp.exp(jnp.where(causal[:, :, None], diff, -jnp.inf))
        attn = jnp.einsum('bhic,bhjc,bhijc->bhij', qi, ki, decay)
        b_last = bi[:, :, -1:, :]
        o = attn @ vi + jnp.einsum('bhic,bhcv->bhiv', qi * jnp.exp(bi), state)
        state = jnp.exp(b_last[:, :, 0, :, None]) * state + jnp.einsum('bhjc,bhjv->bhcv', ki * jnp.exp(b_last - bi), vi)
        return state, o

    state0 = jnp.zeros((bsz, nh, dk, dv), jnp.float32)
    _, o = lax.scan(step, state0, (qc, kc, vc, bc))
    o = o.transpose(1, 0, 3, 2, 4).reshape(bsz, seq, nh, dv)
    o = o * lax.rsqrt(jnp.mean(jnp.square(o), axis=-1, keepdims=True) + LN_EPS)
    o = o.astype(dtype) * norm_g * jax.nn.silu(r).reshape(bsz, seq, nh, dv)
    return o.reshape(bsz, seq, nh * dv)


def compress_blocks(t, pe, w1, w2):
    bsz, seq, g, hd = t.shape
    n_cmp = (seq - NSA_CMP_LEN) // NSA_CMP_STRIDE + 1
    idx = jnp.arange(n_cmp)[:, None] * NSA_CMP_STRIDE + jnp.arange(NSA_CMP_LEN)[None, :]
    blocks = t[:, idx] + pe[:, None, :]
    flat = blocks.transpose(0, 1, 3, 2, 4).reshape(bsz, n_cmp, g, NSA_CMP_LEN * hd)
    return jax.nn.gelu(flat @ w1) @ w2


def nsa_selected_attention(q, k, v, sel_idx):
    bsz, seq, g, r, hd = q.shape
    n_blk = seq // NSA_SEL_LEN
    n_sel = sel_idx.shape[-1]
    kb = k.reshape(bsz, n_blk, NSA_SEL_LEN, g, hd).transpose(0, 3, 1, 2, 4)
    vb = v.reshape(bsz, n_blk, NSA_SEL_LEN, g, hd).transpose(0, 3, 1, 2, 4)
    nqb = seq // NSA_Q_BLOCK
    qx = jnp.moveaxis(q.reshape(bsz, nqb, NSA_Q_BLOCK, g, r, hd), 1, 0)
    ix = jnp.moveaxis(sel_idx.reshape(bsz, g, nqb, NSA_Q_BLOCK, n_sel), 2, 0)
    tp = jnp.arange(seq).reshape(nqb, NSA_Q_BLOCK)
    gather = jax.vmap(jax.vmap(lambda blocks, idx: blocks[idx]))
    offs = jnp.arange(NSA_SEL_LEN)
    scale = hd ** -0.5

    def one_block(args):
        qb, ib, tb = args
        kg = gather(kb, ib)
        vg = gather(vb, ib)
        s = jnp.einsum('bqgrd,bgqnld->bgqrnl', qb, kg).astype(jnp.float32) * scale
        kpos = ib[..., None] * NSA_SEL_LEN + offs
        valid = kpos <= tb[None, None, :, None, None]
        s = jnp.where(valid[:, :, :, None], s, -jnp.inf)
        p = jax.nn.softmax(s.reshape(bsz, g, NSA_Q_BLOCK, r, n_sel * NSA_SEL_LEN), axis=-1)
        p = p.reshape(bsz, g, NSA_Q_BLOCK, r, n_sel, NSA_SEL_LEN)
        return jnp.einsum('bgqrnl,bgqnld->bqgrd', p.astype(vg.dtype), vg)

    o = lax.map(one_block, (qx, ix, tp))
    return jnp.moveaxis(o, 0, 1).reshape(bsz, seq, g, r, hd)


def nsa_mixer(q, k_cmp_in, v_cmp_in, k_sel, v_sel, k_win, v_win, gate_logits,
              pe_k, w1_k, w2_k, pe_v, w1_v, w2_v):
    bsz, seq, g, r, hd = q.shape
    dtype = q.dtype
    pos = jnp.arange(seq)
    scale = hd ** -0.5
    kc = compress_blocks(k_cmp_in, pe_k, w1_k, w2_k)
    vc = compress_blocks(v_cmp_in, pe_v, w1_v, w2_v)
    n_cmp = kc.shape[1]
    cmp_start = jnp.arange(n_cmp) * NSA_CMP_STRIDE
    cvalid = (cmp_start + NSA_CMP_LEN - 1)[None, :] <= pos[:, None]
    s = jnp.einsum('bsgrd,bcgd->bgrsc', q, kc).astype(jnp.float32) * scale
    s = jnp.where(cvalid, s, -jnp.inf)
    m = jnp.max(s, axis=-1, keepdims=True)
    m = jnp.where(jnp.isfinite(m), m, 0.0)
    p = jnp.where(cvalid, jnp.exp(s - m), 0.0)
    p = p / jnp.maximum(jnp.sum(p, axis=-1, keepdims=True), 1e-30)
    o_cmp = jnp.einsum('bgrsc,bcgd->bsgrd', p.astype(dtype), vc)
    n_blk = seq // NSA_SEL_LEN
    sel_start = jnp.arange(n_blk) * NSA_SEL_LEN
    overlap = (cmp_start[:, None] < sel_start[None, :] + NSA_SEL_LEN) & (cmp_start[:, None] + NSA_CMP_LEN > sel_start[None, :])
    imp = jnp.einsum('bgrsc,cj->bgsj', p, overlap.astype(jnp.float32))
    q_blk = pos // NSA_SEL_LEN
    j = jnp.arange(n_blk)[None, :]
    forced = (j == 0) | (j == q_blk[:, None]) | (j == q_blk[:, None] - 1)
    future = j > q_blk[:, None]
    imp = jnp.where(forced, jnp.inf, jnp.where(future, -jnp.inf, imp))
    n_sel = min(NSA_N_SELECT, n_blk)
    _, sel_idx = lax.top_k(imp, n_sel)
    o_sel = nsa_selected_attention(q, k_sel, v_sel, sel_idx)
    o_win, _ = banded_attention(q, k_win, v_win, NSA_WINDOW - 1)
    gates = jax.nn.sigmoid(gate_logits).reshape(bsz, seq, g, r, NSA_BRANCHES)
    o = gates[..., 0:1] * o_cmp + gates[..., 1:2] * o_sel + gates[..., 2:3] * o_win
    return o.reshape(bsz, seq, g * r * hd)


def token_mixer(h, w_in, b_in, w_alpha2, b_alpha, gla_norm_g, cmp_pe_k, cmp_w1_k, cmp_w2_k,
                cmp_pe_v, cmp_w1_v, cmp_w2_v, w_br_a, w_br_b, w_br_c, w_o_mix):
    bsz, seq, d = h.shape
    u = h @ w_in + b_in
    split_points = np.cumsum(IN_WIDTHS)[:-1].tolist()
    (a_q, a_k, a_v, b_q, b_k, b_v, b_r, b_lr, c_q, c_kc, c_vc, c_ks, c_vs, c_kw, c_vw, c_g, m_g) = jnp.split(u, split_points, axis=-1)
    n_grp = len(DIL_GROUPS)

    def a_heads(t):
        return t.reshape(bsz, seq, n_grp, DIL_HEADS, HEAD_DIM)

    y_a = dilated_attention(a_heads(a_q), a_heads(a_k), a_heads(a_v)).reshape(bsz, seq, DIL_HEADS * HEAD_DIM)
    y_b = gla_mixer(b_q, b_k, b_v, b_r, b_lr, w_alpha2, b_alpha, gla_norm_g)

    def c_kv(t):
        return t.reshape(bsz, seq, NSA_KV_GROUPS, HEAD_DIM)

    y_c = nsa_mixer(c_q.reshape(bsz, seq, NSA_KV_GROUPS, NSA_HEADS // NSA_KV_GROUPS, HEAD_DIM),
                    c_kv(c_kc), c_kv(c_vc), c_kv(c_ks), c_kv(c_vs), c_kv(c_kw), c_kv(c_vw), c_g,
                    cmp_pe_k, cmp_w1_k, cmp_w2_k, cmp_pe_v, cmp_w1_v, cmp_w2_v)
    g = jax.nn.sigmoid(m_g).reshape(bsz, seq, N_BRANCHES, d)
    merged = g[:, :, 0] * (y_a @ w_br_a) + g[:, :, 1] * (y_b @ w_br_b) + g[:, :, 2] * (y_c @ w_br_c)
    return merged @ w_o_mix


def memory_cross_attention(x, mem, w_xq, w_xk, w_xv, w_xo):
    bsz, seq, d = x.shape
    hd = d // XATTN_HEADS
    q = (x @ w_xq).reshape(bsz, seq, XATTN_HEADS, hd)
    k = (mem @ w_xk).reshape(bsz, mem.shape[1], XATTN_HEADS, hd)
    v = (mem @ w_xv).reshape(bsz, mem.shape[1], XATTN_HEADS, hd)
    s = jnp.einsum('bshd,bmhd->bhsm', q, k).astype(jnp.float32) * (hd ** -0.5)
    p = jax.nn.softmax(s, axis=-1).astype(x.dtype)
    o = jnp.einsum('bhsm,bmhd->bshd', p, v).reshape(bsz, seq, d)
    return o @ w_xo


def clamped_swiglu(gate, up):
    gate = jnp.minimum(gate, SWIGLU_LIMIT)
    up = jnp.clip(up, -SWIGLU_LIMIT, SWIGLU_LIMIT)
    return (up + 1.0) * gate * jax.nn.sigmoid(SWIGLU_ALPHA * gate)


def moe_ffn(h, w_router, b_router, w_gu, b_gu, w_down, b_down):
    bsz, seq, d = h.shape
    t = h.reshape(-1, d)
    n_assign = t.shape[0] * TOP_K
    logits = t @ w_router + b_router
    top_logits, top_idx = lax.top_k(logits, TOP_K)
    gates = jax.nn.softmax(top_logits.astype(jnp.float32), axis=-1).astype(t.dtype)
    flat_e = top_idx.reshape(-1)
    order = jnp.argsort(flat_e)
    sorted_e = flat_e[order]
    tok_of = order // TOP_K
    counts = jnp.bincount(flat_e, length=N_EXPERTS)
    padded = (counts + MOE_BLOCK - 1) // MOE_BLOCK * MOE_BLOCK
    start = jnp.cumsum(counts) - counts
    pend = jnp.cumsum(padded)
    pstart = pend - padded
    dest = pstart[sorted_e] + (jnp.arange(n_assign) - start[sorted_e])
    n_rows = n_assign + N_EXPERTS * MOE_BLOCK
    n_blocks = n_rows // MOE_BLOCK
    buf = jnp.zeros((n_rows, d), t.dtype).at[dest].set(t[tok_of])
    block_e = jnp.minimum(jnp.searchsorted(pend, jnp.arange(n_blocks) * MOE_BLOCK, side='right'), N_EXPERTS - 1)

    def expert_block(args):
        xb, e = args
        gu = xb @ w_gu[e] + b_gu[e]
        return clamped_swiglu(gu[:, :D_FF], gu[:, D_FF:]) @ w_down[e] + b_down[e]

    out = lax.map(expert_block, (buf.reshape(n_blocks, MOE_BLOCK, d), block_e))
    y_assign = out.reshape(n_rows, d)[dest] * gates.reshape(-1)[order][:, None]
    y = jnp.zeros_like(t).at[tok_of].add(y_assign)
    return y.reshape(bsz, seq, d)


def setup_inputs(seed: int = 0) -> dict:
    key = jax.random.key(seed)
    keys = iter(jax.random.split(key, 40))

    def nrm(shape, scale):
        return jax.random.normal(next(keys), shape, jnp.float32) * scale

    dl, d, hd = DEPTH, D_MODEL, HEAD_DIM
    beta = DEEPNORM_BETA
    return {
        'x': nrm((BATCH, SEQ, d), 1.0),
        'mem': nrm((BATCH, MEM_LEN, d), 1.0),
        'ln0_g': 1.0 + nrm((d,), 0.02),
        'ln0_b': nrm((d,), 0.02),
        'w_in': nrm((dl, d, N_IN), d ** -0.5),
        'b_in': nrm((dl, N_IN), 0.02),
        'w_alpha2': nrm((dl, GLA_RANK, B_K), GLA_RANK ** -0.5),
        'b_alpha': nrm((dl, B_K), 0.1),
        'gla_norm_g': 1.0 + nrm((dl, GLA_DV), 0.02),
        'cmp_pe_k': nrm((dl, NSA_CMP_LEN, hd), 0.02),
        'cmp_w1_k': nrm((dl, NSA_CMP_LEN * hd, NSA_CMP_HIDDEN), (NSA_CMP_LEN * hd) ** -0.5),
        'cmp_w2_k': nrm((dl, NSA_CMP_HIDDEN, hd), NSA_CMP_HIDDEN ** -0.5),
        'cmp_pe_v': nrm((dl, NSA_CMP_LEN, hd), 0.02),
        'cmp_w1_v': nrm((dl, NSA_CMP_LEN * hd, NSA_CMP_HIDDEN), (NSA_CMP_LEN * hd) ** -0.5),
        'cmp_w2_v': nrm((dl, NSA_CMP_HIDDEN, hd), NSA_CMP_HIDDEN ** -0.5),
        'w_br_a': nrm((dl, DIL_HEADS * hd, d), (DIL_HEADS * hd) ** -0.5),
        'w_br_b': nrm((dl, B_V, d), B_V ** -0.5),
        'w_br_c': nrm((dl, C_Q, d), C_Q ** -0.5),
        'w_o_mix': nrm((dl, d, d), beta * d ** -0.5),
        'ln1_g': 1.0 + nrm((dl, d), 0.02),
        'ln1_b': nrm((dl, d), 0.02),
        'w_xq': nrm((dl, d, d), d ** -0.5),
        'w_xk': nrm((dl, d, d), d ** -0.5),
        'w_xv': nrm((dl, d, d), d ** -0.5),
        'w_xo': nrm((dl, d, d), beta * d ** -0.5),
        'ln2_g': 1.0 + nrm((dl, d), 0.02),
        'ln2_b': nrm((dl, d), 0.02),
        'w_router': nrm((dl, d, N_EXPERTS), d ** -0.5),
        'b_router': nrm((dl, N_EXPERTS), 0.01),
        'w_gu': nrm((dl, N_EXPERTS, d, 2 * D_FF), d ** -0.5),
        'b_gu': nrm((dl, N_EXPERTS, 2 * D_FF), 0.02),
        'w_down': nrm((dl, N_EXPERTS, D_FF, d), beta * D_FF ** -0.5),
        'b_down': nrm((dl, N_EXPERTS, d), 0.02),
        'ln3_g': 1.0 + nrm((dl, d), 0.02),
        'ln3_b': nrm((dl, d), 0.02),
    }


def reference(x, mem, ln0_g, ln0_b, w_in, b_in, w_alpha2, b_alpha, gla_norm_g,
              cmp_pe_k, cmp_w1_k, cmp_w2_k, cmp_pe_v, cmp_w1_v, cmp_w2_v,
              w_br_a, w_br_b, w_br_c, w_o_mix, ln1_g, ln1_b,
              w_xq, w_xk, w_xv, w_xo, ln2_g, ln2_b,
              w_router, b_router, w_gu, b_gu, w_down, b_down, ln3_g, ln3_b):
    alpha = DEEPNORM_ALPHA
    x = layer_norm(x, ln0_g, ln0_b)
    for li in range(DEPTH):
        mix = token_mixer(x, w_in[li], b_in[li], w_alpha2[li], b_alpha[li], gla_norm_g[li],
                          cmp_pe_k[li], cmp_w1_k[li], cmp_w2_k[li], cmp_pe_v[li], cmp_w1_v[li], cmp_w2_v[li],
                          w_br_a[li], w_br_b[li], w_br_c[li], w_o_mix[li])
        x = layer_norm(alpha * x + mix, ln1_g[li], ln1_b[li])
        xa = memory_cross_attention(x, mem, w_xq[li], w_xk[li], w_xv[li], w_xo[li])
        x = layer_norm(alpha * x + xa, ln2_g[li], ln2_b[li])
        ff = moe_ffn(x, w_router[li], b_router[li], w_gu[li], b_gu[li], w_down[li], b_down[li])
        x = layer_norm(alpha * x + ff, ln3_g[li], ln3_b[li])
    return x
```

```python
import numpy as np
import concourse.bass as bass
import concourse.mybir as mybir
from concourse.bass_utils import run_bass_kernel_spmd
from contextlib import ExitStack

DT = mybir.dt
F32 = DT.float32
BF16 = DT.bfloat16
AF = mybir.ActivationFunctionType
ALU = mybir.AluOpType
AX = mybir.AxisListType

_ISZ = {F32: 4, BF16: 2, DT.int32: 4, DT.uint32: 4, DT.uint8: 1, DT.int8: 1, DT.uint16: 2, DT.int16: 2, DT.float16: 2}


def region(ap):
    t = ap.tensor
    isz = _ISZ[ap.dtype]
    a = list(ap.ap)
    space = str(ap.space).upper()
    if 'DRAM' in space or 'HBM' in space:
        lo = ap.offset
        hi = ap.offset
        for st, n in a:
            if n > 1:
                if st >= 0:
                    hi += st * (n - 1)
                else:
                    lo += st * (n - 1)
        return (t.name, 0, 1, lo * isz, (hi + 1) * isz)
    pst, pn = a[0]
    if pst == 0:
        p0 = 0
        f0 = ap.offset
    else:
        p0 = ap.offset // pst
        f0 = ap.offset - p0 * pst
    lo = f0
    hi = f0
    for st, n in a[1:]:
        if n > 1:
            if st >= 0:
                hi += st * (n - 1)
            else:
                lo += st * (n - 1)
    return (t.name, p0, p0 + pn, lo * isz, (hi + 1) * isz)


class Prog:
    ENG = ['pe', 'dve', 'act', 'pool', 'sp']

    def __init__(self, nc, slots=None):
        self.nc = nc
        self.es = ExitStack()
        self.stream = {e: [] for e in self.ENG}
        slots = slots or {'sp': 8, 'pool': 6, 'act': 2}
        self.slots = {q: ['%s_d%d' % (q, i) for i in range(n)] for q, n in slots.items()}
        self.slot_rr = {q: 0 for q in slots}
        self.tl = ['pe', 'dve', 'act', 'pool'] + [s for q in self.slots for s in self.slots[q]]
        self.count = {t: 0 for t in self.tl}
        self.clk = {e: {} for e in self.ENG}
        self.opclk = {}
        self.track = {}
        self.sem = {}
        self.nwait = 0
        self.nop = 0
        for t in self.tl:
            self.sem[t] = self.es.enter_context(nc.semaphore('sem_' + t))

    def sb(self, name, shape, dt=F32):
        return self.es.enter_context(self.nc.sbuf_tensor(name, list(shape), dt))

    def ps(self, name, shape, dt=F32):
        return self.es.enter_context(self.nc.psum_tensor(name, list(shape), dt))

    def _deps(self, regs_r, regs_w):
        deps = {}
        for (kind, regs) in (('r', regs_r), ('w', regs_w)):
            for (name, p0, p1, b0, b1) in regs:
                for rec in self.track.get(name, ()):
                    if kind == 'r' and rec[4] == 'r':
                        continue
                    if rec[0] < p1 and p0 < rec[1] and rec[2] < b1 and b0 < rec[3]:
                        t = rec[5]
                        if rec[6] > deps.get(t, 0):
                            deps[t] = rec[6]
        return deps

    def _wait(self, S, t, seq):
        clk = self.clk[S]
        if clk.get(t, 0) >= seq:
            return
        val = seq * 16 if '_d' in t else seq
        self.stream[S].append(('w', t, val))
        self.nwait += 1
        oc = self.opclk.get((t, seq))
        if oc:
            for k, v in oc.items():
                if v > clk.get(k, 0):
                    clk[k] = v
        clk[t] = max(clk.get(t, 0), seq)

    def _record(self, regs_r, regs_w, T, seq):
        for (name, p0, p1, b0, b1) in regs_w:
            lst = self.track.setdefault(name, [])
            lst[:] = [r for r in lst if not (p0 <= r[0] and r[1] <= p1 and b0 <= r[2] and r[3] <= b1)]
            lst.append((p0, p1, b0, b1, 'w', T, seq))
        for (name, p0, p1, b0, b1) in regs_r:
            lst = self.track.setdefault(name, [])
            lst[:] = [r for r in lst if not (r[4] == 'r' and r[5] == T and p0 <= r[0] and r[1] <= p1 and b0 <= r[2] and r[3] <= b1)]
            lst.append((p0, p1, b0, b1, 'r', T, seq))

    def op(self, S, fn, reads=(), writes=(), dma=False):
        regs_r = [region(a) for a in reads if a is not None]
        regs_w = [region(a) for a in writes if a is not None]
        deps = self._deps(regs_r, regs_w)
        if dma:
            sl = self.slots[S]
            T = sl[self.slot_rr[S] % len(sl)]
            self.slot_rr[S] += 1
            if self.count[T] > 0:
                deps[T] = max(deps.get(T, 0), self.count[T])
        else:
            T = S
            if S == 'pe':
                deps.pop('pe', None)
        for t in sorted(deps, key=lambda k: -deps[k]):
            self._wait(S, t, deps[t])
        self.count[T] += 1
        seq = self.count[T]
        self.opclk[(T, seq)] = dict(self.clk[S])
        self.stream[S].append(('o', fn, T, 16 if dma else 1))
        self.nop += 1
        self._record(regs_r, regs_w, T, seq)
        return (T, seq)

    def finish(self, S='sp'):
        for t in self.tl:
            if self.count[t] > 0:
                self._wait(S, t, self.count[t])

    def emit(self):
        nc = self.nc
        sem = self.sem

        def run(items, e):
            for it in items:
                if it[0] == 'w':
                    e.wait_ge(sem[it[1]], it[2])
                else:
                    ins = it[1](e)
                    ins.then_inc(sem[it[2]], it[3])

        with nc.Block() as block:
            @block.tensor
            def _(e):
                run(self.stream['pe'], e)

            @block.vector
            def _(e):
                run(self.stream['dve'], e)

            @block.scalar
            def _(e):
                run(self.stream['act'], e)

            @block.gpsimd
            def _(e):
                run(self.stream['pool'], e)

            @block.sync
            def _(e):
                run(self.stream['sp'], e)
        self.es.close()

    def dma(self, q, out, in_, **kw):
        return self.op(q, lambda e: e.dma_start(out=out, in_=in_, **kw), reads=[in_], writes=[out], dma=True)

    def mm(self, out, lhsT, rhs, start=True, stop=True):
        return self.op('pe', lambda e: e.matmul(out, lhsT, rhs, start=start, stop=stop),
                       reads=[lhsT, rhs], writes=[out])

    def tr(self, out, in_, ident):
        return self.op('pe', lambda e: e.transpose(out, in_, ident), reads=[in_, ident], writes=[out])

    def act(self, out, in_, func, bias=None, scale=None, accum_out=None):
        kw = {}
        rd = [in_]
        if bias is not None:
            kw['bias'] = bias
            if not isinstance(bias, (int, float)):
                rd.append(bias)
        if scale is not None:
            kw['scale'] = scale
            if not isinstance(scale, (int, float)):
                rd.append(scale)
        wr = [out]
        if accum_out is not None:
            kw['accum_out'] = accum_out
            wr.append(accum_out)
        return self.op('act', lambda e: e.activation(out, in_, func, **kw), reads=rd, writes=wr)

    def tt(self, eng, out, in0, in1, op):
        return self.op(eng, lambda e: e.tensor_tensor(out, in0, in1, op), reads=[in0, in1], writes=[out])

    def ts(self, eng, out, in0, s1, op0, s2=None, op1=None):
        rd = [in0]
        if s1 is not None and not isinstance(s1, (int, float)):
            rd.append(s1)
        if s2 is not None and not isinstance(s2, (int, float)):
            rd.append(s2)
        kw = {}
        if op1 is not None:
            kw['op1'] = op1
        return self.op(eng, lambda e: e.tensor_scalar(out, in0, s1, s2, op0, **kw), reads=rd, writes=[out])

    def stt(self, out, in0, scalar, in1, op0, op1):
        rd = [in0, in1]
        if not isinstance(scalar, (int, float)):
            rd.append(scalar)
        return self.op('dve', lambda e: e.scalar_tensor_tensor(out, in0, scalar, in1, op0, op1), reads=rd, writes=[out])

    def copy(self, eng, out, in_):
        if eng == 'act':
            return self.op('act', lambda e: e.copy(out, in_), reads=[in_], writes=[out])
        return self.op(eng, lambda e: e.tensor_copy(out, in_), reads=[in_], writes=[out])

    def memset(self, eng, ap, val):
        return self.op(eng, lambda e: e.memset(ap, val), writes=[ap])


class Arena:
    def __init__(self, P, nbytes):
        self.nbytes = nbytes
        self.t32 = P.sb("arena", [128, nbytes // 4], F32)
        self.t16 = self.t32.bitcast(BF16)
        self.top = 0

    def alloc(self, shape, dt=F32):
        shape = list(shape)
        n = int(np.prod(shape))
        isz = _ISZ[dt]
        off = (self.top + 63) // 64 * 64
        self.top = off + n * isz
        assert self.top <= self.nbytes, ("arena overflow", self.top, self.nbytes)
        base = self.t32 if isz == 4 else self.t16
        ap = base[:, off // isz: off // isz + n]
        if len(shape) == 2:
            ap = ap.rearrange("p (a b) -> p a b", a=shape[0])
        elif len(shape) == 3:
            ap = ap.rearrange("p (a b c) -> p a b c", a=shape[0], b=shape[1])
        elif len(shape) == 4:
            ap = ap.rearrange("p (a b c d) -> p a b c d", a=shape[0], b=shape[1], c=shape[2])
        return ap

    def mark(self):
        return self.top

    def release(self, m):
        self.top = m


S = 2048
D = 1024
NT = 16
DEPTH = 2
MEM = 256
ALPHA = float((2 * DEPTH) ** 0.25)
EPS = 1e-5
NE = 32
NIN = 11072
O_AQ, O_AK, O_AV = 0, 768, 1536
O_BQ, O_BK, O_BV, O_BR, O_BLR = 2304, 2816, 3328, 4352, 5376
O_CQ = 5392
O_CKC, O_CVC, O_CKS, O_CVS, O_CKW, O_CVW = 6416, 6672, 6928, 7184, 7440, 7696
O_CG = 7952
O_MG = 8000
NEGBIG = -30000.0

WNAMES = ['w_in', 'b_in', 'w_alpha2', 'b_alpha', 'gla_norm_g', 'cmp_pe_k', 'cmp_w1_k', 'cmp_w2_k',
          'cmp_pe_v', 'cmp_w1_v', 'cmp_w2_v', 'w_br_a', 'w_br_b', 'w_br_c', 'w_o_mix', 'ln1_g', 'ln1_b',
          'w_xq', 'w_xk', 'w_xv', 'w_xo', 'ln2_g', 'ln2_b', 'w_router', 'b_router', 'w_gu', 'b_gu',
          'w_down', 'b_down', 'ln3_g', 'ln3_b']
WSHAPES = {
    'w_in': [2, 1024, NIN], 'b_in': [2, NIN], 'w_alpha2': [2, 16, 512], 'b_alpha': [2, 512], 'gla_norm_g': [2, 256],
    'cmp_pe_k': [2, 32, 64], 'cmp_w1_k': [2, 2048, 128], 'cmp_w2_k': [2, 128, 64],
    'cmp_pe_v': [2, 32, 64], 'cmp_w1_v': [2, 2048, 128], 'cmp_w2_v': [2, 128, 64],
    'w_br_a': [2, 256, 1024], 'w_br_b': [2, 1024, 1024], 'w_br_c': [2, 1024, 1024], 'w_o_mix': [2, 1024, 1024],
    'ln1_g': [2, 1024], 'ln1_b': [2, 1024], 'w_xq': [2, 1024, 1024], 'w_xk': [2, 1024, 1024], 'w_xv': [2, 1024, 1024],
    'w_xo': [2, 1024, 1024], 'ln2_g': [2, 1024], 'ln2_b': [2, 1024], 'w_router': [2, 1024, 32], 'b_router': [2, 32],
    'w_gu': [2, 32, 1024, 2048], 'b_gu': [2, 32, 2048], 'w_down': [2, 32, 1024, 1024], 'b_down': [2, 32, 1024],
    'ln3_g': [2, 1024], 'ln3_b': [2, 1024],
}


def host_consts():
    c = {}
    c['c_ident'] = np.eye(128, dtype=np.float32)
    p = np.arange(128)[:, None]
    q = np.arange(512)[None, :]
    md = np.zeros((128, 8, 512), np.float32)
    for d in range(4):
        md[:, d, :] = (128 * d + p <= q)
        md[:, 4 + d, :] = 1.0 - md[:, d, :]
    c['c_maskd'] = md
    ma = np.zeros((128, 256), np.float32)
    q1 = np.arange(128)[None, :]
    ma[:, 0:128] = (p <= q1)
    ma[:, 128:256] = (p >= q1)
    c['c_maska'] = ma
    cc = np.arange(128)[:, None]
    t = np.arange(S)[None, :]
    cv = ((16 * cc + 31) <= t).astype(np.float32)
    cv[127, :] = 0.0
    c['c_cmpvalid'] = cv
    ov = np.zeros((128, 32), np.float32)
    for ci in range(127):
        for j in range(32):
            if ci * 16 < j * 64 + 64 and ci * 16 + 32 > j * 64:
                ov[ci, j] = 1.0
    c['c_overlap'] = ov
    pos = np.arange(S)
    qb = pos // 64
    j = np.arange(32)[None, :]
    forced = (j == 0) | (j == qb[:, None]) | (j == qb[:, None] - 1)
    future = j > qb[:, None]
    keep = (~forced & ~future).astype(np.float32)
    add = np.where(forced, 1e4, np.where(future, -1e4, 0.0)).astype(np.float32)
    c['c_keep'] = keep
    c['c_add'] = add
    key = np.arange(S)[None, :]
    c['c_eexp'] = (key // 64 == np.arange(32)[:, None]).astype(np.float32)
    return c


CSHAPES = {'c_ident': [128, 128], 'c_maskd': [128, 8, 512], 'c_maska': [128, 256], 'c_cmpvalid': [128, S],
           'c_overlap': [128, 32], 'c_keep': [S, 32], 'c_add': [S, 32], 'c_eexp': [32, S]}


class Ctx:
    pass


def build_nc(stages, dbg=False):
    nc = bass.Bass("TRN2", target_bir_lowering=False)
    C = Ctx()
    C.nc = nc
    C.dbg = dbg
    dr = {}
    dr['x'] = nc.dram_tensor("x", [S, D], F32, kind="ExternalInput").ap()
    dr['mem'] = nc.dram_tensor("mem", [MEM, D], F32, kind="ExternalInput").ap()
    dr['ln0_g'] = nc.dram_tensor("ln0_g", [1, D], F32, kind="ExternalInput").ap()
    dr['ln0_b'] = nc.dram_tensor("ln0_b", [1, D], F32, kind="ExternalInput").ap()
    for n in WNAMES:
        dr[n] = nc.dram_tensor(n, WSHAPES[n], F32, kind="ExternalInput").ap()
    for n, shp in CSHAPES.items():
        dr[n] = nc.dram_tensor(n, shp, F32, kind="ExternalInput").ap()
    dr['out'] = nc.dram_tensor("out", [S, D], F32, kind="ExternalOutput").ap()
    sk = "ExternalOutput" if dbg else "Internal"
    dr['xs'] = nc.dram_tensor("xs", [S, D], F32, kind="Internal").ap()
    dr['ysa'] = nc.dram_tensor("ysa", [4, 64, S], BF16, kind=sk).ap()
    dr['ysb'] = nc.dram_tensor("ysb", [S, D], BF16, kind=sk).ap()
    dr['ysc'] = nc.dram_tensor("ysc", [S, D], BF16, kind=sk).ap()
    C.dr = dr
    P = Prog(nc)
    C.P = P
    A = Arena(P, 207 * 1024)
    C.A = A
    C.pst = [P.ps("psb%d" % i, [128, 512], F32) for i in range(8)]
    C.ps = [t[:, :] for t in C.pst]
    C.psb = [t.bitcast(BF16)[:, :] for t in C.pst]
    C.xT = A.alloc([8, S], BF16)
    C.ident = A.alloc([128], BF16)
    C.identf = A.alloc([128], F32)
    C.ones = A.alloc([512], BF16)
    P.dma('pool', C.ident, dr['c_ident'])
    P.dma('sp', C.identf, dr['c_ident'])
    P.memset('dve', C.ones, 1.0)
    C.memT = A.alloc([8, MEM], BF16)
    C.maskd = A.alloc([8, 512], BF16)
    C.maska = A.alloc([256], BF16)
    P.dma('pool', C.maskd, dr['c_maskd'])
    P.dma('pool', C.maska, dr['c_maska'])
    for st in stages:
        if st == 'ln0':
            stage_ln0(C, True)
        elif st == 'load':
            stage_ln0(C, False)
        elif st.startswith('moe'):
            stage_moe(C, int(st[3:4]), last=st.endswith('L'))
        elif st == 'out':
            stage_out(C)
        elif st == 'meminit':
            stage_meminit(C)
        elif st.startswith('xattn'):
            stage_xattn(C, int(st[5:]))
        elif st.startswith('mixA'):
            stage_mixA(C, int(st[4:]))
        elif st.startswith('mixB'):
            stage_mixB(C, int(st[4:]))
        elif st.startswith('mixC'):
            stage_mixC(C, int(st[4:]))
        elif st.startswith('merge'):
            stage_merge(C, int(st[5:]))
        else:
            raise ValueError(st)
    P.finish()
    P.emit()
    C.nop = P.nop
    C.nwait = P.nwait
    return nc, C


class LNState:
    pass


def ln_setup(C, g_ap, b_ap):
    P, A = C.P, C.A
    L = LNState()
    L.g = A.alloc([D], F32)
    L.b = A.alloc([D], F32)
    P.dma('sp', L.g, g_ap.broadcast_to([128, D]))
    P.dma('sp', L.b, b_ap.broadcast_to([128, D]))
    L.stats = [A.alloc([2, 6], F32) for _ in range(2)]
    L.mv = [A.alloc([2], F32) for _ in range(2)]
    L.sd = [A.alloc([1], F32) for _ in range(2)]
    L.rs = [A.alloc([1], F32) for _ in range(2)]
    L.xn = [A.alloc([D], F32) for _ in range(2)]
    L.xb = [A.alloc([D], BF16) for _ in range(2)]
    L.n = 0
    return L


def ln_tile(C, L, src, t, dst_dram, pst):
    P = C.P
    i = L.n % 2
    L.n += 1
    st, mv, sd, rs, xn, xb = L.stats[i], L.mv[i], L.sd[i], L.rs[i], L.xn[i], L.xb[i]
    for h in range(2):
        P.op('dve', lambda e, h=h: e.bn_stats(st[:, h, :], src[:, h * 512:(h + 1) * 512]),
             reads=[src[:, h * 512:(h + 1) * 512]], writes=[st[:, h, :]])
    P.op('dve', lambda e: e.bn_aggr(mv, st), reads=[st], writes=[mv])
    P.act(sd, mv[:, 1:2], AF.Sqrt, bias=EPS)
    P.op('dve', lambda e: e.reciprocal(rs, sd), reads=[sd], writes=[rs])
    P.ts('dve', xn, src, mv[:, 0:1], ALU.subtract, rs, ALU.mult)
    P.tt('dve', xn, xn, L.g, ALU.mult)
    P.tt('pool', xn, xn, L.b, ALU.add)
    P.dma('sp', dst_dram[t * 128:(t + 1) * 128, :], xn)
    P.copy('act', xb, xn)
    pv = pst.rearrange("p (c q) -> p c q", c=8)
    for c in range(8):
        P.tr(pv[:, c, :], xb[:, c * 128:(c + 1) * 128], C.ident)
    P.copy('dve', C.xT[:, :, t * 128:(t + 1) * 128], pv)


def stage_ln0(C, do_ln):
    P, A, dr = C.P, C.A, C.dr
    m = A.mark()
    L = ln_setup(C, dr['ln0_g'], dr['ln0_b'])
    xin = [A.alloc([D], F32) for _ in range(2)]
    for t in range(NT):
        xi = xin[t % 2]
        P.dma('sp', xi, dr['x'][t * 128:(t + 1) * 128, :])
        if do_ln:
            ln_tile(C, L, xi, t, dr['xs'], C.psb[t % 2])
        else:
            P.dma('sp', dr['xs'][t * 128:(t + 1) * 128, :], xi)
            xb = L.xb[t % 2]
            P.copy('act', xb, xi)
            pv = C.psb[t % 2].rearrange("p (c q) -> p c q", c=8)
            for c in range(8):
                P.tr(pv[:, c, :], xb[:, c * 128:(c + 1) * 128], C.ident)
            P.copy('dve', C.xT[:, :, t * 128:(t + 1) * 128], pv)
    A.release(m)


def stage_out(C):
    P, A, dr = C.P, C.A, C.dr
    m = A.mark()
    buf = [A.alloc([D], F32) for _ in range(2)]
    for t in range(NT):
        b = buf[t % 2]
        P.dma('sp', b, dr['xs'][t * 128:(t + 1) * 128, :])
        P.dma('sp', dr['out'][t * 128:(t + 1) * 128, :], b)
    A.release(m)


def stage_moe(C, li, last=False):
    dst_final = C.dr['out'] if last else C.dr['xs']
    P, A, dr = C.P, C.A, C.dr
    ps, psb = C.ps, C.psb
    xT = C.xT
    m0 = A.mark()
    wr = A.alloc([8, NE], BF16)
    P.dma('pool', wr, dr['w_router'][li].rearrange("(k p) n -> p k n", p=128))
    brow = A.alloc([NE], BF16)
    P.dma('pool', brow[0:1, :], dr['b_router'][li:li + 1, :])
    lg = A.alloc([NT, NE], F32)
    G = A.alloc([NT, NE], F32)
    m8 = A.alloc([NT, 8], F32)
    negmx = A.alloc([NT], F32)
    lgp = ps[0][:, :].rearrange("p (t e) -> p t e", t=NT)
    for t in range(NT):
        for k in range(8):
            P.mm(lgp[:, t, :], xT[:, k, t * 128:(t + 1) * 128], wr[:, k, :], start=(k == 0), stop=False)
        P.mm(lgp[:, t, :], C.ones[0:1, 0:128], brow[0:1, :], start=False, stop=True)
    P.copy('act', lg, lgp)
    tmpm = A.alloc([NE], F32)
    tmpe = A.alloc([NE], F32)
    ssum = A.alloc([1], F32)
    rsum = A.alloc([1], F32)
    for t in range(NT):
        P.op('dve', lambda e, t=t: e.max(m8[:, t, :], lg[:, t, :]), reads=[lg[:, t, :]], writes=[m8[:, t, :]])
    P.ts('dve', negmx, m8[:, :, 0], -1.0, ALU.mult)
    for t in range(NT):
        P.ts('dve', tmpm, lg[:, t, :], m8[:, t, 3:4], ALU.is_ge)
        P.act(tmpe, lg[:, t, :], AF.Exp, bias=negmx[:, t:t + 1])
        P.tt('dve', tmpe, tmpe, tmpm, ALU.mult)
        P.op('dve', lambda e: e.tensor_reduce(ssum, tmpe, AX.X, ALU.add), reads=[tmpe], writes=[ssum])
        P.op('dve', lambda e: e.reciprocal(rsum, ssum), reads=[ssum], writes=[rsum])
        P.ts('dve', G[:, t, :], tmpe, rsum, ALU.mult)
    bgu_raw = A.alloc([2048], F32)
    P.dma('sp', bgu_raw[0:NE, :], dr['b_gu'][li])
    bguT = A.alloc([16, NE], F32)
    bp = ps[1][:, :].rearrange("p (c e) -> p c e", c=16)
    for c in range(16):
        P.tr(bp[:, c, :], bgu_raw[0:NE, c * 128:(c + 1) * 128], C.identf[0:NE, 0:NE])
    P.copy('act', bguT, bp)
    bd = A.alloc([D], BF16)
    P.dma('pool', bd[0:NE, :], dr['b_down'][li])
    m1 = A.mark()
    NB = 8
    for half in range(2):
        A.release(m1)
        yacc = A.alloc([8, D], F32)
        m2 = A.mark()
        ring = [A.alloc([8, 512], BF16) for _ in range(NB)]
        actT = [A.alloc([8, 512], BF16) for _ in range(2)]
        tg = [A.alloc([512], F32) for _ in range(2)]
        tsg = [A.alloc([512], F32) for _ in range(2)]
        tu = [A.alloc([512], F32) for _ in range(2)]
        rr = [0]

        def getblk(src2d):
            b = ring[rr[0] % NB]
            rr[0] += 1
            P.dma('pool', b, src2d.rearrange("(k p) n -> p k n", p=128))
            return b

        pending = []
        cnt = 0
        fgc = 0
        dcn = 0

        def do_down(job):
            nonlocal dcn
            (e, tok0, aT, dblk) = job
            for sub in range(4):
                tile = (tok0 + sub * 128) // 128
                lt = tile - half * 8
                for hc in range(2):
                    pD = ps[4 + dcn % 4]
                    dcn += 1
                    for fk in range(8):
                        P.mm(pD, aT[:, fk, sub * 128:(sub + 1) * 128], dblk[hc][:, fk, :], start=(fk == 0), stop=(fk == 7))
                    ysl = yacc[:, lt, hc * 512:(hc + 1) * 512]
                    if e == 0:
                        P.ts('dve', ysl, pD, G[:, tile, e:e + 1], ALU.mult)
                    else:
                        P.stt(ysl, pD, G[:, tile, e:e + 1], ysl, ALU.mult, ALU.add)

        for e in range(NE):
            gu = [getblk(dr['w_gu'][li, e][:, b * 512:(b + 1) * 512]) for b in range(4)]
            dblk = [getblk(dr['w_down'][li, e][:, b * 512:(b + 1) * 512]) for b in range(2)]
            for qt in range(2):
                tok0 = half * 1024 + qt * 512
                aT = actT[cnt % 2]
                cnt += 1
                for fg in range(8):
                    pA = ps[(2 * fgc) % 4]
                    pB = ps[(2 * fgc + 1) % 4]
                    i2 = fgc % 2
                    fgc += 1
                    cols = (fg % 4) * 128
                    bg = gu[fg // 4]
                    bu = gu[2 + fg // 4]
                    for k in range(8):
                        P.mm(pA, bg[:, k, cols:cols + 128], xT[:, k, tok0:tok0 + 512], start=(k == 0), stop=(k == 7))
                    for k in range(8):
                        P.mm(pB, bu[:, k, cols:cols + 128], xT[:, k, tok0:tok0 + 512], start=(k == 0), stop=(k == 7))
                    g1, sg, ua = tg[i2], tsg[i2], tu[i2]
                    P.ts('dve', g1, pA, bguT[:, fg, e:e + 1], ALU.add, 7.0, ALU.min)
                    P.act(sg, g1, AF.Sigmoid, scale=1.702)
                    P.act(ua, pB, AF.Identity, bias=bguT[:, 8 + fg, e:e + 1])
                    P.ts('dve', ua, ua, 7.0, ALU.min, -7.0, ALU.max)
                    P.tt('dve', g1, g1, sg, ALU.mult)
                    P.stt(aT[:, fg, :], ua, 1.0, g1, ALU.add, ALU.mult)
                pending.append((e, tok0, aT, dblk))
                if len(pending) > 1:
                    do_down(pending.pop(0))
        while pending:
            do_down(pending.pop(0))
        A.release(m2)
        L = ln_setup(C, dr['ln3_g'][li:li + 1, :], dr['ln3_b'][li:li + 1, :])
        gTb = [A.alloc([128], BF16) for _ in range(2)]
        xo = [A.alloc([D], F32) for _ in range(2)]
        for lt in range(8):
            tile = half * 8 + lt
            i2 = lt % 2
            gp = ps[0][0:NE, 0:128]
            P.tr(gp, G[:, tile, :], C.identf)
            P.copy('act', gTb[i2][0:NE, :], gp)
            for hc in range(2):
                P.mm(ps[2 + hc], gTb[i2][0:NE, :], bd[0:NE, hc * 512:(hc + 1) * 512], start=True, stop=True)
            P.dma('sp', xo[i2], dr['xs'][tile * 128:(tile + 1) * 128, :])
            P.stt(xo[i2], xo[i2], ALPHA, yacc[:, lt, :], ALU.mult, ALU.add)
            for hc in range(2):
                P.tt('dve', xo[i2][:, hc * 512:(hc + 1) * 512], xo[i2][:, hc * 512:(hc + 1) * 512], ps[2 + hc], ALU.add)
            ln_tile(C, L, xo[i2], tile, dst_final, psb[1])
    A.release(m0)


def run_pipe(jobs, depth=1):
    q = []
    for (s1, s2) in jobs:
        ctx = s1()
        q.append((s2, ctx))
        if len(q) > depth:
            f, c = q.pop(0)
            f(c)
    while q:
        f, c = q.pop(0)
        f(c)


def load_w(C, dst, src2d, q='pool'):
    C.P.dma(q, dst, src2d.rearrange("(k p) n -> p k n", p=128))


def bias_col(C, dst, src_row):
    C.P.dma('sp', dst, src_row.rearrange("o (p i) -> (o p) i", i=1))


def stage_meminit(C):
    P, A, dr = C.P, C.A, C.dr
    m = A.mark()
    mf = A.alloc([2, D], F32)
    mb = A.alloc([2, D], BF16)
    P.dma('sp', mf, dr['mem'].rearrange("(b p) d -> p b d", p=128))
    P.copy('act', mb, mf)
    for b in range(2):
        pv = C.psb[b].rearrange("p (c q) -> p c q", c=8)
        for c in range(8):
            P.tr(pv[:, c, :], mb[:, b, c * 128:(c + 1) * 128], C.ident)
        P.copy('dve', C.memT[:, :, b * 128:(b + 1) * 128], pv)
    A.release(m)


def stage_xattn(C, li):
    P, A, dr = C.P, C.A, C.dr
    ps, psb, xT = C.ps, C.psb, C.xT
    m0 = A.mark()
    kTx = A.alloc([4, 2, MEM], BF16)
    Vx = A.alloc([2, 4, 258], BF16)
    wk = A.alloc([8, D], BF16)
    wv = A.alloc([8, D], BF16)
    wq = A.alloc([8, D], BF16)
    wo = A.alloc([8, D], BF16)
    load_w(C, wk, dr['w_xk'][li])
    load_w(C, wv, dr['w_xv'][li])
    load_w(C, wq, dr['w_xq'][li])
    load_w(C, wo, dr['w_xo'][li])
    P.memset('dve', Vx[:, :, :, 256:258], 1.0)
    n = 0
    for h in range(4):
        for c in range(2):
            pp = ps[n % 2][:, 0:MEM]
            n += 1
            col = h * 256 + c * 128
            for k in range(8):
                P.mm(pp, wk[:, k, col:col + 128], C.memT[:, k, :], start=(k == 0), stop=(k == 7))
            P.copy('act', kTx[:, h, c, :], pp)
    for mb in range(2):
        for hc in range(2):
            pp = ps[2 + hc]
            for k in range(8):
                P.mm(pp, C.memT[:, k, mb * 128:(mb + 1) * 128], wv[:, k, hc * 512:(hc + 1) * 512], start=(k == 0), stop=(k == 7))
            P.copy('act', Vx[:, mb, hc * 2:hc * 2 + 2, 0:256], pp.rearrange("p (h d) -> p h d", h=2))
    L = ln_setup(C, dr['ln2_g'][li:li + 1, :], dr['ln2_b'][li:li + 1, :])
    qTx = [A.alloc([8, 512], BF16) for _ in range(2)]
    ox = [A.alloc([4, D], BF16) for _ in range(2)]
    oT = [A.alloc([8, 128], BF16) for _ in range(2)]
    xo = [A.alloc([D], F32) for _ in range(2)]
    rec = [A.alloc([1], F32) for _ in range(4)]
    NPX = 8
    pT = [A.alloc([512], BF16) for _ in range(NPX)]
    cnt = {'npt': 0, 'nr': 0, 'no': 0}
    jobs = []
    for qt in range(4):
        for h in range(4):
            def s1(qt=qt, h=h):
                qx = qTx[qt % 2]
                if h == 0:
                    for j in range(8):
                        pp = ps[j % 2]
                        for k in range(8):
                            P.mm(pp, wq[:, k, j * 128:(j + 1) * 128], xT[:, k, qt * 512:(qt + 1) * 512], start=(k == 0), stop=(k == 7))
                        P.copy('act', qx[:, j, :], pp)
                pts = []
                for mb in range(2):
                    sp_ = ps[2 + mb]
                    for c in range(2):
                        P.mm(sp_, kTx[:, h, c, mb * 128:(mb + 1) * 128], qx[:, 2 * h + c, :], start=(c == 0), stop=(c == 1))
                    pt = pT[cnt['npt'] % NPX]
                    cnt['npt'] += 1
                    P.act(pt, sp_, AF.Exp, scale=1.0 / 16.0)
                    pts.append(pt)
                return pts

            def s2(pts, qt=qt, h=h):
                oxx = ox[qt % 2]
                for sub in range(4):
                    po = ps[4 + (cnt['no'] % 2)][:, 0:257]
                    cnt['no'] += 1
                    for mb in range(2):
                        P.mm(po, pts[mb][:, sub * 128:(sub + 1) * 128], Vx[:, mb, h, 0:257], start=(mb == 0), stop=(mb == 1))
                    rc = rec[cnt['nr'] % 4]
                    cnt['nr'] += 1
                    P.op('dve', lambda e, rc=rc, po=po: e.reciprocal(rc, po[:, 256:257]), reads=[po[:, 256:257]], writes=[rc])
                    P.ts('dve', oxx[:, sub, h * 256:(h + 1) * 256], po[:, 0:256], rc, ALU.mult)
                if h == 3:
                    for sub in range(4):
                        tile = qt * 4 + sub
                        i2 = sub % 2
                        pv = psb[6].rearrange("p (c q) -> p c q", c=8)
                        for c in range(8):
                            P.tr(pv[:, c, :], oxx[:, sub, c * 128:(c + 1) * 128], C.ident)
                        P.copy('act', oT[i2], pv)
                        P.dma('sp', xo[i2], dr['xs'][tile * 128:(tile + 1) * 128, :])
                        for hc in range(2):
                            pp = ps[hc]
                            for k in range(8):
                                P.mm(pp, oT[i2][:, k, :], wo[:, k, hc * 512:(hc + 1) * 512], start=(k == 0), stop=(k == 7))
                            P.stt(xo[i2][:, hc * 512:(hc + 1) * 512], xo[i2][:, hc * 512:(hc + 1) * 512], ALPHA, pp, ALU.mult, ALU.add)
                        ln_tile(C, L, xo[i2], tile, dr['xs'], psb[7])
            jobs.append((s1, s2))
    run_pipe(jobs, 2)
    A.release(m0)


def stage_mixA(C, li):
    P, A, dr = C.P, C.A, C.dr
    ps, psb, xT = C.ps, C.psb, C.xT
    m0 = A.mark()
    accA = A.alloc([4, S], F32)
    qT = A.alloc([2, S], BF16)
    kT = A.alloc([2, S], BF16)
    Va = A.alloc([16, 4, 128], BF16)
    wq = A.alloc([8, 256], BF16)
    wk = A.alloc([8, 256], BF16)
    wv = A.alloc([8, 256], BF16)
    bq = A.alloc([2], F32)
    bk = A.alloc([2], F32)
    bvr = A.alloc([256], BF16)
    NPA = 8
    pT = [A.alloc([256], BF16) for _ in range(NPA)]
    P.memset('dve', Va[:, :, :, 64:128], 1.0)
    DIL = [1, 4, 16]
    npt = 0
    nsp = 0
    nob = 0
    for g in range(3):
        dil = DIL[g]
        nb = S // dil // 128
        load_w(C, wq, dr['w_in'][li][:, O_AQ + g * 256:O_AQ + (g + 1) * 256])
        load_w(C, wk, dr['w_in'][li][:, O_AK + g * 256:O_AK + (g + 1) * 256])
        load_w(C, wv, dr['w_in'][li][:, O_AV + g * 256:O_AV + (g + 1) * 256])
        for j in range(2):
            bias_col(C, bq[:, j:j + 1], dr['b_in'][li:li + 1, O_AQ + g * 256 + j * 128:O_AQ + g * 256 + (j + 1) * 128])
            bias_col(C, bk[:, j:j + 1], dr['b_in'][li:li + 1, O_AK + g * 256 + j * 128:O_AK + g * 256 + (j + 1) * 128])
        P.dma('pool', bvr[0:1, :], dr['b_in'][li:li + 1, O_AV + g * 256:O_AV + (g + 1) * 256])
        n = 0
        for (w_, b_, dst) in ((wq, bq, qT), (wk, bk, kT)):
            for j in range(2):
                for qt in range(4):
                    pp = ps[n % 2]
                    n += 1
                    for k in range(8):
                        P.mm(pp, w_[:, k, j * 128:(j + 1) * 128], xT[:, k, qt * 512:(qt + 1) * 512], start=(k == 0), stop=(k == 7))
                    P.act(dst[:, j, qt * 512:(qt + 1) * 512], pp, AF.Identity, bias=b_[:, j:j + 1])

        def toks(r, blk, cnt):
            st = blk * 128 * dil + r
            return slice(st, st + (cnt - 1) * dil + 1, dil)

        for r in range(dil):
            for blk in range(nb):
                bi = r * nb + blk
                pp = ps[2 + bi % 2][:, 0:256]
                for k in range(8):
                    P.mm(pp, xT[:, k, toks(r, blk, 128)], wv[:, k, :], start=(k == 0), stop=False)
                P.mm(pp, C.ones[0:1, 0:128], bvr[0:1, :], start=False, stop=True)
                P.copy('act', Va[:, bi, :, 0:64], pp.rearrange("p (h d) -> p h d", h=4))
        jobs = []
        for h in range(4):
            for r0 in range(0, dil, 4 if g == 2 else 1):
                rs_ = list(range(r0, min(dil, r0 + (4 if g == 2 else 1))))
                if g < 2:
                    batches = [[(rs_[0], qb) for qb in range(b0, b0 + 4)] for b0 in range(0, nb, 4)]
                else:
                    batches = [[(r, 0) for r in rs_]]
                prevp = {}
                for batch in batches:
                    bst = {'ob': None}
                    for si, (r, qb) in enumerate(batch):
                        def s1(h=h, r=r, qb=qb):
                            nonlocal nsp, npt
                            j = h // 2
                            pb = (h % 2) * 64
                            nq = 256 if qb < nb - 1 else 128
                            sp_ = ps[4 + nsp % 2][:, 0:nq]
                            nsp += 1
                            P.mm(sp_, kT[pb:pb + 64, j, toks(r, qb, 128)], qT[pb:pb + 64, j, toks(r, qb, nq)], start=True, stop=True)
                            pt = pT[npt % NPA]
                            npt += 1
                            P.act(pt[:, 0:nq], sp_, AF.Exp, scale=0.125)
                            P.tt('dve', pt[:, 0:nq], pt[:, 0:nq], C.maska[:, 0:nq], ALU.mult)
                            return pt

                        def s2(pt, h=h, r=r, qb=qb, si=si, batch=batch, bst=bst, prevp=prevp, rs_=rs_):
                            nonlocal nob
                            if bst['ob'] is None:
                                bst['ob'] = ps[6 + nob % 2]
                                nob += 1
                            ob = bst['ob']
                            oo = ob[:, si * 128:(si + 1) * 128]
                            first = True
                            if qb > 0:
                                pp_ = prevp[(r, qb - 1)]
                                P.mm(oo, Va[:, r * nb + qb - 1, h, :], pp_[:, 128:256], start=True, stop=False)
                                first = False
                            P.mm(oo, Va[:, r * nb + qb, h, :], pt[:, 0:128], start=first, stop=True)
                            prevp[(r, qb)] = pt
                            if si == len(batch) - 1:
                                nbk = len(batch)
                                if g < 2:
                                    r_, qb0 = batch[0]
                                    st = qb0 * 128 * dil + r_
                                    dst = accA[:, h, st:st + (nbk * 128 - 1) * dil + 1:dil]
                                    src = ob[:, 0:nbk * 128]
                                else:
                                    dst = accA[:, h, :].rearrange("p (i r) -> p i r", r=16)[:, :, rs_[0]:rs_[0] + nbk]
                                    src = ob[:, 0:nbk * 128].rearrange("p (r i) -> p i r", r=nbk)
                                if g == 0:
                                    P.copy('act', dst, src)
                                else:
                                    P.tt('dve', dst, dst, src, ALU.add)
                        jobs.append((s1, s2))
        run_pipe(jobs, 2)
    rt = A.alloc([S], F32)
    rsft = A.alloc([S], F32)
    yb_ = [A.alloc([S], BF16) for _ in range(2)]
    for h in range(4):
        P.op('dve', lambda e, h=h: e.reciprocal(rt[64:128, :], accA[64:128, h, :]), reads=[accA[64:128, h, :]], writes=[rt[64:128, :]])
        P.copy('act', rsft[0:64, :], rt[64:128, :])
        P.tt('dve', yb_[h % 2][0:64, :], accA[0:64, h, :], rsft[0:64, :], ALU.mult)
        P.dma('sp', dr['ysa'][h], yb_[h % 2][0:64, :])
    A.release(m0)


def stage_mixB(C, li):
    P, A, dr = C.P, C.A, C.dr
    ps, psb, xT = C.ps, C.psb, C.xT
    m0 = A.mark()
    d0 = A.alloc([S], F32)
    P.memset('dve', d0, 1.0)
    P.memset('dve', d0.rearrange("p (t i) -> p t i", i=128)[:, :, 0:1], 0.0)
    wlr = A.alloc([8, 16], BF16)
    load_w(C, wlr, dr['w_in'][li][:, O_BLR:O_BLR + 16])
    blr = A.alloc([1], F32)
    bias_col(C, blr[0:16, :], dr['b_in'][li:li + 1, O_BLR:O_BLR + 16])
    lrT = A.alloc([S], BF16)
    for qt in range(4):
        pp = ps[qt % 2][0:16, :]
        for k in range(8):
            P.mm(pp, wlr[:, k, :], xT[:, k, qt * 512:(qt + 1) * 512], start=(k == 0), stop=(k == 7))
        P.act(lrT[0:16, qt * 512:(qt + 1) * 512], pp, AF.Identity, bias=blr[0:16, :])
    wa2 = A.alloc([512], BF16)
    P.dma('pool', wa2[0:16, :], dr['w_alpha2'][li])
    bal = A.alloc([4], F32)
    for h in range(4):
        bias_col(C, bal[:, h:h + 1], dr['b_alpha'][li:li + 1, h * 128:(h + 1) * 128])
    nbal = A.alloc([4], F32)
    P.ts('dve', nbal, bal, -1.0, ALU.mult)
    ngb = A.alloc([256], F32)
    P.dma('sp', ngb, dr['gla_norm_g'][li:li + 1, :].broadcast_to([128, 256]))
    cs = A.alloc([S], F32)
    wq = A.alloc([8, 128], BF16)
    wk = A.alloc([8, 128], BF16)
    wv = A.alloc([8, 256], BF16)
    wr = A.alloc([8, 256], BF16)
    bq = A.alloc([1], F32)
    bk = A.alloc([1], F32)
    bvr = A.alloc([256], BF16)
    brr = A.alloc([256], BF16)
    eqs = [A.alloc([S], F32) for _ in range(2)]
    enbs = [A.alloc([S], F32) for _ in range(2)]
    elasts = [A.alloc([NT], F32) for _ in range(2)]
    qtls = [A.alloc([S], BF16) for _ in range(2)]
    ktls = [A.alloc([S], BF16) for _ in range(2)]
    Vs_ = [A.alloc([NT, 256], BF16) for _ in range(2)]
    srs = [A.alloc([NT, 256], F32) for _ in range(2)]
    ybts = [A.alloc([NT, 256], BF16) for _ in range(2)]
    Sts = [A.alloc([256], F32) for _ in range(2)]
    Sbs = [A.alloc([256], BF16) for _ in range(2)]
    aTms = [A.alloc([128], BF16) for _ in range(2)]
    kTts = [A.alloc([128], BF16) for _ in range(2)]
    ssqs = [A.alloc([1], F32) for _ in range(2)]
    sds = [A.alloc([1], F32) for _ in range(2)]
    rstds = [A.alloc([1], F32) for _ in range(2)]
    junks = [A.alloc([256], F32) for _ in range(2)]
    LNQ = float(np.log(128.0 ** -0.5))
    ysb_v = dr['ysb'].rearrange("(t p) c -> p t c", p=128)

    def setup(h, i):
        eq, enb, elast, qtl, ktl, V, sr = eqs[i], enbs[i], elasts[i], qtls[i], ktls[i], Vs_[i], srs[i]
        load_w(C, wq, dr['w_in'][li][:, O_BQ + h * 128:O_BQ + (h + 1) * 128])
        load_w(C, wk, dr['w_in'][li][:, O_BK + h * 128:O_BK + (h + 1) * 128])
        load_w(C, wv, dr['w_in'][li][:, O_BV + h * 256:O_BV + (h + 1) * 256])
        load_w(C, wr, dr['w_in'][li][:, O_BR + h * 256:O_BR + (h + 1) * 256])
        bias_col(C, bq, dr['b_in'][li:li + 1, O_BQ + h * 128:O_BQ + (h + 1) * 128])
        bias_col(C, bk, dr['b_in'][li:li + 1, O_BK + h * 128:O_BK + (h + 1) * 128])
        P.dma('pool', bvr[0:1, :], dr['b_in'][li:li + 1, O_BV + h * 256:O_BV + (h + 1) * 256])
        P.dma('pool', brr[0:1, :], dr['b_in'][li:li + 1, O_BR + h * 256:O_BR + (h + 1) * 256])
        for qt in range(4):
            pp = ps[qt % 2]
            P.mm(pp, wa2[0:16, h * 128:(h + 1) * 128], lrT[0:16, qt * 512:(qt + 1) * 512], start=True, stop=True)
            P.act(eq[:, qt * 512:(qt + 1) * 512], pp, AF.Exp, bias=nbal[:, h:h + 1], scale=-1.0)
        P.act(enb, eq, AF.Ln, bias=1.0)
        P.op('dve', lambda e: e.tensor_tensor_scan(cs, d0, enb, 0.0, ALU.mult, ALU.add), reads=[d0, enb], writes=[cs])
        P.act(eq, cs, AF.Exp, scale=-1.0 / 16.0, bias=LNQ)
        P.act(enb, cs, AF.Exp, scale=1.0 / 16.0)
        P.act(elast, cs.rearrange("p (t i) -> p t i", i=128)[:, :, 127], AF.Exp, scale=-1.0 / 16.0)
        for qt in range(4):
            sl = slice(qt * 512, (qt + 1) * 512)
            pp = ps[qt % 2]
            for k in range(8):
                P.mm(pp, wq[:, k, :], xT[:, k, sl], start=(k == 0), stop=(k == 7))
            P.stt(qtl[:, sl], pp, bq, eq[:, sl], ALU.add, ALU.mult)
            pp2 = ps[2 + qt % 2]
            for k in range(8):
                P.mm(pp2, wk[:, k, :], xT[:, k, sl], start=(k == 0), stop=(k == 7))
            P.stt(ktl[:, sl], pp2, bk, enb[:, sl], ALU.add, ALU.mult)
        for t in range(NT):
            tsl = slice(t * 128, (t + 1) * 128)
            pp = ps[t % 2][:, 0:256]
            for k in range(8):
                P.mm(pp, xT[:, k, tsl], wv[:, k, :], start=(k == 0), stop=False)
            P.mm(pp, C.ones[0:1, 0:128], bvr[0:1, :], start=False, stop=True)
            P.copy('act', V[:, t, :], pp)
            pp2 = ps[2 + t % 2][:, 0:256]
            for k in range(8):
                P.mm(pp2, xT[:, k, tsl], wr[:, k, :], start=(k == 0), stop=False)
            P.mm(pp2, C.ones[0:1, 0:128], brr[0:1, :], start=False, stop=True)
            P.act(sr[:, t, :], pp2, AF.Silu)
            P.tt('pool', sr[:, t, :], sr[:, t, :], ngb, ALU.mult)

    def step(h, i, t):
        elast, qtl, ktl, V, sr, ybt = elasts[i], qtls[i], ktls[i], Vs_[i], srs[i], ybts[i]
        St, Sb = Sts[i], Sbs[i]
        tsl = slice(t * 128, (t + 1) * 128)
        pa = ps[4 + i][:, 0:128]
        P.mm(pa, ktl[:, tsl], qtl[:, tsl], start=True, stop=True)
        P.tt('dve', aTms[i], pa, C.maskd[:, 0, 0:128], ALU.mult)
        po = ps[6 + i][:, 0:256]
        P.mm(po, aTms[i], V[:, t, :], start=True, stop=(t == 0))
        if t > 0:
            P.mm(po, qtl[:, tsl], Sb, start=False, stop=True)
        P.act(junks[i], po, AF.Square, accum_out=ssqs[i])
        P.act(sds[i], ssqs[i], AF.Sqrt, scale=1.0 / 256.0, bias=EPS)
        P.op('dve', lambda e: e.reciprocal(rstds[i], sds[i]), reads=[sds[i]], writes=[rstds[i]])
        P.stt(ybt[:, t, :], po, rstds[i], sr[:, t, :], ALU.mult, ALU.mult)
        if t < NT - 1:
            pk = psb[4 + i][:, 256:384]
            P.tr(pk, ktl[:, tsl], C.ident)
            P.copy('act', kTts[i], pk)
            pm = ps[2 + i][:, 256:512]
            P.mm(pm, kTts[i], V[:, t, :], start=True, stop=True)
            if t == 0:
                P.ts('dve', St, pm, elast[:, 0:1], ALU.mult)
            else:
                P.tt('dve', St, St, pm, ALU.add)
                P.ts('dve', St, St, elast[:, t:t + 1], ALU.mult)
            P.copy('act', Sb, St)

    for hp in (0, 2):
        for i in range(2):
            setup(hp + i, i)
        for t in range(NT):
            for i in range(2):
                step(hp + i, i, t)
        for i in range(2):
            h = hp + i
            P.dma('sp', ysb_v[:, :, h * 256:(h + 1) * 256], ybts[i])
    A.release(m0)


def stage_mixC(C, li):
    P, A, dr = C.P, C.A, C.dr
    ps, psb, xT = C.ps, C.psb, C.xT
    m0 = A.mark()
    cmpv = A.alloc([S], BF16)
    P.dma('pool', cmpv, dr['c_cmpvalid'])
    keep = A.alloc([NT, 32], F32)
    addb = A.alloc([NT, 32], F32)
    P.dma('sp', keep, dr['c_keep'].rearrange("(t p) j -> p t j", p=128))
    P.dma('sp', addb, dr['c_add'].rearrange("(t p) j -> p t j", p=128))
    wcg = A.alloc([8, 48], BF16)
    load_w(C, wcg, dr['w_in'][li][:, O_CG:O_CG + 48])
    bcg = A.alloc([48], BF16)
    P.dma('pool', bcg[0:1, :], dr['b_in'][li:li + 1, O_CG:O_CG + 48])
    gates = A.alloc([NT, 48], F32)
    for t in range(NT):
        pp = ps[t % 2][:, 0:48]
        for k in range(8):
            P.mm(pp, xT[:, k, t * 128:(t + 1) * 128], wcg[:, k, :], start=(k == 0), stop=False)
        P.mm(pp, C.ones[0:1, 0:128], bcg[0:1, :], start=False, stop=True)
        P.act(gates[:, t, :], pp, AF.Sigmoid)
    wq = A.alloc([8, 256], BF16)
    wkc = A.alloc([8, 64], BF16)
    wvc = A.alloc([8, 64], BF16)
    wks = A.alloc([8, 64], BF16)
    wkw = A.alloc([8, 64], BF16)
    wvs = A.alloc([8, 64], BF16)
    wvw = A.alloc([8, 64], BF16)
    bq = A.alloc([4], F32)
    bkc = A.alloc([1], F32)
    bvc = A.alloc([1], F32)
    bks = A.alloc([1], F32)
    bkw = A.alloc([1], F32)
    bvs = A.alloc([64], BF16)
    bvw = A.alloc([64], BF16)
    qT = A.alloc([4, S], BF16)
    negst = A.alloc([S], BF16)
    kcin = A.alloc([S], BF16)
    vcin = A.alloc([S], BF16)
    ksd = A.alloc([S], BF16)
    kwd = A.alloc([S], BF16)
    VS = A.alloc([NT, 66], BF16)
    VW = A.alloc([NT, 66], BF16)
    P.memset('dve', qT[64:128, :, :], 0.0)
    P.memset('dve', ksd[64:128, :], 0.0)
    P.memset('dve', kwd[64:128, :], 0.0)
    P.dma('pool', ksd[64:96, :], dr['c_eexp'])
    P.memset('dve', VS[:, :, 64:66], 1.0)
    P.memset('dve', VW[:, :, 64:66], 1.0)
    w1k = A.alloc([32, 128], BF16)
    w1v = A.alloc([32, 128], BF16)
    w2kd = A.alloc([128], BF16)
    w2v = A.alloc([64], BF16)
    pe_raw = A.alloc([64], F32)
    peT = A.alloc([2, 32], BF16)
    hb = A.alloc([2], F32)
    gh = A.alloc([2, 128], BF16)
    kcTd = A.alloc([128], BF16)
    VC = A.alloc([98], BF16)
    ovl = A.alloc([32], BF16)
    P.dma('pool', ovl, dr['c_overlap'])
    yc = A.alloc([NT, 256], F32)
    ycb = A.alloc([NT, 256], BF16)
    impacc = A.alloc([NT, 32], F32)
    NPT = 6
    pT = [A.alloc([512], BF16) for _ in range(NPT)]
    rec = [A.alloc([4], F32) for _ in range(4)]
    gsc = [A.alloc([4], F32) for _ in range(4)]
    imadj = [A.alloc([32], F32) for _ in range(2)]
    imw = [A.alloc([32], F32) for _ in range(2)]
    m8a = [A.alloc([8], F32) for _ in range(2)]
    m8b = [A.alloc([8], F32) for _ in range(2)]
    selm = [A.alloc([32], F32) for _ in range(2)]
    selb = [A.alloc([32], BF16) for _ in range(2)]
    P.dma('pool', w1k[0:64, :, :], dr['cmp_w1_k'][li].rearrange("(l d) h -> d l h", d=64))
    P.dma('pool', w1v[0:64, :, :], dr['cmp_w1_v'][li].rearrange("(l d) h -> d l h", d=64))
    P.dma('pool', w2kd[:, 0:64], dr['cmp_w2_k'][li])
    P.dma('pool', w2kd[:, 64:128], dr['cmp_w2_k'][li])
    P.dma('pool', w2v, dr['cmp_w2_v'][li])
    for wi, (pn, w1) in enumerate((('cmp_pe_k', w1k), ('cmp_pe_v', w1v))):
        P.dma('sp', pe_raw[0:32, :], dr[pn][li])
        pp = ps[wi][0:64, 0:32]
        P.tr(pp, pe_raw[0:32, :], C.identf[0:32, 0:32])
        P.copy('act', peT[0:64, wi, :], pp)
        pb_ = ps[2 + wi][:, 0:1]
        for l in range(32):
            P.mm(pb_, w1[0:64, l, :], peT[0:64, wi, l:l + 1], start=(l == 0), stop=(l == 31))
        P.copy('act', hb[:, wi:wi + 1], pb_)
    ysc_v = dr['ysc'].rearrange("(t p) c -> p t c", p=128)
    nsp = 0
    npt = 0
    nob = 0
    nrc = 0

    def evac(po, W, qt, gcol, r, first_y, imp_mode):
        nonlocal nrc
        rc = rec[nrc % 4]
        gs = gsc[nrc % 4]
        nrc += 1
        if imp_mode is not None:
            P.ts('dve', rc, po[:, :, 64], 1e-30, ALU.max)
            P.op('dve', lambda e: e.reciprocal(rc, rc), reads=[rc], writes=[rc])
        else:
            P.op('dve', lambda e: e.reciprocal(rc, po[:, :, 64]), reads=[po[:, :, 64]], writes=[rc])
        P.tt('dve', gs, rc, gates[:, qt * 4:(qt + 1) * 4, gcol], ALU.mult)
        for sub in range(4):
            t = qt * 4 + sub
            ysl = yc[:, t, r * 64:(r + 1) * 64]
            if first_y:
                P.ts('dve', ysl, po[:, sub, 0:64], gs[:, sub:sub + 1], ALU.mult)
            else:
                P.stt(ysl, po[:, sub, 0:64], gs[:, sub:sub + 1], ysl, ALU.mult, ALU.add)
            if imp_mode == 'first':
                P.ts('dve', impacc[:, t, :], po[:, sub, 65:97], rc[:, sub:sub + 1], ALU.mult)
            elif imp_mode == 'add':
                P.stt(impacc[:, t, :], po[:, sub, 65:97], rc[:, sub:sub + 1], impacc[:, t, :], ALU.mult, ALU.add)

    for g in range(4):
        cq0 = O_CQ + g * 256
        load_w(C, wq, dr['w_in'][li][:, cq0:cq0 + 256])
        load_w(C, wkc, dr['w_in'][li][:, O_CKC + g * 64:O_CKC + (g + 1) * 64])
        load_w(C, wvc, dr['w_in'][li][:, O_CVC + g * 64:O_CVC + (g + 1) * 64])
        load_w(C, wks, dr['w_in'][li][:, O_CKS + g * 64:O_CKS + (g + 1) * 64])
        load_w(C, wkw, dr['w_in'][li][:, O_CKW + g * 64:O_CKW + (g + 1) * 64])
        load_w(C, wvs, dr['w_in'][li][:, O_CVS + g * 64:O_CVS + (g + 1) * 64])
        load_w(C, wvw, dr['w_in'][li][:, O_CVW + g * 64:O_CVW + (g + 1) * 64])
        for r in range(4):
            bias_col(C, bq[0:64, r:r + 1], dr['b_in'][li:li + 1, cq0 + r * 64:cq0 + (r + 1) * 64])
        bias_col(C, bkc[0:64, :], dr['b_in'][li:li + 1, O_CKC + g * 64:O_CKC + (g + 1) * 64])
        bias_col(C, bvc[0:64, :], dr['b_in'][li:li + 1, O_CVC + g * 64:O_CVC + (g + 1) * 64])
        bias_col(C, bks[0:64, :], dr['b_in'][li:li + 1, O_CKS + g * 64:O_CKS + (g + 1) * 64])
        bias_col(C, bkw[0:64, :], dr['b_in'][li:li + 1, O_CKW + g * 64:O_CKW + (g + 1) * 64])
        P.dma('pool', bvs[0:1, :], dr['b_in'][li:li + 1, O_CVS + g * 64:O_CVS + (g + 1) * 64])
        P.dma('pool', bvw[0:1, :], dr['b_in'][li:li + 1, O_CVW + g * 64:O_CVW + (g + 1) * 64])
        n = 0
        jobs = [(wq[:, :, r * 64:(r + 1) * 64], bq[0:64, r:r + 1], qT[0:64, r, :], 64) for r in range(4)]
        jobs += [(wkc, bkc[0:64, :], kcin[0:64, :], 64), (wvc, bvc[0:64, :], vcin[0:64, :], 64),
                 (wks, bks[0:64, :], ksd[0:64, :], 64), (wkw, bkw[0:64, :], kwd[0:64, :], 64)]
        for (w_, b_, dst, m_) in jobs:
            for qt in range(4):
                pp = ps[n % 2][0:m_, :]
                n += 1
                for k in range(8):
                    P.mm(pp, w_[:, k, :], xT[:, k, qt * 512:(qt + 1) * 512], start=(k == 0), stop=(k == 7))
                P.act(dst[:, qt * 512:(qt + 1) * 512], pp, AF.Identity, bias=b_)
        for (w_, br_, dst) in ((wvs, bvs, VS), (wvw, bvw, VW)):
            for t in range(NT):
                pp = ps[2 + t % 2][:, 0:64]
                for k in range(8):
                    P.mm(pp, xT[:, k, t * 128:(t + 1) * 128], w_[:, k, :], start=(k == 0), stop=False)
                P.mm(pp, C.ones[0:1, 0:128], br_[0:1, :], start=False, stop=True)
                P.copy('act', dst[:, t, 0:64], pp)
        for wi, (src, w1) in enumerate(((kcin, w1k), (vcin, w1v))):
            ph = ps[4 + wi][:, 0:127]
            for l in range(32):
                P.mm(ph, w1[0:64, l, :], src[0:64, l:l + 16 * 126 + 1:16], start=(l == 0), stop=(l == 31))
            P.act(gh[:, wi, 0:127], ph, AF.Gelu_apprx_tanh, bias=hb[:, wi:wi + 1])
        pk = ps[6][:, 0:127]
        P.mm(pk, w2kd, gh[:, 0, 0:127], start=True, stop=True)
        P.memset('dve', kcTd, 0.0)
        P.copy('act', kcTd[0:64, 0:127], pk[0:64, :])
        pv_ = ps[7][0:127, 0:64]
        P.mm(pv_, gh[:, 1, 0:127], w2v, start=True, stop=True)
        P.memset('dve', VC, 0.0)
        P.copy('act', VC[0:127, 0:64], pv_)
        P.memset('dve', VC[0:127, 64:65], 1.0)
        P.copy('dve', VC[0:127, 65:97], ovl[0:127, :])
        jobs = []
        for r in range(4):
            for qt in range(4):
                def s1(r=r, qt=qt):
                    nonlocal nsp, npt
                    qs = slice(qt * 512, (qt + 1) * 512)
                    sp_ = ps[nsp % 4][0:127, :]
                    nsp += 1
                    P.mm(sp_, kcTd[:, 0:127], qT[:, r, qs], start=True, stop=True)
                    pt = pT[npt % NPT]
                    npt += 1
                    P.act(pt[0:127, :], sp_, AF.Exp, scale=0.125)
                    P.tt('dve', pt[0:127, :], pt[0:127, :], cmpv[0:127, qs], ALU.mult)
                    return pt

                def s2(pt, r=r, qt=qt):
                    nonlocal nob
                    gcol0 = (g * 4 + r) * 3
                    po = ps[4 + nob % 4].rearrange("p (s w) -> p s w", s=4)
                    nob += 1
                    for sub in range(4):
                        P.mm(po[:, sub, 0:97], pt[0:127, sub * 128:(sub + 1) * 128], VC[0:127, 0:97], start=True, stop=True)
                    evac(po, 97, qt, gcol0 + 0, r, True, 'first' if r == 0 else 'add')
                jobs.append((s1, s2))
        run_pipe(jobs, 2)
        for t in range(NT):
            i2 = t % 2
            P.tt('dve', imadj[i2], impacc[:, t, :], keep[:, t, :], ALU.mult)
            P.tt('dve', imadj[i2], imadj[i2], addb[:, t, :], ALU.add)
            P.op('dve', lambda e, i2=i2: e.max(m8a[i2], imadj[i2]), reads=[imadj[i2]], writes=[m8a[i2]])
            P.op('dve', lambda e, i2=i2: e.match_replace(imw[i2], m8a[i2], imadj[i2], -1e9),
                 reads=[m8a[i2], imadj[i2]], writes=[imw[i2]])
            P.op('dve', lambda e, i2=i2: e.max(m8b[i2], imw[i2]), reads=[imw[i2]], writes=[m8b[i2]])
            P.ts('dve', selm[i2], imadj[i2], m8b[i2][:, 7:8], ALU.is_ge)
            P.ts('dve', selb[i2], selm[i2], -1.0, ALU.add, -NEGBIG, ALU.mult)
            pn = psb[t % 2][0:32, 0:128]
            P.tr(pn, selb[i2], C.ident)
            P.copy('act', negst[64:96, t * 128:(t + 1) * 128], pn)
        for r in range(4):
            P.copy('pool' if r % 2 else 'dve', qT[64:96, r, :], negst[64:96, :])
        jobs = []
        for r in range(4):
            for qt in range(4):
                for br in (1, 2):
                    st_ = {'po': None, 'first': True}
                    kb_lo = 0 if br == 1 else max(0, 4 * qt - 4)
                    kb_hi = 4 * qt + 3
                    for kb in range(kb_lo, kb_hi + 1):
                        def s1(r=r, qt=qt, br=br, kb=kb):
                            nonlocal nsp, npt
                            qs = slice(qt * 512, (qt + 1) * 512)
                            ksrc = ksd if br == 1 else kwd
                            ks_ = slice(kb * 128, (kb + 1) * 128)
                            sp_ = ps[nsp % 4]
                            nsp += 1
                            P.mm(sp_, ksrc[:, ks_], qT[:, r, qs], start=True, stop=True)
                            pt = pT[npt % NPT]
                            npt += 1
                            P.act(pt, sp_, AF.Exp, scale=0.125)
                            d = kb - 4 * qt
                            if d >= 0:
                                P.tt('dve', pt, pt, C.maskd[:, d, :], ALU.mult)
                            elif br == 2:
                                P.tt('dve', pt, pt, C.maskd[:, 8 + d, :], ALU.mult)
                            return pt

                        def s2(pt, r=r, qt=qt, br=br, kb=kb, st_=st_, kb_lo=kb_lo, kb_hi=kb_hi):
                            nonlocal nob
                            if st_['po'] is None:
                                st_['po'] = ps[4 + nob % 4].rearrange("p (s w) -> p s w", s=4)
                                nob += 1
                            po = st_['po']
                            vsrc = VS if br == 1 else VW
                            for sub in range(4):
                                lo = kb_lo if br == 1 else max(0, 4 * qt + sub - 4)
                                hi = 4 * qt + sub
                                if kb < lo or kb > hi:
                                    continue
                                P.mm(po[:, sub, 0:65], pt[:, sub * 128:(sub + 1) * 128], vsrc[:, kb, 0:65], start=st_['first'], stop=(kb == kb_hi and sub == 3))
                                st_['first'] = False
                            if kb == kb_hi:
                                evac(po, 65, qt, (g * 4 + r) * 3 + br, r, False, None)
                        jobs.append((s1, s2))
        run_pipe(jobs, 2)
        P.copy('act', ycb, yc)
        P.dma('sp', ysc_v[:, :, g * 256:(g + 1) * 256], ycb)
    A.release(m0)


def stage_merge(C, li):
    P, A, dr = C.P, C.A, C.dr
    ps, psb, xT = C.ps, C.psb, C.xT
    m0 = A.mark()
    mgT = A.alloc([8, S], BF16)
    m1 = A.mark()
    wbas = [A.alloc([4, 512], BF16) for _ in range(2)]
    wbbs = [A.alloc([8, 512], BF16) for _ in range(2)]
    wbcs = [A.alloc([8, 512], BF16) for _ in range(2)]
    wmgs = [A.alloc([3, 8, 512], BF16) for _ in range(2)]
    bmgs = [A.alloc([3, 512], BF16) for _ in range(2)]

    def load_half(hc):
        cs_ = slice(hc * 512, (hc + 1) * 512)
        P.dma('pool', wbas[hc][0:64, :, :], dr['w_br_a'][li][:, cs_].rearrange("(h d) n -> d h n", d=64))
        load_w(C, wbbs[hc], dr['w_br_b'][li][:, cs_])
        load_w(C, wbcs[hc], dr['w_br_c'][li][:, cs_])
        for b in range(3):
            load_w(C, wmgs[hc][:, b, :, :], dr['w_in'][li][:, O_MG + b * D + hc * 512:O_MG + b * D + (hc + 1) * 512])
            P.dma('pool', bmgs[hc][0:1, b, :], dr['b_in'][li:li + 1, O_MG + b * D + hc * 512:O_MG + b * D + (hc + 1) * 512])
    load_half(0)
    load_half(1)
    yaT = [A.alloc([4, 128], BF16) for _ in range(2)]
    ybl = [A.alloc([D], BF16) for _ in range(2)]
    ycl = [A.alloc([D], BF16) for _ in range(2)]
    ybT = [A.alloc([8, 128], BF16) for _ in range(2)]
    ycT = [A.alloc([8, 128], BF16) for _ in range(2)]
    mg = [A.alloc([512], F32) for _ in range(2)]
    sg = [A.alloc([512], F32) for _ in range(2)]
    mgb = [A.alloc([512], BF16) for _ in range(2)]
    nsg = 0
    for hc in range(2):
        wba, wbb, wbc, wmg, bmg = wbas[hc], wbbs[hc], wbcs[hc], wmgs[hc], bmgs[hc]
        for t in range(NT):
            i2 = t % 2
            tsl = slice(t * 128, (t + 1) * 128)
            P.dma('sp', yaT[i2][0:64, :, :], dr['ysa'][:, :, tsl].rearrange("h d t -> d h t"))
            P.dma('sp', ybl[i2], dr['ysb'][tsl, :])
            P.dma('sp', ycl[i2], dr['ysc'][tsl, :])
            for (src, dst, bank) in ((ybl[i2], ybT[i2], 6), (ycl[i2], ycT[i2], 7)):
                pv = psb[bank].rearrange("p (c q) -> p c q", c=8)
                for c in range(8):
                    P.tr(pv[:, c, :], src[:, c * 128:(c + 1) * 128], C.ident)
                P.copy('act', dst, pv)
            for b in range(3):
                pg = ps[b % 2]
                for k in range(8):
                    P.mm(pg, xT[:, k, tsl], wmg[:, b, k, :], start=(k == 0), stop=False)
                P.mm(pg, C.ones[0:1, 0:128], bmg[0:1, b, :], start=False, stop=True)
                s_ = sg[nsg % 2]
                nsg += 1
                P.act(s_, pg, AF.Sigmoid)
                pp = ps[2 + b % 2]
                if b == 0:
                    for h in range(4):
                        P.mm(pp, yaT[i2][0:64, h, :], wba[0:64, h, :], start=(h == 0), stop=(h == 3))
                else:
                    yT_ = ybT[i2] if b == 1 else ycT[i2]
                    w_ = wbb if b == 1 else wbc
                    for k in range(8):
                        P.mm(pp, yT_[:, k, :], w_[:, k, :], start=(k == 0), stop=(k == 7))
                if b == 0:
                    P.tt('dve', mg[i2], s_, pp, ALU.mult)
                else:
                    P.tt('dve', s_, s_, pp, ALU.mult)
                    P.tt('pool', mg[i2], mg[i2], s_, ALU.add)
            P.copy('act', mgb[i2], mg[i2])
            pv = psb[4 + i2][:, 0:512].rearrange("p (c q) -> p c q", c=4)
            for c in range(4):
                P.tr(pv[:, c, :], mgb[i2][:, c * 128:(c + 1) * 128], C.ident)
            P.copy('act', mgT[:, hc * 4:(hc + 1) * 4, tsl], pv)
    A.release(m1)
    wom = A.alloc([8, D], BF16)
    load_w(C, wom, dr['w_o_mix'][li])
    L = ln_setup(C, dr['ln1_g'][li:li + 1, :], dr['ln1_b'][li:li + 1, :])
    xo = [A.alloc([D], F32) for _ in range(2)]
    for t in range(NT):
        i2 = t % 2
        tsl = slice(t * 128, (t + 1) * 128)
        P.dma('sp', xo[i2], dr['xs'][tsl, :])
        for hc in range(2):
            cs_ = slice(hc * 512, (hc + 1) * 512)
            pp = ps[hc]
            for k in range(8):
                P.mm(pp, mgT[:, k, tsl], wom[:, k, cs_], start=(k == 0), stop=(k == 7))
            P.stt(xo[i2][:, cs_], xo[i2][:, cs_], ALPHA, pp, ALU.mult, ALU.add)
        ln_tile(C, L, xo[i2], t, dr['xs'], psb[4 + i2])
    A.release(m0)


_CACHE = {}


def _get_nc(stages, dbg=False):
    key = (tuple(stages), dbg)
    if key not in _CACHE:
        _CACHE[key] = build_nc(stages, dbg)
    return _CACHE[key]


def make_in_maps(inputs, ncores=8):
    consts = host_consts()
    shared = {}
    for n in WNAMES:
        shared[n] = np.ascontiguousarray(inputs[n], dtype=np.float32)
    shared['ln0_g'] = np.ascontiguousarray(inputs['ln0_g'], dtype=np.float32).reshape(1, D)
    shared['ln0_b'] = np.ascontiguousarray(inputs['ln0_b'], dtype=np.float32).reshape(1, D)
    shared.update(consts)
    maps = []
    for b in range(ncores):
        m = dict(shared)
        m['x'] = np.ascontiguousarray(inputs['x'][b], dtype=np.float32)
        m['mem'] = np.ascontiguousarray(inputs['mem'][b], dtype=np.float32)
        maps.append(m)
    return maps


FULL_STAGES = ['ln0', 'meminit']
for _li in range(DEPTH):
    FULL_STAGES += ['mixA%d' % _li, 'mixB%d' % _li, 'mixC%d' % _li, 'merge%d' % _li, 'xattn%d' % _li, 'moe%d' % _li]
FULL_STAGES[-1] += 'L'


def kernel(**inputs):
    nc, C = _get_nc(FULL_STAGES)
    maps = make_in_maps(inputs, 8)
    res = run_bass_kernel_spmd(nc, maps, core_ids=list(range(8)))
    out = np.stack([np.asarray(r['out']) for r in res.results], axis=0)
    return out.astype(np.float32)
```

```python
import numpy as np
import concourse.bass as bass
import concourse.mybir as mybir
from concourse.bass_utils import run_bass_kernel_spmd
from contextlib import ExitStack

DT = mybir.dt
F32 = DT.float32
BF16 = DT.bfloat16
AF = mybir.ActivationFunctionType
ALU = mybir.AluOpType
AX = mybir.AxisListType

_ISZ = {F32: 4, BF16: 2, DT.int32: 4, DT.uint32: 4, DT.uint8: 1, DT.int8: 1, DT.uint16: 2, DT.int16: 2, DT.float16: 2}


def region(ap):
    t = ap.tensor
    isz = _ISZ[ap.dtype]
    a = list(ap.ap)
    space = str(ap.space).upper()
    if 'DRAM' in space or 'HBM' in space:
        lo = ap.offset
        hi = ap.offset
        for st, n in a:
            if n > 1:
                if st >= 0:
                    hi += st * (n - 1)
                else:
                    lo += st * (n - 1)
        return (t.name, 0, 1, lo * isz, (hi + 1) * isz)
    pst, pn = a[0]
    if pst == 0:
        p0 = 0
        f0 = ap.offset
    else:
        p0 = ap.offset // pst
        f0 = ap.offset - p0 * pst
    lo = f0
    hi = f0
    for st, n in a[1:]:
        if n > 1:
            if st >= 0:
                hi += st * (n - 1)
            else:
                lo += st * (n - 1)
    return (t.name, p0, p0 + pn, lo * isz, (hi + 1) * isz)


class Prog:
    ENG = ['pe', 'dve', 'act', 'pool', 'sp']

    def __init__(self, nc, slots=None):
        self.nc = nc
        self.es = ExitStack()
        self.stream = {e: [] for e in self.ENG}
        slots = slots or {'sp': 8, 'pool': 6, 'act': 2}
        self.slots = {q: ['%s_d%d' % (q, i) for i in range(n)] for q, n in slots.items()}
        self.slot_rr = {q: 0 for q in slots}
        self.tl = ['pe', 'dve', 'act', 'pool'] + [s for q in self.slots for s in self.slots[q]]
        self.count = {t: 0 for t in self.tl}
        self.clk = {e: {} for e in self.ENG}
        self.opclk = {}
        self.track = {}
        self.sem = {}
        self.nwait = 0
        self.nop = 0
        for t in self.tl:
            self.sem[t] = self.es.enter_context(nc.semaphore('sem_' + t))

    def sb(self, name, shape, dt=F32):
        return self.es.enter_context(self.nc.sbuf_tensor(name, list(shape), dt))

    def ps(self, name, shape, dt=F32):
        return self.es.enter_context(self.nc.psum_tensor(name, list(shape), dt))

    def _deps(self, regs_r, regs_w):
        deps = {}
        for (kind, regs) in (('r', regs_r), ('w', regs_w)):
            for (name, p0, p1, b0, b1) in regs:
                for rec in self.track.get(name, ()):
                    if kind == 'r' and rec[4] == 'r':
                        continue
                    if rec[0] < p1 and p0 < rec[1] and rec[2] < b1 and b0 < rec[3]:
                        t = rec[5]
                        if rec[6] > deps.get(t, 0):
                            deps[t] = rec[6]
        return deps

    def _wait(self, S, t, seq):
        clk = self.clk[S]
        if clk.get(t, 0) >= seq:
            return
        val = seq * 16 if '_d' in t else seq
        self.stream[S].append(('w', t, val))
        self.nwait += 1
        oc = self.opclk.get((t, seq))
        if oc:
            for k, v in oc.items():
                if v > clk.get(k, 0):
                    clk[k] = v
        clk[t] = max(clk.get(t, 0), seq)

    def _record(self, regs_r, regs_w, T, seq):
        for (name, p0, p1, b0, b1) in regs_w:
            lst = self.track.setdefault(name, [])
            lst[:] = [r for r in lst if not (p0 <= r[0] and r[1] <= p1 and b0 <= r[2] and r[3] <= b1)]
            lst.append((p0, p1, b0, b1, 'w', T, seq))
        for (name, p0, p1, b0, b1) in regs_r:
            lst = self.track.setdefault(name, [])
            lst[:] = [r for r in lst if not (r[4] == 'r' and r[5] == T and p0 <= r[0] and r[1] <= p1 and b0 <= r[2] and r[3] <= b1)]
            lst.append((p0, p1, b0, b1, 'r', T, seq))

    def op(self, S, fn, reads=(), writes=(), dma=False):
        regs_r = [region(a) for a in reads if a is not None]
        regs_w = [region(a) for a in writes if a is not None]
        deps = self._deps(regs_r, regs_w)
        if dma:
            sl = self.slots[S]
            T = sl[self.slot_rr[S] % len(sl)]
            self.slot_rr[S] += 1
            if self.count[T] > 0:
                deps[T] = max(deps.get(T, 0), self.count[T])
        else:
            T = S
            if S == 'pe':
                deps.pop('pe', None)
        for t in sorted(deps, key=lambda k: -deps[k]):
            self._wait(S, t, deps[t])
        self.count[T] += 1
        seq = self.count[T]
        self.opclk[(T, seq)] = dict(self.clk[S])
        self.stream[S].append(('o', fn, T, 16 if dma else 1))
        self.nop += 1
        self._record(regs_r, regs_w, T, seq)
        return (T, seq)

    def finish(self, S='sp'):
        for t in self.tl:
            if self.count[t] > 0:
                self._wait(S, t, self.count[t])

    def emit(self):
        nc = self.nc
        sem = self.sem

        def run(items, e):
            for it in items:
                if it[0] == 'w':
                    e.wait_ge(sem[it[1]], it[2])
                else:
                    ins = it[1](e)
                    ins.then_inc(sem[it[2]], it[3])

        with nc.Block() as block:
            @block.tensor
            def _(e):
                run(self.stream['pe'], e)

            @block.vector
            def _(e):
                run(self.stream['dve'], e)

            @block.scalar
            def _(e):
                run(self.stream['act'], e)

            @block.gpsimd
            def _(e):
                run(self.stream['pool'], e)

            @block.sync
            def _(e):
                run(self.stream['sp'], e)
        self.es.close()

    def dma(self, q, out, in_, **kw):
        return self.op(q, lambda e: e.dma_start(out=out, in_=in_, **kw), reads=[in_], writes=[out], dma=True)

    def mm(self, out, lhsT, rhs, start=True, stop=True):
        return self.op('pe', lambda e: e.matmul(out, lhsT, rhs, start=start, stop=stop),
                       reads=[lhsT, rhs], writes=[out])

    def tr(self, out, in_, ident):
        return self.op('pe', lambda e: e.transpose(out, in_, ident), reads=[in_, ident], writes=[out])

    def act(self, out, in_, func, bias=None, scale=None, accum_out=None):
        kw = {}
        rd = [in_]
        if bias is not None:
            kw['bias'] = bias
            if not isinstance(bias, (int, float)):
                rd.append(bias)
        if scale is not None:
            kw['scale'] = scale
            if not isinstance(scale, (int, float)):
                rd.append(scale)
        wr = [out]
        if accum_out is not None:
            kw['accum_out'] = accum_out
            wr.append(accum_out)
        return self.op('act', lambda e: e.activation(out, in_, func, **kw), reads=rd, writes=wr)

    def tt(self, eng, out, in0, in1, op):
        return self.op(eng, lambda e: e.tensor_tensor(out, in0, in1, op), reads=[in0, in1], writes=[out])

    def ts(self, eng, out, in0, s1, op0, s2=None, op1=None):
        rd = [in0]
        if s1 is not None and not isinstance(s1, (int, float)):
            rd.append(s1)
        if s2 is not None and not isinstance(s2, (int, float)):
            rd.append(s2)
        kw = {}
        if op1 is not None:
            kw['op1'] = op1
        return self.op(eng, lambda e: e.tensor_scalar(out, in0, s1, s2, op0, **kw), reads=rd, writes=[out])

    def stt(self, out, in0, scalar, in1, op0, op1):
        rd = [in0, in1]
        if not isinstance(scalar, (int, float)):
            rd.append(scalar)
        return self.op('dve', lambda e: e.scalar_tensor_tensor(out, in0, scalar, in1, op0, op1), reads=rd, writes=[out])

    def copy(self, eng, out, in_):
        if eng == 'act':
            return self.op('act', lambda e: e.copy(out, in_), reads=[in_], writes=[out])
        return self.op(eng, lambda e: e.tensor_copy(out, in_), reads=[in_], writes=[out])

    def memset(self, eng, ap, val):
        return self.op(eng, lambda e: e.memset(ap, val), writes=[ap])


class Arena:
    def __init__(self, P, nbytes):
        self.nbytes = nbytes
        self.t32 = P.sb("arena", [128, nbytes // 4], F32)
        self.t16 = self.t32.bitcast(BF16)
        self.top = 0

    def alloc(self, shape, dt=F32):
        shape = list(shape)
        n = int(np.prod(shape))
        isz = _ISZ[dt]
        off = (self.top + 63) // 64 * 64
        self.top = off + n * isz
        assert self.top <= self.nbytes, ("arena overflow", self.top, self.nbytes)
        base = self.t32 if isz == 4 else self.t16
        ap = base[:, off // isz: off // isz + n]
        if len(shape) == 2:
            ap = ap.rearrange("p (a b) -> p a b", a=shape[0])
        elif len(shape) == 3:
            ap = ap.rearrange("p (a b c) -> p a b c", a=shape[0], b=shape[1])
        elif len(shape) == 4:
            ap = ap.rearrange("p (a b c d) -> p a b c d", a=shape[0], b=shape[1], c=shape[2])
        return ap

    def mark(self):
        return self.top

    def release(self, m):
        self.top = m


S = 2048
D = 1024
NT = 16
DEPTH = 2
MEM = 256
ALPHA = float((2 * DEPTH) ** 0.25)
EPS = 1e-5
NE = 32
NIN = 11072
O_AQ, O_AK, O_AV = 0, 768, 1536
O_BQ, O_BK, O_BV, O_BR, O_BLR = 2304, 2816, 3328, 4352, 5376
O_CQ = 5392
O_CKC, O_CVC, O_CKS, O_CVS, O_CKW, O_CVW = 6416, 6672, 6928, 7184, 7440, 7696
O_CG = 7952
O_MG = 8000
NEGBIG = -30000.0

WNAMES = ['w_in', 'b_in', 'w_alpha2', 'b_alpha', 'gla_norm_g', 'cmp_pe_k', 'cmp_w1_k', 'cmp_w2_k',
          'cmp_pe_v', 'cmp_w1_v', 'cmp_w2_v', 'w_br_a', 'w_br_b', 'w_br_c', 'w_o_mix', 'ln1_g', 'ln1_b',
          'w_xq', 'w_xk', 'w_xv', 'w_xo', 'ln2_g', 'ln2_b', 'w_router', 'b_router', 'w_gu', 'b_gu',
          'w_down', 'b_down', 'ln3_g', 'ln3_b']
WSHAPES = {
    'w_in': [2, 1024, NIN], 'b_in': [2, NIN], 'w_alpha2': [2, 16, 512], 'b_alpha': [2, 512], 'gla_norm_g': [2, 256],
    'cmp_pe_k': [2, 32, 64], 'cmp_w1_k': [2, 2048, 128], 'cmp_w2_k': [2, 128, 64],
    'cmp_pe_v': [2, 32, 64], 'cmp_w1_v': [2, 2048, 128], 'cmp_w2_v': [2, 128, 64],
    'w_br_a': [2, 256, 1024], 'w_br_b': [2, 1024, 1024], 'w_br_c': [2, 1024, 1024], 'w_o_mix': [2, 1024, 1024],
    'ln1_g': [2, 1024], 'ln1_b': [2, 1024], 'w_xq': [2, 1024, 1024], 'w_xk': [2, 1024, 1024], 'w_xv': [2, 1024, 1024],
    'w_xo': [2, 1024, 1024], 'ln2_g': [2, 1024], 'ln2_b': [2, 1024], 'w_router': [2, 1024, 32], 'b_router': [2, 32],
    'w_gu': [2, 32, 1024, 2048], 'b_gu': [2, 32, 2048], 'w_down': [2, 32, 1024, 1024], 'b_down': [2, 32, 1024],
    'ln3_g': [2, 1024], 'ln3_b': [2, 1024],
}


def host_consts():
    c = {}
    c['c_ident'] = np.eye(128, dtype=np.float32)
    p = np.arange(128)[:, None]
    q = np.arange(512)[None, :]
    md = np.zeros((128, 8, 512), np.float32)
    for d in range(4):
        md[:, d, :] = (128 * d + p <= q)
        md[:, 4 + d, :] = 1.0 - md[:, d, :]
    c['c_maskd'] = md
    ma = np.zeros((128, 256), np.float32)
    q1 = np.arange(128)[None, :]
    ma[:, 0:128] = (p <= q1)
    ma[:, 128:256] = (p >= q1)
    c['c_maska'] = ma
    cc = np.arange(128)[:, None]
    t = np.arange(S)[None, :]
    cv = ((16 * cc + 31) <= t).astype(np.float32)
    cv[127, :] = 0.0
    c['c_cmpvalid'] = cv
    ov = np.zeros((128, 32), np.float32)
    for ci in range(127):
        for j in range(32):
            if ci * 16 < j * 64 + 64 and ci * 16 + 32 > j * 64:
                ov[ci, j] = 1.0
    c['c_overlap'] = ov
    pos = np.arange(S)
    qb = pos // 64
    j = np.arange(32)[None, :]
    forced = (j == 0) | (j == qb[:, None]) | (j == qb[:, None] - 1)
    future = j > qb[:, None]
    keep = (~forced & ~future).astype(np.float32)
    add = np.where(forced, 1e4, np.where(future, -1e4, 0.0)).astype(np.float32)
    c['c_keep'] = keep
    c['c_add'] = add
    key = np.arange(S)[None, :]
    c['c_eexp'] = (key // 64 == np.arange(32)[:, None]).astype(np.float32)
    return c


CSHAPES = {'c_ident': [128, 128], 'c_maskd': [128, 8, 512], 'c_maska': [128, 256], 'c_cmpvalid': [128, S],
           'c_overlap': [128, 32], 'c_keep': [S, 32], 'c_add': [S, 32], 'c_eexp': [32, S]}


class Ctx:
    pass


def build_nc(stages, dbg=False):
    nc = bass.Bass("TRN2", target_bir_lowering=False)
    C = Ctx()
    C.nc = nc
    C.dbg = dbg
    dr = {}
    dr['x'] = nc.dram_tensor("x", [S, D], F32, kind="ExternalInput").ap()
    dr['mem'] = nc.dram_tensor("mem", [MEM, D], F32, kind="ExternalInput").ap()
    dr['ln0_g'] = nc.dram_tensor("ln0_g", [1, D], F32, kind="ExternalInput").ap()
    dr['ln0_b'] = nc.dram_tensor("ln0_b", [1, D], F32, kind="ExternalInput").ap()
    for n in WNAMES:
        dr[n] = nc.dram_tensor(n, WSHAPES[n], F32, kind="ExternalInput").ap()
    for n, shp in CSHAPES.items():
        dr[n] = nc.dram_tensor(n, shp, F32, kind="ExternalInput").ap()
    dr['out'] = nc.dram_tensor("out", [S, D], F32, kind="ExternalOutput").ap()
    sk = "ExternalOutput" if dbg else "Internal"
    dr['xs'] = nc.dram_tensor("xs", [S, D], F32, kind="Internal").ap()
    dr['ysa'] = nc.dram_tensor("ysa", [4, 64, S], BF16, kind=sk).ap()
    dr['ysb'] = nc.dram_tensor("ysb", [S, D], BF16, kind=sk).ap()
    dr['ysc'] = nc.dram_tensor("ysc", [S, D], BF16, kind=sk).ap()
    C.dr = dr
    P = Prog(nc)
    C.P = P
    A = Arena(P, 207 * 1024)
    C.A = A
    C.pst = [P.ps("psb%d" % i, [128, 512], F32) for i in range(8)]
    C.ps = [t[:, :] for t in C.pst]
    C.psb = [t.bitcast(BF16)[:, :] for t in C.pst]
    C.xT = A.alloc([8, S], BF16)
    C.ident = A.alloc([128], BF16)
    C.identf = A.alloc([128], F32)
    C.ones = A.alloc([512], BF16)
    P.dma('pool', C.ident, dr['c_ident'])
    P.dma('sp', C.identf, dr['c_ident'])
    P.memset('dve', C.ones, 1.0)
    C.memT = A.alloc([8, MEM], BF16)
    C.maskd = A.alloc([8, 512], BF16)
    C.maska = A.alloc([256], BF16)
    P.dma('pool', C.maskd, dr['c_maskd'])
    P.dma('pool', C.maska, dr['c_maska'])
    for st in stages:
        if st == 'ln0':
            stage_ln0(C, True)
        elif st == 'load':
            stage_ln0(C, False)
        elif st.startswith('moe'):
            stage_moe(C, int(st[3:4]), last=st.endswith('L'))
        elif st == 'out':
            stage_out(C)
        elif st == 'meminit':
            stage_meminit(C)
        elif st.startswith('xattn'):
            stage_xattn(C, int(st[5:]))
        elif st.startswith('mixA'):
            stage_mixA(C, int(st[4:]))
        elif st.startswith('mixB'):
            stage_mixB(C, int(st[4:]))
        elif st.startswith('mixC'):
            stage_mixC(C, int(st[4:]))
        elif st.startswith('merge'):
            stage_merge(C, int(st[5:]))
        else:
            raise ValueError(st)
    P.finish()
    P.emit()
    C.nop = P.nop
    C.nwait = P.nwait
    return nc, C


class LNState:
    pass


def ln_setup(C, g_ap, b_ap):
    P, A = C.P, C.A
    L = LNState()
    L.g = A.alloc([D], F32)
    L.b = A.alloc([D], F32)
    P.dma('sp', L.g, g_ap.broadcast_to([128, D]))
    P.dma('sp', L.b, b_ap.broadcast_to([128, D]))
    L.stats = [A.alloc([2, 6], F32) for _ in range(2)]
    L.mv = [A.alloc([2], F32) for _ in range(2)]
    L.sd = [A.alloc([1], F32) for _ in range(2)]
    L.rs = [A.alloc([1], F32) for _ in range(2)]
    L.xn = [A.alloc([D], F32) for _ in range(2)]
    L.xb = [A.alloc([D], BF16) for _ in range(2)]
    L.n = 0
    return L


def ln_tile(C, L, src, t, dst_dram, pst):
    P = C.P
    i = L.n % 2
    L.n += 1
    st, mv, sd, rs, xn, xb = L.stats[i], L.mv[i], L.sd[i], L.rs[i], L.xn[i], L.xb[i]
    for h in range(2):
        P.op('dve', lambda e, h=h: e.bn_stats(st[:, h, :], src[:, h * 512:(h + 1) * 512]),
             reads=[src[:, h * 512:(h + 1) * 512]], writes=[st[:, h, :]])
    P.op('dve', lambda e: e.bn_aggr(mv, st), reads=[st], writes=[mv])
    P.act(sd, mv[:, 1:2], AF.Sqrt, bias=EPS)
    P.op('dve', lambda e: e.reciprocal(rs, sd), reads=[sd], writes=[rs])
    P.ts('dve', xn, src, mv[:, 0:1], ALU.subtract, rs, ALU.mult)
    P.tt('dve', xn, xn, L.g, ALU.mult)
    P.tt('pool', xn, xn, L.b, ALU.add)
    P.dma('sp', dst_dram[t * 128:(t + 1) * 128, :], xn)
    P.copy('act', xb, xn)
    pv = pst.rearrange("p (c q) -> p c q", c=8)
    for c in range(8):
        P.tr(pv[:, c, :], xb[:, c * 128:(c + 1) * 128], C.ident)
    P.copy('dve', C.xT[:, :, t * 128:(t + 1) * 128], pv)


def stage_ln0(C, do_ln):
    P, A, dr = C.P, C.A, C.dr
    m = A.mark()
    L = ln_setup(C, dr['ln0_g'], dr['ln0_b'])
    xin = [A.alloc([D], F32) for _ in range(2)]
    for t in range(NT):
        xi = xin[t % 2]
        P.dma('sp', xi, dr['x'][t * 128:(t + 1) * 128, :])
        if do_ln:
            ln_tile(C, L, xi, t, dr['xs'], C.psb[t % 2])
        else:
            P.dma('sp', dr['xs'][t * 128:(t + 1) * 128, :], xi)
            xb = L.xb[t % 2]
            P.copy('act', xb, xi)
            pv = C.psb[t % 2].rearrange("p (c q) -> p c q", c=8)
            for c in range(8):
                P.tr(pv[:, c, :], xb[:, c * 128:(c + 1) * 128], C.ident)
            P.copy('dve', C.xT[:, :, t * 128:(t + 1) * 128], pv)
    A.release(m)


def stage_out(C):
    P, A, dr = C.P, C.A, C.dr
    m = A.mark()
    buf = [A.alloc([D], F32) for _ in range(2)]
    for t in range(NT):
        b = buf[t % 2]
        P.dma('sp', b, dr['xs'][t * 128:(t + 1) * 128, :])
        P.dma('sp', dr['out'][t * 128:(t + 1) * 128, :], b)
    A.release(m)


def stage_moe(C, li, last=False):
    dst_final = C.dr['out'] if last else C.dr['xs']
    P, A, dr = C.P, C.A, C.dr
    ps, psb = C.ps, C.psb
    xT = C.xT
    m0 = A.mark()
    wr = A.alloc([8, NE], BF16)
    P.dma('pool', wr, dr['w_router'][li].rearrange("(k p) n -> p k n", p=128))
    brow = A.alloc([NE], BF16)
    P.dma('pool', brow[0:1, :], dr['b_router'][li:li + 1, :])
    lg = A.alloc([NT, NE], F32)
    G = A.alloc([NT, NE], F32)
    m8 = A.alloc([NT, 8], F32)
    negmx = A.alloc([NT], F32)
    lgp = ps[0][:, :].rearrange("p (t e) -> p t e", t=NT)
    for t in range(NT):
        for k in range(8):
            P.mm(lgp[:, t, :], xT[:, k, t * 128:(t + 1) * 128], wr[:, k, :], start=(k == 0), stop=False)
        P.mm(lgp[:, t, :], C.ones[0:1, 0:128], brow[0:1, :], start=False, stop=True)
    P.copy('act', lg, lgp)
    tmpm = A.alloc([NE], F32)
    tmpe = A.alloc([NE], F32)
    ssum = A.alloc([1], F32)
    rsum = A.alloc([1], F32)
    for t in range(NT):
        P.op('dve', lambda e, t=t: e.max(m8[:, t, :], lg[:, t, :]), reads=[lg[:, t, :]], writes=[m8[:, t, :]])
    P.ts('dve', negmx, m8[:, :, 0], -1.0, ALU.mult)
    for t in range(NT):
        P.ts('dve', tmpm, lg[:, t, :], m8[:, t, 3:4], ALU.is_ge)
        P.act(tmpe, lg[:, t, :], AF.Exp, bias=negmx[:, t:t + 1])
        P.tt('dve', tmpe, tmpe, tmpm, ALU.mult)
        P.op('dve', lambda e: e.tensor_reduce(ssum, tmpe, AX.X, ALU.add), reads=[tmpe], writes=[ssum])
        P.op('dve', lambda e: e.reciprocal(rsum, ssum), reads=[ssum], writes=[rsum])
        P.ts('dve', G[:, t, :], tmpe, rsum, ALU.mult)
    bgu_raw = A.alloc([2048], F32)
    P.dma('sp', bgu_raw[0:NE, :], dr['b_gu'][li])
    bguT = A.alloc([16, NE], F32)
    bp = ps[1][:, :].rearrange("p (c e) -> p c e", c=16)
    for c in range(16):
        P.tr(bp[:, c, :], bgu_raw[0:NE, c * 128:(c + 1) * 128], C.identf[0:NE, 0:NE])
    P.copy('act', bguT, bp)
    bd = A.alloc([D], BF16)
    P.dma('pool', bd[0:NE, :], dr['b_down'][li])
    m1 = A.mark()
    NB = 8
    for half in range(2):
        A.release(m1)
        yacc = A.alloc([8, D], F32)
        m2 = A.mark()
        ring = [A.alloc([8, 512], BF16) for _ in range(NB)]
        actT = [A.alloc([8, 512], BF16) for _ in range(2)]
        tg = [A.alloc([512], F32) for _ in range(2)]
        tsg = [A.alloc([512], F32) for _ in range(2)]
        tu = [A.alloc([512], F32) for _ in range(2)]
        rr = [0]

        def getblk(src2d):
            b = ring[rr[0] % NB]
            rr[0] += 1
            P.dma('pool', b, src2d.rearrange("(k p) n -> p k n", p=128))
            return b

        pending = []
        cnt = 0
        fgc = 0
        dcn = 0

        def do_down(job):
            nonlocal dcn
            (e, tok0, aT, dblk) = job
            for sub in range(4):
                tile = (tok0 + sub * 128) // 128
                lt = tile - half * 8
                for hc in range(2):
                    pD = ps[4 + dcn % 4]
                    dcn += 1
                    for fk in range(8):
                        P.mm(pD, aT[:, fk, sub * 128:(sub + 1) * 128], dblk[hc][:, fk, :], start=(fk == 0), stop=(fk == 7))
                    ysl = yacc[:, lt, hc * 512:(hc + 1) * 512]
                    if e == 0:
                        P.ts('dve', ysl, pD, G[:, tile, e:e + 1], ALU.mult)
                    else:
                        P.stt(ysl, pD, G[:, tile, e:e + 1], ysl, ALU.mult, ALU.add)

        for e in range(NE):
            gu = [getblk(dr['w_gu'][li, e][:, b * 512:(b + 1) * 512]) for b in range(4)]
            dblk = [getblk(dr['w_down'][li, e][:, b * 512:(b + 1) * 512]) for b in range(2)]
            for qt in range(2):
                tok0 = half * 1024 + qt * 512
                aT = actT[cnt % 2]
                cnt += 1
                for fg in range(8):
                    pA = ps[(2 * fgc) % 4]
                    pB = ps[(2 * fgc + 1) % 4]
                    i2 = fgc % 2
                    fgc += 1
                    cols = (fg % 4) * 128
                    bg = gu[fg // 4]
                    bu = gu[2 + fg // 4]
                    for k in range(8):
                        P.mm(pA, bg[:, k, cols:cols + 128], xT[:, k, tok0:tok0 + 512], start=(k == 0), stop=(k == 7))
                    for k in range(8):
                        P.mm(pB, bu[:, k, cols:cols + 128], xT[:, k, tok0:tok0 + 512], start=(k == 0), stop=(k == 7))
                    g1, sg, ua = tg[i2], tsg[i2], tu[i2]
                    P.ts('dve', g1, pA, bguT[:, fg, e:e + 1], ALU.add, 7.0, ALU.min)
                    P.act(sg, g1, AF.Sigmoid, scale=1.702)
                    P.act(ua, pB, AF.Identity, bias=bguT[:, 8 + fg, e:e + 1])
                    P.ts('dve', ua, ua, 7.0, ALU.min, -7.0, ALU.max)
                    P.tt('dve', g1, g1, sg, ALU.mult)
                    P.stt(aT[:, fg, :], ua, 1.0, g1, ALU.add, ALU.mult)
                pending.append((e, tok0, aT, dblk))
                if len(pending) > 1:
                    do_down(pending.pop(0))
        while pending:
            do_down(pending.pop(0))
        A.release(m2)
        L = ln_setup(C, dr['ln3_g'][li:li + 1, :], dr['ln3_b'][li:li + 1, :])
        gTb = [A.alloc([128], BF16) for _ in range(2)]
        xo = [A.alloc([D], F32) for _ in range(2)]
        for lt in range(8):
            tile = half * 8 + lt
            i2 = lt % 2
            gp = ps[0][0:NE, 0:128]
            P.tr(gp, G[:, tile, :], C.identf)
            P.copy('act', gTb[i2][0:NE, :], gp)
            for hc in range(2):
                P.mm(ps[2 + hc], gTb[i2][0:NE, :], bd[0:NE, hc * 512:(hc + 1) * 512], start=True, stop=True)
            P.dma('sp', xo[i2], dr['xs'][tile * 128:(tile + 1) * 128, :])
            P.stt(xo[i2], xo[i2], ALPHA, yacc[:, lt, :], ALU.mult, ALU.add)
            for hc in range(2):
                P.tt('dve', xo[i2][:, hc * 512:(hc + 1) * 512], xo[i2][:, hc * 512:(hc + 1) * 512], ps[2 + hc], ALU.add)
            ln_tile(C, L, xo[i2], tile, dst_final, psb[1])
    A.release(m0)


def run_pipe(jobs, depth=1):
    q = []
    for (s1, s2) in jobs:
        ctx = s1()
        q.append((s2, ctx))
        if len(q) > depth:
            f, c = q.pop(0)
            f(c)
    while q:
        f, c = q.pop(0)
        f(c)


def load_w(C, dst, src2d, q='pool'):
    C.P.dma(q, dst, src2d.rearrange("(k p) n -> p k n", p=128))


def bias_col(C, dst, src_row):
    C.P.dma('sp', dst, src_row.rearrange("o (p i) -> (o p) i", i=1))


def stage_meminit(C):
    P, A, dr = C.P, C.A, C.dr
    m = A.mark()
    mf = A.alloc([2, D], F32)
    mb = A.alloc([2, D], BF16)
    P.dma('sp', mf, dr['mem'].rearrange("(b p) d -> p b d", p=128))
    P.copy('act', mb, mf)
    for b in range(2):
        pv = C.psb[b].rearrange("p (c q) -> p c q", c=8)
        for c in range(8):
            P.tr(pv[:, c, :], mb[:, b, c * 128:(c + 1) * 128], C.ident)
        P.copy('dve', C.memT[:, :, b * 128:(b + 1) * 128], pv)
    A.release(m)


def stage_xattn(C, li):
    P, A, dr = C.P, C.A, C.dr
    ps, psb, xT = C.ps, C.psb, C.xT
    m0 = A.mark()
    kTx = A.alloc([4, 2, MEM], BF16)
    Vx = A.alloc([2, 4, 258], BF16)
    wk = A.alloc([8, D], BF16)
    wv = A.alloc([8, D], BF16)
    wq = A.alloc([8, D], BF16)
    wo = A.alloc([8, D], BF16)
    load_w(C, wk, dr['w_xk'][li])
    load_w(C, wv, dr['w_xv'][li])
    load_w(C, wq, dr['w_xq'][li])
    load_w(C, wo, dr['w_xo'][li])
    P.memset('dve', Vx[:, :, :, 256:258], 1.0)
    n = 0
    for h in range(4):
        for c in range(2):
            pp = ps[n % 2][:, 0:MEM]
            n += 1
            col = h * 256 + c * 128
            for k in range(8):
                P.mm(pp, wk[:, k, col:col + 128], C.memT[:, k, :], start=(k == 0), stop=(k == 7))
            P.copy('act', kTx[:, h, c, :], pp)
    for mb in range(2):
        for hc in range(2):
            pp = ps[2 + hc]
            for k in range(8):
                P.mm(pp, C.memT[:, k, mb * 128:(mb + 1) * 128], wv[:, k, hc * 512:(hc + 1) * 512], start=(k == 0), stop=(k == 7))
            P.copy('act', Vx[:, mb, hc * 2:hc * 2 + 2, 0:256], pp.rearrange("p (h d) -> p h d", h=2))
    L = ln_setup(C, dr['ln2_g'][li:li + 1, :], dr['ln2_b'][li:li + 1, :])
    qTx = [A.alloc([8, 512], BF16) for _ in range(2)]
    ox = [A.alloc([4, D], BF16) for _ in range(2)]
    oT = [A.alloc([8, 128], BF16) for _ in range(2)]
    xo = [A.alloc([D], F32) for _ in range(2)]
    rec = [A.alloc([1], F32) for _ in range(4)]
    NPX = 8
    pT = [A.alloc([512], BF16) for _ in range(NPX)]
    cnt = {'npt': 0, 'nr': 0, 'no': 0}
    jobs = []
    for qt in range(4):
        for h in range(4):
            def s1(qt=qt, h=h):
                qx = qTx[qt % 2]
                if h == 0:
                    for j in range(8):
                        pp = ps[j % 2]
                        for k in range(8):
                            P.mm(pp, wq[:, k, j * 128:(j + 1) * 128], xT[:, k, qt * 512:(qt + 1) * 512], start=(k == 0), stop=(k == 7))
                        P.copy('act', qx[:, j, :], pp)
                pts = []
                for mb in range(2):
                    sp_ = ps[2 + mb]
                    for c in range(2):
                        P.mm(sp_, kTx[:, h, c, mb * 128:(mb + 1) * 128], qx[:, 2 * h + c, :], start=(c == 0), stop=(c == 1))
                    pt = pT[cnt['npt'] % NPX]
                    cnt['npt'] += 1
                    P.act(pt, sp_, AF.Exp, scale=1.0 / 16.0)
                    pts.append(pt)
                return pts

            def s2(pts, qt=qt, h=h):
                oxx = ox[qt % 2]
                for sub in range(4):
                    po = ps[4 + (cnt['no'] % 2)][:, 0:257]
                    cnt['no'] += 1
                    for mb in range(2):
                        P.mm(po, pts[mb][:, sub * 128:(sub + 1) * 128], Vx[:, mb, h, 0:257], start=(mb == 0), stop=(mb == 1))
                    rc = rec[cnt['nr'] % 4]
                    cnt['nr'] += 1
                    P.op('dve', lambda e, rc=rc, po=po: e.reciprocal(rc, po[:, 256:257]), reads=[po[:, 256:257]], writes=[rc])
                    P.ts('dve', oxx[:, sub, h * 256:(h + 1) * 256], po[:, 0:256], rc, ALU.mult)
                if h == 3:
                    for sub in range(4):
                        tile = qt * 4 + sub
                        i2 = sub % 2
                        pv = psb[6].rearrange("p (c q) -> p c q", c=8)
                        for c in range(8):
                            P.tr(pv[:, c, :], oxx[:, sub, c * 128:(c + 1) * 128], C.ident)
                        P.copy('act', oT[i2], pv)
                        P.dma('sp', xo[i2], dr['xs'][tile * 128:(tile + 1) * 128, :])
                        for hc in range(2):
                            pp = ps[hc]
                            for k in range(8):
                                P.mm(pp, oT[i2][:, k, :], wo[:, k, hc * 512:(hc + 1) * 512], start=(k == 0), stop=(k == 7))
                            P.stt(xo[i2][:, hc * 512:(hc + 1) * 512], xo[i2][:, hc * 512:(hc + 1) * 512], ALPHA, pp, ALU.mult, ALU.add)
                        ln_tile(C, L, xo[i2], tile, dr['xs'], psb[7])
            jobs.append((s1, s2))
    run_pipe(jobs, 2)
    A.release(m0)


def stage_mixA(C, li):
    P, A, dr = C.P, C.A, C.dr
    ps, psb, xT = C.ps, C.psb, C.xT
    m0 = A.mark()
    accA = A.alloc([4, S], F32)
    qT = A.alloc([2, S], BF16)
    kT = A.alloc([2, S], BF16)
    Va = A.alloc([16, 4, 128], BF16)
    WA = [dict(wq=A.alloc([8, 256], BF16), wk=A.alloc([8, 256], BF16), wv=A.alloc([8, 256], BF16),
               bq=A.alloc([2], F32), bk=A.alloc([2], F32), bvr=A.alloc([256], BF16)) for _ in range(2)]

    def load_group_a(g):
        W = WA[g % 2]
        load_w(C, W['wq'], dr['w_in'][li][:, O_AQ + g * 256:O_AQ + (g + 1) * 256])
        load_w(C, W['wk'], dr['w_in'][li][:, O_AK + g * 256:O_AK + (g + 1) * 256])
        load_w(C, W['wv'], dr['w_in'][li][:, O_AV + g * 256:O_AV + (g + 1) * 256])
        for j in range(2):
            bias_col(C, W['bq'][:, j:j + 1], dr['b_in'][li:li + 1, O_AQ + g * 256 + j * 128:O_AQ + g * 256 + (j + 1) * 128])
            bias_col(C, W['bk'][:, j:j + 1], dr['b_in'][li:li + 1, O_AK + g * 256 + j * 128:O_AK + g * 256 + (j + 1) * 128])
        P.dma('pool', W['bvr'][0:1, :], dr['b_in'][li:li + 1, O_AV + g * 256:O_AV + (g + 1) * 256])
    load_group_a(0)
    load_group_a(1)
    NPA = 8
    pT = [A.alloc([256], BF16) for _ in range(NPA)]
    P.memset('dve', Va[:, :, :, 64:128], 1.0)
    DIL = [1, 4, 16]
    npt = 0
    nsp = 0
    nob = 0
    for g in range(3):
        dil = DIL[g]
        nb = S // dil // 128
        W = WA[g % 2]
        wq, wk, wv, bq, bk, bvr = W['wq'], W['wk'], W['wv'], W['bq'], W['bk'], W['bvr']
        n = 0
        for (w_, b_, dst) in ((wq, bq, qT), (wk, bk, kT)):
            for j in range(2):
                for qt in range(4):
                    pp = ps[n % 2]
                    n += 1
                    for k in range(8):
                        P.mm(pp, w_[:, k, j * 128:(j + 1) * 128], xT[:, k, qt * 512:(qt + 1) * 512], start=(k == 0), stop=(k == 7))
                    P.act(dst[:, j, qt * 512:(qt + 1) * 512], pp, AF.Identity, bias=b_[:, j:j + 1])

        def toks(r, blk, cnt):
            st = blk * 128 * dil + r
            return slice(st, st + (cnt - 1) * dil + 1, dil)

        for r in range(dil):
            for blk in range(nb):
                bi = r * nb + blk
                pp = ps[2 + bi % 2][:, 0:256]
                for k in range(8):
                    P.mm(pp, xT[:, k, toks(r, blk, 128)], wv[:, k, :], start=(k == 0), stop=False)
                P.mm(pp, C.ones[0:1, 0:128], bvr[0:1, :], start=False, stop=True)
                P.copy('act', Va[:, bi, :, 0:64], pp.rearrange("p (h d) -> p h d", h=4))
        if g == 0:
            load_group_a(2)
        jobs = []
        for h in range(4):
            for r0 in range(0, dil, 4 if g == 2 else 1):
                rs_ = list(range(r0, min(dil, r0 + (4 if g == 2 else 1))))
                if g < 2:
                    batches = [[(rs_[0], qb) for qb in range(b0, b0 + 4)] for b0 in range(0, nb, 4)]
                else:
                    batches = [[(r, 0) for r in rs_]]
                prevp = {}
                for batch in batches:
                    bst = {'ob': None}
                    for si, (r, qb) in enumerate(batch):
                        def s1(h=h, r=r, qb=qb):
                            nonlocal nsp, npt
                            j = h // 2
                            pb = (h % 2) * 64
                            nq = 256 if qb < nb - 1 else 128
                            sp_ = ps[4 + nsp % 2][:, 0:nq]
                            nsp += 1
                            P.mm(sp_, kT[pb:pb + 64, j, toks(r, qb, 128)], qT[pb:pb + 64, j, toks(r, qb, nq)], start=True, stop=True)
                            pt = pT[npt % NPA]
                            npt += 1
                            P.act(pt[:, 0:nq], sp_, AF.Exp, scale=0.125)
                            P.tt('dve', pt[:, 0:nq], pt[:, 0:nq], C.maska[:, 0:nq], ALU.mult)
                            return pt

                        def s2(pt, h=h, r=r, qb=qb, si=si, batch=batch, bst=bst, prevp=prevp, rs_=rs_):
                            nonlocal nob
                            if bst['ob'] is None:
                                bst['ob'] = ps[6 + nob % 2]
                                nob += 1
                            ob = bst['ob']
                            oo = ob[:, si * 128:(si + 1) * 128]
                            first = True
                            if qb > 0:
                                pp_ = prevp[(r, qb - 1)]
                                P.mm(oo, Va[:, r * nb + qb - 1, h, :], pp_[:, 128:256], start=True, stop=False)
                                first = False
                            P.mm(oo, Va[:, r * nb + qb, h, :], pt[:, 0:128], start=first, stop=True)
                            prevp[(r, qb)] = pt
                            if si == len(batch) - 1:
                                nbk = len(batch)
                                if g < 2:
                                    r_, qb0 = batch[0]
                                    st = qb0 * 128 * dil + r_
                                    dst = accA[:, h, st:st + (nbk * 128 - 1) * dil + 1:dil]
                                    src = ob[:, 0:nbk * 128]
                                else:
                                    dst = accA[:, h, :].rearrange("p (i r) -> p i r", r=16)[:, :, rs_[0]:rs_[0] + nbk]
                                    src = ob[:, 0:nbk * 128].rearrange("p (r i) -> p i r", r=nbk)
                                if g == 0:
                                    P.copy('act', dst, src)
                                else:
                                    P.tt('dve', dst, dst, src, ALU.add)
                        jobs.append((s1, s2))
        run_pipe(jobs, 2)
    rt = A.alloc([S], F32)
    rsft = A.alloc([S], F32)
    yb_ = [A.alloc([S], BF16) for _ in range(2)]
    for h in range(4):
        P.op('dve', lambda e, h=h: e.reciprocal(rt[64:128, :], accA[64:128, h, :]), reads=[accA[64:128, h, :]], writes=[rt[64:128, :]])
        P.copy('act', rsft[0:64, :], rt[64:128, :])
        P.tt('dve', yb_[h % 2][0:64, :], accA[0:64, h, :], rsft[0:64, :], ALU.mult)
        P.dma('sp', dr['ysa'][h], yb_[h % 2][0:64, :])
    A.release(m0)


def stage_mixB(C, li):
    P, A, dr = C.P, C.A, C.dr
    ps, psb, xT = C.ps, C.psb, C.xT
    m0 = A.mark()
    d0 = A.alloc([S], BF16)
    P.memset('dve', d0, 1.0)
    P.memset('dve', d0.rearrange("p (t i) -> p t i", i=128)[:, :, 0:1], 0.0)
    wlr = A.alloc([8, 16], BF16)
    load_w(C, wlr, dr['w_in'][li][:, O_BLR:O_BLR + 16])
    blr = A.alloc([1], F32)
    bias_col(C, blr[0:16, :], dr['b_in'][li:li + 1, O_BLR:O_BLR + 16])
    lrT = A.alloc([S], BF16)
    for qt in range(4):
        pp = ps[qt % 2][0:16, :]
        for k in range(8):
            P.mm(pp, wlr[:, k, :], xT[:, k, qt * 512:(qt + 1) * 512], start=(k == 0), stop=(k == 7))
        P.act(lrT[0:16, qt * 512:(qt + 1) * 512], pp, AF.Identity, bias=blr[0:16, :])
    wa2 = A.alloc([512], BF16)
    P.dma('pool', wa2[0:16, :], dr['w_alpha2'][li])
    bal = A.alloc([4], F32)
    for h in range(4):
        bias_col(C, bal[:, h:h + 1], dr['b_alpha'][li:li + 1, h * 128:(h + 1) * 128])
    nbal = A.alloc([4], F32)
    P.ts('dve', nbal, bal, -1.0, ALU.mult)
    ngb = A.alloc([256], F32)
    P.dma('sp', ngb, dr['gla_norm_g'][li:li + 1, :].broadcast_to([128, 256]))
    cs = A.alloc([S], F32)
    WB = [dict(wq=A.alloc([8, 128], BF16), wk=A.alloc([8, 128], BF16), wv=A.alloc([8, 256], BF16), wr=A.alloc([8, 256], BF16),
               bq=A.alloc([1], F32), bk=A.alloc([1], F32), bvr=A.alloc([256], BF16), brr=A.alloc([256], BF16)) for _ in range(2)]

    def load_head_b(h, i):
        W = WB[i]
        load_w(C, W['wq'], dr['w_in'][li][:, O_BQ + h * 128:O_BQ + (h + 1) * 128])
        load_w(C, W['wk'], dr['w_in'][li][:, O_BK + h * 128:O_BK + (h + 1) * 128])
        load_w(C, W['wv'], dr['w_in'][li][:, O_BV + h * 256:O_BV + (h + 1) * 256])
        load_w(C, W['wr'], dr['w_in'][li][:, O_BR + h * 256:O_BR + (h + 1) * 256])
        bias_col(C, W['bq'], dr['b_in'][li:li + 1, O_BQ + h * 128:O_BQ + (h + 1) * 128])
        bias_col(C, W['bk'], dr['b_in'][li:li + 1, O_BK + h * 128:O_BK + (h + 1) * 128])
        P.dma('pool', W['bvr'][0:1, :], dr['b_in'][li:li + 1, O_BV + h * 256:O_BV + (h + 1) * 256])
        P.dma('pool', W['brr'][0:1, :], dr['b_in'][li:li + 1, O_BR + h * 256:O_BR + (h + 1) * 256])
    load_head_b(0, 0)
    load_head_b(1, 1)
    eqs = [A.alloc([S], F32) for _ in range(2)]
    enbs = [A.alloc([S], F32) for _ in range(2)]
    elasts = [A.alloc([NT], F32) for _ in range(2)]
    qtls = [A.alloc([S], BF16) for _ in range(2)]
    ktls = [A.alloc([S], BF16) for _ in range(2)]
    Vs_ = [A.alloc([NT, 256], BF16) for _ in range(2)]
    srs = [A.alloc([NT, 256], F32) for _ in range(2)]
    ybts = [A.alloc([2, 256], BF16) for _ in range(2)]
    Sts = [A.alloc([256], F32) for _ in range(2)]
    Sbs = [A.alloc([256], BF16) for _ in range(2)]
    aTms = [A.alloc([128], BF16) for _ in range(2)]
    kTts = [A.alloc([128], BF16) for _ in range(2)]
    ssqs = [A.alloc([1], F32) for _ in range(2)]
    sds = [A.alloc([1], F32) for _ in range(2)]
    rstds = [A.alloc([1], F32) for _ in range(2)]
    junks = [A.alloc([256], F32) for _ in range(2)]
    LNQ = float(np.log(128.0 ** -0.5))
    ysb_v = dr['ysb'].rearrange("(t p) c -> p t c", p=128)

    def setup(h, i):
        eq, enb, elast, qtl, ktl, V, sr = eqs[i], enbs[i], elasts[i], qtls[i], ktls[i], Vs_[i], srs[i]
        W = WB[i]
        wq, wk, wv, wr, bq, bk, bvr, brr = W['wq'], W['wk'], W['wv'], W['wr'], W['bq'], W['bk'], W['bvr'], W['brr']
        for qt in range(4):
            pp = ps[qt % 2]
            P.mm(pp, wa2[0:16, h * 128:(h + 1) * 128], lrT[0:16, qt * 512:(qt + 1) * 512], start=True, stop=True)
            P.act(eq[:, qt * 512:(qt + 1) * 512], pp, AF.Exp, bias=nbal[:, h:h + 1], scale=-1.0)
        P.act(enb, eq, AF.Ln, bias=1.0)
        P.op('dve', lambda e: e.tensor_tensor_scan(cs, d0, enb, 0.0, ALU.mult, ALU.add), reads=[d0, enb], writes=[cs])
        P.act(eq, cs, AF.Exp, scale=-1.0 / 16.0, bias=LNQ)
        P.act(enb, cs, AF.Exp, scale=1.0 / 16.0)
        P.act(elast, cs.rearrange("p (t i) -> p t i", i=128)[:, :, 127], AF.Exp, scale=-1.0 / 16.0)
        for qt in range(4):
            sl = slice(qt * 512, (qt + 1) * 512)
            pp = ps[qt % 2]
            for k in range(8):
                P.mm(pp, wq[:, k, :], xT[:, k, sl], start=(k == 0), stop=(k == 7))
            P.stt(qtl[:, sl], pp, bq, eq[:, sl], ALU.add, ALU.mult)
            pp2 = ps[2 + qt % 2]
            for k in range(8):
                P.mm(pp2, wk[:, k, :], xT[:, k, sl], start=(k == 0), stop=(k == 7))
            P.stt(ktl[:, sl], pp2, bk, enb[:, sl], ALU.add, ALU.mult)
        for t in range(NT):
            tsl = slice(t * 128, (t + 1) * 128)
            pp = ps[t % 2][:, 0:256]
            for k in range(8):
                P.mm(pp, xT[:, k, tsl], wv[:, k, :], start=(k == 0), stop=False)
            P.mm(pp, C.ones[0:1, 0:128], bvr[0:1, :], start=False, stop=True)
            P.copy('act', V[:, t, :], pp)
            pp2 = ps[2 + t % 2][:, 0:256]
            for k in range(8):
                P.mm(pp2, xT[:, k, tsl], wr[:, k, :], start=(k == 0), stop=False)
            P.mm(pp2, C.ones[0:1, 0:128], brr[0:1, :], start=False, stop=True)
            P.act(sr[:, t, :], pp2, AF.Silu)
            P.tt('pool', sr[:, t, :], sr[:, t, :], ngb, ALU.mult)

    def step(h, i, t):
        elast, qtl, ktl, V, sr, ybt = elasts[i], qtls[i], ktls[i], Vs_[i], srs[i], ybts[i]
        St, Sb = Sts[i], Sbs[i]
        tsl = slice(t * 128, (t + 1) * 128)
        pa = ps[4 + i][:, 0:128]
        P.mm(pa, ktl[:, tsl], qtl[:, tsl], start=True, stop=True)
        P.tt('dve', aTms[i], pa, C.maskd[:, 0, 0:128], ALU.mult)
        po = ps[6 + i][:, 0:256]
        P.mm(po, aTms[i], V[:, t, :], start=True, stop=(t == 0))
        if t > 0:
            P.mm(po, qtl[:, tsl], Sb, start=False, stop=True)
        P.act(junks[i], po, AF.Square, accum_out=ssqs[i])
        P.act(sds[i], ssqs[i], AF.Sqrt, scale=1.0 / 256.0, bias=EPS)
        P.op('dve', lambda e: e.reciprocal(rstds[i], sds[i]), reads=[sds[i]], writes=[rstds[i]])
        P.stt(ybt[:, t % 2, :], po, rstds[i], sr[:, t, :], ALU.mult, ALU.mult)
        P.dma('sp', dr['ysb'][t * 128:(t + 1) * 128, h * 256:(h + 1) * 256], ybt[:, t % 2, :])
        if t < NT - 1:
            pk = psb[4 + i][:, 256:384]
            P.tr(pk, ktl[:, tsl], C.ident)
            P.copy('act', kTts[i], pk)
            pm = ps[2 + i][:, 256:512]
            P.mm(pm, kTts[i], V[:, t, :], start=True, stop=True)
            if t == 0:
                P.ts('dve', St, pm, elast[:, 0:1], ALU.mult)
            else:
                P.tt('dve', St, St, pm, ALU.add)
                P.ts('dve', St, St, elast[:, t:t + 1], ALU.mult)
            P.copy('act', Sb, St)

    for hp in (0, 2):
        for i in range(2):
            setup(hp + i, i)
        if hp == 0:
            load_head_b(2, 0)
            load_head_b(3, 1)
        for t in range(NT):
            for i in range(2):
                step(hp + i, i, t)
    A.release(m0)


def stage_mixC(C, li):
    P, A, dr = C.P, C.A, C.dr
    ps, psb, xT = C.ps, C.psb, C.xT
    m0 = A.mark()
    cmpv = A.alloc([S], BF16)
    P.dma('pool', cmpv, dr['c_cmpvalid'])
    keep = A.alloc([NT, 32], F32)
    addb = A.alloc([NT, 32], F32)
    P.dma('sp', keep, dr['c_keep'].rearrange("(t p) j -> p t j", p=128))
    P.dma('sp', addb, dr['c_add'].rearrange("(t p) j -> p t j", p=128))
    wcg = A.alloc([8, 48], BF16)
    load_w(C, wcg, dr['w_in'][li][:, O_CG:O_CG + 48])
    bcg = A.alloc([48], BF16)
    P.dma('pool', bcg[0:1, :], dr['b_in'][li:li + 1, O_CG:O_CG + 48])
    gates = A.alloc([NT, 48], F32)
    for t in range(NT):
        pp = ps[t % 2][:, 0:48]
        for k in range(8):
            P.mm(pp, xT[:, k, t * 128:(t + 1) * 128], wcg[:, k, :], start=(k == 0), stop=False)
        P.mm(pp, C.ones[0:1, 0:128], bcg[0:1, :], start=False, stop=True)
        P.act(gates[:, t, :], pp, AF.Sigmoid)
    WC = [dict(wq=A.alloc([8, 256], BF16), wkc=A.alloc([8, 64], BF16), wvc=A.alloc([8, 64], BF16), wks=A.alloc([8, 64], BF16),
               wkw=A.alloc([8, 64], BF16), wvs=A.alloc([8, 64], BF16), wvw=A.alloc([8, 64], BF16), bq=A.alloc([4], F32),
               bkc=A.alloc([1], F32), bvc=A.alloc([1], F32), bks=A.alloc([1], F32), bkw=A.alloc([1], F32),
               bvs=A.alloc([64], BF16), bvw=A.alloc([64], BF16)) for _ in range(2)]

    def load_group_c(g):
        W = WC[g % 2]
        cq0 = O_CQ + g * 256
        load_w(C, W['wq'], dr['w_in'][li][:, cq0:cq0 + 256])
        for nm, off in (('wkc', O_CKC), ('wvc', O_CVC), ('wks', O_CKS), ('wkw', O_CKW), ('wvs', O_CVS), ('wvw', O_CVW)):
            load_w(C, W[nm], dr['w_in'][li][:, off + g * 64:off + (g + 1) * 64])
        for r in range(4):
            bias_col(C, W['bq'][0:64, r:r + 1], dr['b_in'][li:li + 1, cq0 + r * 64:cq0 + (r + 1) * 64])
        for nm, off in (('bkc', O_CKC), ('bvc', O_CVC), ('bks', O_CKS), ('bkw', O_CKW)):
            bias_col(C, W[nm][0:64, :], dr['b_in'][li:li + 1, off + g * 64:off + (g + 1) * 64])
        P.dma('pool', W['bvs'][0:1, :], dr['b_in'][li:li + 1, O_CVS + g * 64:O_CVS + (g + 1) * 64])
        P.dma('pool', W['bvw'][0:1, :], dr['b_in'][li:li + 1, O_CVW + g * 64:O_CVW + (g + 1) * 64])
    load_group_c(0)
    load_group_c(1)
    qT = A.alloc([4, S], BF16)
    negst = A.alloc([S], BF16)
    kcin = A.alloc([S], BF16)
    vcin = A.alloc([S], BF16)
    ksd = A.alloc([S], BF16)
    kwd = A.alloc([S], BF16)
    VS = A.alloc([NT, 66], BF16)
    VW = A.alloc([NT, 66], BF16)
    P.memset('dve', qT[64:128, :, :], 0.0)
    P.memset('dve', ksd[64:128, :], 0.0)
    P.memset('dve', kwd[64:128, :], 0.0)
    P.dma('pool', ksd[64:96, :], dr['c_eexp'])
    P.memset('dve', VS[:, :, 64:66], 1.0)
    P.memset('dve', VW[:, :, 64:66], 1.0)
    w1k = A.alloc([32, 128], BF16)
    w1v = A.alloc([32, 128], BF16)
    w2kd = A.alloc([128], BF16)
    w2v = A.alloc([64], BF16)
    pe_raw = A.alloc([64], F32)
    peT = A.alloc([2, 32], BF16)
    hb = A.alloc([2], F32)
    gh = A.alloc([2, 128], BF16)
    kcTd = A.alloc([128], BF16)
    VC = A.alloc([98], BF16)
    ovl = A.alloc([32], BF16)
    P.dma('pool', ovl, dr['c_overlap'])
    yc = A.alloc([NT, 256], F32)
    impacc = A.alloc([NT, 32], F32)
    NPT = 6
    pT = [A.alloc([512], BF16) for _ in range(NPT)]
    rec = [A.alloc([4], F32) for _ in range(4)]
    gsc = [A.alloc([4], F32) for _ in range(4)]
    imadj = [A.alloc([32], F32) for _ in range(2)]
    imw = [A.alloc([32], F32) for _ in range(2)]
    m8a = [A.alloc([8], F32) for _ in range(2)]
    m8b = [A.alloc([8], F32) for _ in range(2)]
    selm = [A.alloc([32], F32) for _ in range(2)]
    selb = [A.alloc([32], BF16) for _ in range(2)]
    P.dma('pool', w1k[0:64, :, :], dr['cmp_w1_k'][li].rearrange("(l d) h -> d l h", d=64))
    P.dma('pool', w1v[0:64, :, :], dr['cmp_w1_v'][li].rearrange("(l d) h -> d l h", d=64))
    P.dma('pool', w2kd[:, 0:64], dr['cmp_w2_k'][li])
    P.dma('pool', w2kd[:, 64:128], dr['cmp_w2_k'][li])
    P.dma('pool', w2v, dr['cmp_w2_v'][li])
    for wi, (pn, w1) in enumerate((('cmp_pe_k', w1k), ('cmp_pe_v', w1v))):
        P.dma('sp', pe_raw[0:32, :], dr[pn][li])
        pp = ps[wi][0:64, 0:32]
        P.tr(pp, pe_raw[0:32, :], C.identf[0:32, 0:32])
        P.copy('act', peT[0:64, wi, :], pp)
        pb_ = ps[2 + wi][:, 0:1]
        for l in range(32):
            P.mm(pb_, w1[0:64, l, :], peT[0:64, wi, l:l + 1], start=(l == 0), stop=(l == 31))
        P.copy('act', hb[:, wi:wi + 1], pb_)
    ysc_v = dr['ysc'].rearrange("(t p) c -> p t c", p=128)
    nsp = 0
    npt = 0
    nob = 0
    nrc = 0

    def evac(po, W, qt, gcol, r, first_y, imp_mode):
        nonlocal nrc
        rc = rec[nrc % 4]
        gs = gsc[nrc % 4]
        nrc += 1
        if imp_mode is not None:
            P.ts('dve', rc, po[:, :, 64], 1e-30, ALU.max)
            P.op('dve', lambda e: e.reciprocal(rc, rc), reads=[rc], writes=[rc])
        else:
            P.op('dve', lambda e: e.reciprocal(rc, po[:, :, 64]), reads=[po[:, :, 64]], writes=[rc])
        P.tt('dve', gs, rc, gates[:, qt * 4:(qt + 1) * 4, gcol], ALU.mult)
        for sub in range(4):
            t = qt * 4 + sub
            ysl = yc[:, t, r * 64:(r + 1) * 64]
            if first_y:
                P.ts('dve', ysl, po[:, sub, 0:64], gs[:, sub:sub + 1], ALU.mult)
            else:
                P.stt(ysl, po[:, sub, 0:64], gs[:, sub:sub + 1], ysl, ALU.mult, ALU.add)
            if imp_mode == 'first':
                P.ts('dve', impacc[:, t, :], po[:, sub, 65:97], rc[:, sub:sub + 1], ALU.mult)
            elif imp_mode == 'add':
                P.stt(impacc[:, t, :], po[:, sub, 65:97], rc[:, sub:sub + 1], impacc[:, t, :], ALU.mult, ALU.add)

    for g in range(4):
        W = WC[g % 2]
        wq, wkc, wvc, wks, wkw, wvs, wvw = W['wq'], W['wkc'], W['wvc'], W['wks'], W['wkw'], W['wvs'], W['wvw']
        bq, bkc, bvc, bks, bkw, bvs, bvw = W['bq'], W['bkc'], W['bvc'], W['bks'], W['bkw'], W['bvs'], W['bvw']
        n = 0
        jobs = [(wq[:, :, r * 64:(r + 1) * 64], bq[0:64, r:r + 1], qT[0:64, r, :], 64) for r in range(4)]
        jobs += [(wkc, bkc[0:64, :], kcin[0:64, :], 64), (wvc, bvc[0:64, :], vcin[0:64, :], 64),
                 (wks, bks[0:64, :], ksd[0:64, :], 64), (wkw, bkw[0:64, :], kwd[0:64, :], 64)]
        for (w_, b_, dst, m_) in jobs:
            for qt in range(4):
                pp = ps[n % 2][0:m_, :]
                n += 1
                for k in range(8):
                    P.mm(pp, w_[:, k, :], xT[:, k, qt * 512:(qt + 1) * 512], start=(k == 0), stop=(k == 7))
                P.act(dst[:, qt * 512:(qt + 1) * 512], pp, AF.Identity, bias=b_)
        for (w_, br_, dst) in ((wvs, bvs, VS), (wvw, bvw, VW)):
            for t in range(NT):
                pp = ps[2 + t % 2][:, 0:64]
                for k in range(8):
                    P.mm(pp, xT[:, k, t * 128:(t + 1) * 128], w_[:, k, :], start=(k == 0), stop=False)
                P.mm(pp, C.ones[0:1, 0:128], br_[0:1, :], start=False, stop=True)
                P.copy('act', dst[:, t, 0:64], pp)
        if 1 <= g <= 2:
            load_group_c(g + 1)
        for wi, (src, w1) in enumerate(((kcin, w1k), (vcin, w1v))):
            ph = ps[4 + wi][:, 0:127]
            for l in range(32):
                P.mm(ph, w1[0:64, l, :], src[0:64, l:l + 16 * 126 + 1:16], start=(l == 0), stop=(l == 31))
            P.act(gh[:, wi, 0:127], ph, AF.Gelu_apprx_tanh, bias=hb[:, wi:wi + 1])
        pk = ps[6][:, 0:127]
        P.mm(pk, w2kd, gh[:, 0, 0:127], start=True, stop=True)
        P.memset('dve', kcTd, 0.0)
        P.copy('act', kcTd[0:64, 0:127], pk[0:64, :])
        pv_ = ps[7][0:127, 0:64]
        P.mm(pv_, gh[:, 1, 0:127], w2v, start=True, stop=True)
        P.memset('dve', VC, 0.0)
        P.copy('act', VC[0:127, 0:64], pv_)
        P.memset('dve', VC[0:127, 64:65], 1.0)
        P.copy('dve', VC[0:127, 65:97], ovl[0:127, :])
        jobs = []
        for r in range(4):
            for qt in range(4):
                def s1(r=r, qt=qt):
                    nonlocal nsp, npt
                    qs = slice(qt * 512, (qt + 1) * 512)
                    sp_ = ps[nsp % 4][0:127, :]
                    nsp += 1
                    P.mm(sp_, kcTd[:, 0:127], qT[:, r, qs], start=True, stop=True)
                    pt = pT[npt % NPT]
                    npt += 1
                    P.act(pt[0:127, :], sp_, AF.Exp, scale=0.125)
                    P.tt('dve', pt[0:127, :], pt[0:127, :], cmpv[0:127, qs], ALU.mult)
                    return pt

                def s2(pt, r=r, qt=qt):
                    nonlocal nob
                    gcol0 = (g * 4 + r) * 3
                    po = ps[4 + nob % 4].rearrange("p (s w) -> p s w", s=4)
                    nob += 1
                    for sub in range(4):
                        P.mm(po[:, sub, 0:97], pt[0:127, sub * 128:(sub + 1) * 128], VC[0:127, 0:97], start=True, stop=True)
                    evac(po, 97, qt, gcol0 + 0, r, True, 'first' if r == 0 else 'add')
                jobs.append((s1, s2))
        run_pipe(jobs, 2)
        for t in range(NT):
            i2 = t % 2
            P.tt('dve', imadj[i2], impacc[:, t, :], keep[:, t, :], ALU.mult)
            P.tt('dve', imadj[i2], imadj[i2], addb[:, t, :], ALU.add)
            P.op('dve', lambda e, i2=i2: e.max(m8a[i2], imadj[i2]), reads=[imadj[i2]], writes=[m8a[i2]])
            P.op('dve', lambda e, i2=i2: e.match_replace(imw[i2], m8a[i2], imadj[i2], -1e9),
                 reads=[m8a[i2], imadj[i2]], writes=[imw[i2]])
            P.op('dve', lambda e, i2=i2: e.max(m8b[i2], imw[i2]), reads=[imw[i2]], writes=[m8b[i2]])
            P.ts('dve', selm[i2], imadj[i2], m8b[i2][:, 7:8], ALU.is_ge)
            P.ts('dve', selb[i2], selm[i2], -1.0, ALU.add, -NEGBIG, ALU.mult)
            pn = psb[t % 2][0:32, 0:128]
            P.tr(pn, selb[i2], C.ident)
            P.copy('act', negst[64:96, t * 128:(t + 1) * 128], pn)
        for r in range(4):
            P.copy('pool' if r % 2 else 'dve', qT[64:96, r, :], negst[64:96, :])
        jobs = []
        for r in range(4):
            for qt in range(4):
                for br in (1, 2):
                    st_ = {'po': None, 'first': True}
                    kb_lo = 0 if br == 1 else max(0, 4 * qt - 4)
                    kb_hi = 4 * qt + 3
                    for kb in range(kb_lo, kb_hi + 1):
                        def s1(r=r, qt=qt, br=br, kb=kb):
                            nonlocal nsp, npt
                            qs = slice(qt * 512, (qt + 1) * 512)
                            ksrc = ksd if br == 1 else kwd
                            ks_ = slice(kb * 128, (kb + 1) * 128)
                            sp_ = ps[nsp % 4]
                            nsp += 1
                            P.mm(sp_, ksrc[:, ks_], qT[:, r, qs], start=True, stop=True)
                            pt = pT[npt % NPT]
                            npt += 1
                            P.act(pt, sp_, AF.Exp, scale=0.125)
                            d = kb - 4 * qt
                            if d >= 0:
                                P.tt('dve', pt, pt, C.maskd[:, d, :], ALU.mult)
                            elif br == 2:
                                P.tt('dve', pt, pt, C.maskd[:, 8 + d, :], ALU.mult)
                            return pt

                        def s2(pt, r=r, qt=qt, br=br, kb=kb, st_=st_, kb_lo=kb_lo, kb_hi=kb_hi):
                            nonlocal nob
                            if st_['po'] is None:
                                st_['po'] = ps[4 + nob % 4].rearrange("p (s w) -> p s w", s=4)
                                nob += 1
                            po = st_['po']
                            vsrc = VS if br == 1 else VW
                            for sub in range(4):
                                lo = kb_lo if br == 1 else max(0, 4 * qt + sub - 4)
                                hi = 4 * qt + sub
                                if kb < lo or kb > hi:
                                    continue
                                P.mm(po[:, sub, 0:65], pt[:, sub * 128:(sub + 1) * 128], vsrc[:, kb, 0:65], start=st_['first'], stop=(kb == kb_hi and sub == 3))
                                st_['first'] = False
                            if kb == kb_hi:
                                evac(po, 65, qt, (g * 4 + r) * 3 + br, r, False, None)
                        jobs.append((s1, s2))
        run_pipe(jobs, 2)
        P.dma('pool', ysc_v[:, :, g * 256:(g + 1) * 256], yc)
    A.release(m0)


def stage_merge(C, li):
    P, A, dr = C.P, C.A, C.dr
    ps, psb, xT = C.ps, C.psb, C.xT
    m0 = A.mark()
    mgT = A.alloc([8, S], BF16)
    m1 = A.mark()
    wbas = [A.alloc([4, 512], BF16) for _ in range(2)]
    wbbs = [A.alloc([8, 512], BF16) for _ in range(2)]
    wbcs = [A.alloc([8, 512], BF16) for _ in range(2)]
    wmgs = [A.alloc([3, 8, 512], BF16) for _ in range(2)]
    bmgs = [A.alloc([3, 512], BF16) for _ in range(2)]

    def load_half(hc):
        cs_ = slice(hc * 512, (hc + 1) * 512)
        P.dma('pool', wbas[hc][0:64, :, :], dr['w_br_a'][li][:, cs_].rearrange("(h d) n -> d h n", d=64))
        load_w(C, wbbs[hc], dr['w_br_b'][li][:, cs_])
        load_w(C, wbcs[hc], dr['w_br_c'][li][:, cs_])
        for b in range(3):
            load_w(C, wmgs[hc][:, b, :, :], dr['w_in'][li][:, O_MG + b * D + hc * 512:O_MG + b * D + (hc + 1) * 512])
            P.dma('pool', bmgs[hc][0:1, b, :], dr['b_in'][li:li + 1, O_MG + b * D + hc * 512:O_MG + b * D + (hc + 1) * 512])
    load_half(0)
    load_half(1)
    yaT = [A.alloc([4, 128], BF16) for _ in range(2)]
    ybl = [A.alloc([D], BF16) for _ in range(2)]
    ycl = [A.alloc([D], BF16) for _ in range(2)]
    ybT = [A.alloc([8, 128], BF16) for _ in range(2)]
    ycT = [A.alloc([8, 128], BF16) for _ in range(2)]
    mg = [A.alloc([512], F32) for _ in range(2)]
    sg = [A.alloc([512], F32) for _ in range(2)]
    mgb = [A.alloc([512], BF16) for _ in range(2)]
    nsg = 0
    for hc in range(2):
        wba, wbb, wbc, wmg, bmg = wbas[hc], wbbs[hc], wbcs[hc], wmgs[hc], bmgs[hc]
        for t in range(NT):
            i2 = t % 2
            tsl = slice(t * 128, (t + 1) * 128)
            P.dma('sp', yaT[i2][0:64, :, :], dr['ysa'][:, :, tsl].rearrange("h d t -> d h t"))
            P.dma('sp', ybl[i2], dr['ysb'][tsl, :])
            P.dma('sp', ycl[i2], dr['ysc'][tsl, :])
            for (src, dst, bank) in ((ybl[i2], ybT[i2], 6), (ycl[i2], ycT[i2], 7)):
                pv = psb[bank].rearrange("p (c q) -> p c q", c=8)
                for c in range(8):
                    P.tr(pv[:, c, :], src[:, c * 128:(c + 1) * 128], C.ident)
                P.copy('act', dst, pv)
            for b in range(3):
                pg = ps[b % 2]
                for k in range(8):
                    P.mm(pg, xT[:, k, tsl], wmg[:, b, k, :], start=(k == 0), stop=False)
                P.mm(pg, C.ones[0:1, 0:128], bmg[0:1, b, :], start=False, stop=True)
                s_ = sg[nsg % 2]
                nsg += 1
                P.act(s_, pg, AF.Sigmoid)
                pp = ps[2 + b % 2]
                if b == 0:
                    for h in range(4):
                        P.mm(pp, yaT[i2][0:64, h, :], wba[0:64, h, :], start=(h == 0), stop=(h == 3))
                else:
                    yT_ = ybT[i2] if b == 1 else ycT[i2]
                    w_ = wbb if b == 1 else wbc
                    for k in range(8):
                        P.mm(pp, yT_[:, k, :], w_[:, k, :], start=(k == 0), stop=(k == 7))
                if b == 0:
                    P.tt('dve', mg[i2], s_, pp, ALU.mult)
                else:
                    P.tt('dve', s_, s_, pp, ALU.mult)
                    P.tt('pool', mg[i2], mg[i2], s_, ALU.add)
            P.copy('act', mgb[i2], mg[i2])
            pv = psb[4 + i2][:, 0:512].rearrange("p (c q) -> p c q", c=4)
            for c in range(4):
                P.tr(pv[:, c, :], mgb[i2][:, c * 128:(c + 1) * 128], C.ident)
            P.copy('act', mgT[:, hc * 4:(hc + 1) * 4, tsl], pv)
    A.release(m1)
    wom = A.alloc([8, D], BF16)
    load_w(C, wom, dr['w_o_mix'][li])
    L = ln_setup(C, dr['ln1_g'][li:li + 1, :], dr['ln1_b'][li:li + 1, :])
    xo = [A.alloc([D], F32) for _ in range(2)]
    for t in range(NT):
        i2 = t % 2
        tsl = slice(t * 128, (t + 1) * 128)
        P.dma('sp', xo[i2], dr['xs'][tsl, :])
        for hc in range(2):
            cs_ = slice(hc * 512, (hc + 1) * 512)
            pp = ps[hc]
            for k in range(8):
                P.mm(pp, mgT[:, k, tsl], wom[:, k, cs_], start=(k == 0), stop=(k == 7))
            P.stt(xo[i2][:, cs_], xo[i2][:, cs_], ALPHA, pp, ALU.mult, ALU.add)
        ln_tile(C, L, xo[i2], t, dr['xs'], psb[4 + i2])
    A.release(m0)


_CACHE = {}


def _get_nc(stages, dbg=False):
    key = (tuple(stages), dbg)
    if key not in _CACHE:
        _CACHE[key] = build_nc(stages, dbg)
    return _CACHE[key]


def make_in_maps(inputs, ncores=8):
    consts = host_consts()
    shared = {}
    for n in WNAMES:
        shared[n] = np.ascontiguousarray(inputs[n], dtype=np.float32)
    shared['ln0_g'] = np.ascontiguousarray(inputs['ln0_g'], dtype=np.float32).reshape(1, D)
    shared['ln0_b'] = np.ascontiguousarray(inputs['ln0_b'], dtype=np.float32).reshape(1, D)
    shared.update(consts)
    maps = []
    for b in range(ncores):
        m = dict(shared)
        m['x'] = np.ascontiguousarray(inputs['x'][b], dtype=np.float32)
        m['mem'] = np.ascontiguousarray(inputs['mem'][b], dtype=np.float32)
        maps.append(m)
    return maps


FULL_STAGES = ['ln0', 'meminit']
for _li in range(DEPTH):
    FULL_STAGES += ['mixA%d' % _li, 'mixB%d' % _li, 'mixC%d' % _li, 'merge%d' % _li, 'xattn%d' % _li, 'moe%d' % _li]
FULL_STAGES[-1] += 'L'


def kernel(**inputs):
    nc, C = _get_nc(FULL_STAGES)
    maps = make_in_maps(inputs, 8)
    res = run_bass_kernel_spmd(nc, maps, core_ids=list(range(8)))
    out = np.stack([np.asarray(r['out']) for r in res.results], axis=0)
    return out.astype(np.float32)
```

```python
import numpy as np
import concourse.bass as bass
import concourse.mybir as mybir
from concourse.bass_utils import run_bass_kernel_spmd
from contextlib import ExitStack

DT = mybir.dt
F32 = DT.float32
BF16 = DT.bfloat16
AF = mybir.ActivationFunctionType
ALU = mybir.AluOpType
AX = mybir.AxisListType

_ISZ = {F32: 4, BF16: 2, DT.int32: 4, DT.uint32: 4, DT.uint8: 1, DT.int8: 1, DT.uint16: 2, DT.int16: 2, DT.float16: 2}


def region(ap):
    t = ap.tensor
    isz = _ISZ[ap.dtype]
    a = list(ap.ap)
    space = str(ap.space).upper()
    if 'DRAM' in space or 'HBM' in space:
        lo = ap.offset
        hi = ap.offset
        for st, n in a:
            if n > 1:
                if st >= 0:
                    hi += st * (n - 1)
                else:
                    lo += st * (n - 1)
        return (t.name, 0, 1, lo * isz, (hi + 1) * isz)
    pst, pn = a[0]
    if pst == 0:
        p0 = 0
        f0 = ap.offset
    else:
        p0 = ap.offset // pst
        f0 = ap.offset - p0 * pst
    lo = f0
    hi = f0
    for st, n in a[1:]:
        if n > 1:
            if st >= 0:
                hi += st * (n - 1)
            else:
                lo += st * (n - 1)
    return (t.name, p0, p0 + pn, lo * isz, (hi + 1) * isz)


class Prog:
    ENG = ['pe', 'dve', 'act', 'pool', 'sp']

    def __init__(self, nc, slots=None):
        self.nc = nc
        self.es = ExitStack()
        self.stream = {e: [] for e in self.ENG}
        slots = slots or {'sp': 8, 'pool': 6, 'act': 2}
        self.slots = {q: ['%s_d%d' % (q, i) for i in range(n)] for q, n in slots.items()}
        self.slot_rr = {q: 0 for q in slots}
        self.tl = ['pe', 'dve', 'act', 'pool'] + [s for q in self.slots for s in self.slots[q]]
        self.count = {t: 0 for t in self.tl}
        self.clk = {e: {} for e in self.ENG}
        self.opclk = {}
        self.track = {}
        self.sem = {}
        self.nwait = 0
        self.nop = 0
        for t in self.tl:
            self.sem[t] = self.es.enter_context(nc.semaphore('sem_' + t))

    def sb(self, name, shape, dt=F32):
        return self.es.enter_context(self.nc.sbuf_tensor(name, list(shape), dt))

    def ps(self, name, shape, dt=F32):
        return self.es.enter_context(self.nc.psum_tensor(name, list(shape), dt))

    def _deps(self, regs_r, regs_w):
        deps = {}
        for (kind, regs) in (('r', regs_r), ('w', regs_w)):
            for (name, p0, p1, b0, b1) in regs:
                for rec in self.track.get(name, ()):
                    if kind == 'r' and rec[4] == 'r':
                        continue
                    if rec[0] < p1 and p0 < rec[1] and rec[2] < b1 and b0 < rec[3]:
                        t = rec[5]
                        if rec[6] > deps.get(t, 0):
                            deps[t] = rec[6]
        return deps

    def _wait(self, S, t, seq):
        clk = self.clk[S]
        if clk.get(t, 0) >= seq:
            return
        val = seq * 16 if '_d' in t else seq
        self.stream[S].append(('w', t, val))
        self.nwait += 1
        oc = self.opclk.get((t, seq))
        if oc:
            for k, v in oc.items():
                if v > clk.get(k, 0):
                    clk[k] = v
        clk[t] = max(clk.get(t, 0), seq)

    def _record(self, regs_r, regs_w, T, seq):
        for (name, p0, p1, b0, b1) in regs_w:
            lst = self.track.setdefault(name, [])
            lst[:] = [r for r in lst if not (p0 <= r[0] and r[1] <= p1 and b0 <= r[2] and r[3] <= b1)]
            lst.append((p0, p1, b0, b1, 'w', T, seq))
        for (name, p0, p1, b0, b1) in regs_r:
            lst = self.track.setdefault(name, [])
            lst[:] = [r for r in lst if not (r[4] == 'r' and r[5] == T and p0 <= r[0] and r[1] <= p1 and b0 <= r[2] and r[3] <= b1)]
            lst.append((p0, p1, b0, b1, 'r', T, seq))

    def op(self, S, fn, reads=(), writes=(), dma=False):
        regs_r = [a if isinstance(a, tuple) else region(a) for a in reads if a is not None]
        regs_w = [a if isinstance(a, tuple) else region(a) for a in writes if a is not None]
        deps = self._deps(regs_r, regs_w)
        if dma:
            sl = self.slots[S]
            T = sl[self.slot_rr[S] % len(sl)]
            self.slot_rr[S] += 1
            if self.count[T] > 0:
                deps[T] = max(deps.get(T, 0), self.count[T])
        else:
            T = S
            if S == 'pe':
                deps.pop('pe', None)
        for t in sorted(deps, key=lambda k: -deps[k]):
            self._wait(S, t, deps[t])
        self.count[T] += 1
        seq = self.count[T]
        self.opclk[(T, seq)] = dict(self.clk[S])
        self.stream[S].append(('o', fn, T, 16 if dma else 1))
        self.nop += 1
        self._record(regs_r, regs_w, T, seq)
        return (T, seq)

    def finish(self, S='sp'):
        for t in self.tl:
            if self.count[t] > 0:
                self._wait(S, t, self.count[t])

    def emit(self):
        nc = self.nc
        sem = self.sem

        def run(items, e):
            for it in items:
                if it[0] == 'w':
                    e.wait_ge(sem[it[1]], it[2])
                else:
                    ins = it[1](e)
                    ins.then_inc(sem[it[2]], it[3])

        with nc.Block() as block:
            @block.tensor
            def _(e):
                run(self.stream['pe'], e)

            @block.vector
            def _(e):
                run(self.stream['dve'], e)

            @block.scalar
            def _(e):
                run(self.stream['act'], e)

            @block.gpsimd
            def _(e):
                run(self.stream['pool'], e)

            @block.sync
            def _(e):
                run(self.stream['sp'], e)
        self.es.close()

    def dma(self, q, out, in_, **kw):
        return self.op(q, lambda e: e.dma_start(out=out, in_=in_, **kw), reads=[in_], writes=[out], dma=True)

    @staticmethod
    def _bank(out):
        r = region(out)
        return (r[0], 0, 128, 0, 2048)

    def mm(self, out, lhsT, rhs, start=True, stop=True):
        return self.op('pe', lambda e: e.matmul(out, lhsT, rhs, start=start, stop=stop),
                       reads=[lhsT, rhs], writes=[self._bank(out) if start else out])

    def tr(self, out, in_, ident):
        return self.op('pe', lambda e: e.transpose(out, in_, ident), reads=[in_, ident], writes=[self._bank(out)])

    def act(self, out, in_, func, bias=None, scale=None, accum_out=None):
        kw = {}
        rd = [in_]
        if bias is not None:
            kw['bias'] = bias
            if not isinstance(bias, (int, float)):
                rd.append(bias)
        if scale is not None:
            kw['scale'] = scale
            if not isinstance(scale, (int, float)):
                rd.append(scale)
        wr = [out]
        if accum_out is not None:
            kw['accum_out'] = accum_out
            wr.append(accum_out)
        return self.op('act', lambda e: e.activation(out, in_, func, **kw), reads=rd, writes=wr)

    def tt(self, eng, out, in0, in1, op):
        return self.op(eng, lambda e: e.tensor_tensor(out, in0, in1, op), reads=[in0, in1], writes=[out])

    def ts(self, eng, out, in0, s1, op0, s2=None, op1=None):
        rd = [in0]
        if s1 is not None and not isinstance(s1, (int, float)):
            rd.append(s1)
        if s2 is not None and not isinstance(s2, (int, float)):
            rd.append(s2)
        kw = {}
        if op1 is not None:
            kw['op1'] = op1
        return self.op(eng, lambda e: e.tensor_scalar(out, in0, s1, s2, op0, **kw), reads=rd, writes=[out])

    def stt(self, out, in0, scalar, in1, op0, op1):
        rd = [in0, in1]
        if not isinstance(scalar, (int, float)):
            rd.append(scalar)
        return self.op('dve', lambda e: e.scalar_tensor_tensor(out, in0, scalar, in1, op0, op1), reads=rd, writes=[out])

    def copy(self, eng, out, in_):
        if eng == 'act':
            return self.op('act', lambda e: e.copy(out, in_), reads=[in_], writes=[out])
        return self.op(eng, lambda e: e.tensor_copy(out, in_), reads=[in_], writes=[out])

    def memset(self, eng, ap, val):
        return self.op(eng, lambda e: e.memset(ap, val), writes=[ap])


class Arena:
    def __init__(self, P, nbytes):
        self.nbytes = nbytes
        self.t32 = P.sb("arena", [128, nbytes // 4], F32)
        self.t16 = self.t32.bitcast(BF16)
        self.top = 0

    def alloc(self, shape, dt=F32):
        shape = list(shape)
        n = int(np.prod(shape))
        isz = _ISZ[dt]
        off = (self.top + 63) // 64 * 64
        self.top = off + n * isz
        assert self.top <= self.nbytes, ("arena overflow", self.top, self.nbytes)
        base = self.t32 if isz == 4 else self.t16
        ap = base[:, off // isz: off // isz + n]
        if len(shape) == 2:
            ap = ap.rearrange("p (a b) -> p a b", a=shape[0])
        elif len(shape) == 3:
            ap = ap.rearrange("p (a b c) -> p a b c", a=shape[0], b=shape[1])
        elif len(shape) == 4:
            ap = ap.rearrange("p (a b c d) -> p a b c d", a=shape[0], b=shape[1], c=shape[2])
        return ap

    def mark(self):
        return self.top

    def release(self, m):
        self.top = m


S = 2048
D = 1024
NT = 16
DEPTH = 2
MEM = 256
ALPHA = float((2 * DEPTH) ** 0.25)
EPS = 1e-5
NE = 32
NIN = 11072
O_AQ, O_AK, O_AV = 0, 768, 1536
O_BQ, O_BK, O_BV, O_BR, O_BLR = 2304, 2816, 3328, 4352, 5376
O_CQ = 5392
O_CKC, O_CVC, O_CKS, O_CVS, O_CKW, O_CVW = 6416, 6672, 6928, 7184, 7440, 7696
O_CG = 7952
O_MG = 8000
NEGBIG = -30000.0

WNAMES = ['w_in', 'b_in', 'w_alpha2', 'b_alpha', 'gla_norm_g', 'cmp_pe_k', 'cmp_w1_k', 'cmp_w2_k',
          'cmp_pe_v', 'cmp_w1_v', 'cmp_w2_v', 'w_br_a', 'w_br_b', 'w_br_c', 'w_o_mix', 'ln1_g', 'ln1_b',
          'w_xq', 'w_xk', 'w_xv', 'w_xo', 'ln2_g', 'ln2_b', 'w_router', 'b_router', 'w_gu', 'b_gu',
          'w_down', 'b_down', 'ln3_g', 'ln3_b']
WSHAPES = {
    'w_in': [2, 1024, NIN], 'b_in': [2, NIN], 'w_alpha2': [2, 16, 512], 'b_alpha': [2, 512], 'gla_norm_g': [2, 256],
    'cmp_pe_k': [2, 32, 64], 'cmp_w1_k': [2, 2048, 128], 'cmp_w2_k': [2, 128, 64],
    'cmp_pe_v': [2, 32, 64], 'cmp_w1_v': [2, 2048, 128], 'cmp_w2_v': [2, 128, 64],
    'w_br_a': [2, 256, 1024], 'w_br_b': [2, 1024, 1024], 'w_br_c': [2, 1024, 1024], 'w_o_mix': [2, 1024, 1024],
    'ln1_g': [2, 1024], 'ln1_b': [2, 1024], 'w_xq': [2, 1024, 1024], 'w_xk': [2, 1024, 1024], 'w_xv': [2, 1024, 1024],
    'w_xo': [2, 1024, 1024], 'ln2_g': [2, 1024], 'ln2_b': [2, 1024], 'w_router': [2, 1024, 32], 'b_router': [2, 32],
    'w_gu': [2, 32, 1024, 2048], 'b_gu': [2, 32, 2048], 'w_down': [2, 32, 1024, 1024], 'b_down': [2, 32, 1024],
    'ln3_g': [2, 1024], 'ln3_b': [2, 1024],
}


def host_consts():
    c = {}
    c['c_ident'] = np.eye(128, dtype=np.float32)
    p = np.arange(128)[:, None]
    q = np.arange(512)[None, :]
    md = np.zeros((128, 8, 512), np.float32)
    for d in range(4):
        md[:, d, :] = (128 * d + p <= q)
        md[:, 4 + d, :] = 1.0 - md[:, d, :]
    c['c_maskd'] = md
    ma = np.zeros((128, 256), np.float32)
    q1 = np.arange(128)[None, :]
    ma[:, 0:128] = (p <= q1)
    ma[:, 128:256] = (p >= q1)
    c['c_maska'] = ma
    cc = np.arange(128)[:, None]
    t = np.arange(S)[None, :]
    cv = ((16 * cc + 31) <= t).astype(np.float32)
    cv[127, :] = 0.0
    c['c_cmpvalid'] = cv
    ov = np.zeros((128, 32), np.float32)
    for ci in range(127):
        for j in range(32):
            if ci * 16 < j * 64 + 64 and ci * 16 + 32 > j * 64:
                ov[ci, j] = 1.0
    c['c_overlap'] = ov
    pos = np.arange(S)
    qb = pos // 64
    j = np.arange(32)[None, :]
    forced = (j == 0) | (j == qb[:, None]) | (j == qb[:, None] - 1)
    future = j > qb[:, None]
    keep = (~forced & ~future).astype(np.float32)
    add = np.where(forced, 1e4, np.where(future, -1e4, 0.0)).astype(np.float32)
    c['c_keep'] = keep
    c['c_add'] = add
    key = np.arange(S)[None, :]
    c['c_eexp'] = (key // 64 == np.arange(32)[:, None]).astype(np.float32)
    return c


CSHAPES = {'c_ident': [128, 128], 'c_maskd': [128, 8, 512], 'c_maska': [128, 256], 'c_cmpvalid': [128, S],
           'c_overlap': [128, 32], 'c_keep': [S, 32], 'c_add': [S, 32], 'c_eexp': [32, S]}


class Ctx:
    pass


def build_nc(stages, dbg=False):
    nc = bass.Bass("TRN2", target_bir_lowering=False)
    C = Ctx()
    C.nc = nc
    C.dbg = dbg
    dr = {}
    dr['x'] = nc.dram_tensor("x", [S, D], F32, kind="ExternalInput").ap()
    dr['mem'] = nc.dram_tensor("mem", [MEM, D], F32, kind="ExternalInput").ap()
    dr['ln0_g'] = nc.dram_tensor("ln0_g", [1, D], F32, kind="ExternalInput").ap()
    dr['ln0_b'] = nc.dram_tensor("ln0_b", [1, D], F32, kind="ExternalInput").ap()
    for n in WNAMES:
        dr[n] = nc.dram_tensor(n, WSHAPES[n], F32, kind="ExternalInput").ap()
    for n, shp in CSHAPES.items():
        dr[n] = nc.dram_tensor(n, shp, F32, kind="ExternalInput").ap()
    dr['out'] = nc.dram_tensor("out", [S, D], F32, kind="ExternalOutput").ap()
    sk = "ExternalOutput" if dbg else "Internal"
    dr['xs'] = nc.dram_tensor("xs", [S, D], F32, kind="Internal").ap()
    dr['ysa'] = nc.dram_tensor("ysa", [4, 64, S], BF16, kind=sk).ap()
    dr['ysb'] = nc.dram_tensor("ysb", [S, D], BF16, kind=sk).ap()
    dr['ysc'] = nc.dram_tensor("ysc", [S, D], BF16, kind=sk).ap()
    C.dr = dr
    P = Prog(nc)
    C.P = P
    A = Arena(P, 207 * 1024)
    C.A = A
    C.pst = [P.ps("psb%d" % i, [128, 512], F32) for i in range(8)]
    C.ps = [t[:, :] for t in C.pst]
    C.psb = [t.bitcast(BF16)[:, :] for t in C.pst]
    C.xT = A.alloc([8, S], BF16)
    C.ident = A.alloc([128], BF16)
    C.identf = A.alloc([128], F32)
    C.ones = A.alloc([512], BF16)
    P.dma('pool', C.ident, dr['c_ident'])
    P.dma('sp', C.identf, dr['c_ident'])
    P.memset('dve', C.ones, 1.0)
    C.memT = A.alloc([8, MEM], BF16)
    C.maskd = A.alloc([8, 512], BF16)
    C.maska = A.alloc([256], BF16)
    P.dma('pool', C.maskd, dr['c_maskd'])
    P.dma('pool', C.maska, dr['c_maska'])
    for st in stages:
        if st == 'ln0':
            stage_ln0(C, True)
        elif st == 'load':
            stage_ln0(C, False)
        elif st.startswith('moe'):
            stage_moe(C, int(st[3:4]), last=st.endswith('L'))
        elif st == 'out':
            stage_out(C)
        elif st == 'meminit':
            stage_meminit(C)
        elif st.startswith('xattn'):
            stage_xattn(C, int(st[5:]))
        elif st.startswith('mixA'):
            stage_mixA(C, int(st[4:]))
        elif st.startswith('mixB'):
            stage_mixB(C, int(st[4:]))
        elif st.startswith('mixC'):
            stage_mixC(C, int(st[4:]))
        elif st.startswith('merge'):
            stage_merge(C, int(st[5:]))
        else:
            raise ValueError(st)
    P.finish()
    P.emit()
    C.nop = P.nop
    C.nwait = P.nwait
    return nc, C


class LNState:
    pass


def ln_setup(C, g_ap, b_ap):
    P, A = C.P, C.A
    L = LNState()
    L.g = A.alloc([D], F32)
    L.b = A.alloc([D], F32)
    P.dma('sp', L.g, g_ap.broadcast_to([128, D]))
    P.dma('sp', L.b, b_ap.broadcast_to([128, D]))
    L.stats = [A.alloc([2, 6], F32) for _ in range(2)]
    L.mv = [A.alloc([2], F32) for _ in range(2)]
    L.sd = [A.alloc([1], F32) for _ in range(2)]
    L.rs = [A.alloc([1], F32) for _ in range(2)]
    L.xn = [A.alloc([D], F32) for _ in range(2)]
    L.xb = [A.alloc([D], BF16) for _ in range(2)]
    L.n = 0
    return L


def ln_tile(C, L, src, t, dst_dram, pst):
    P = C.P
    i = L.n % 2
    L.n += 1
    st, mv, sd, rs, xn, xb = L.stats[i], L.mv[i], L.sd[i], L.rs[i], L.xn[i], L.xb[i]
    for h in range(2):
        P.op('dve', lambda e, h=h: e.bn_stats(st[:, h, :], src[:, h * 512:(h + 1) * 512]),
             reads=[src[:, h * 512:(h + 1) * 512]], writes=[st[:, h, :]])
    P.op('dve', lambda e: e.bn_aggr(mv, st), reads=[st], writes=[mv])
    P.act(sd, mv[:, 1:2], AF.Sqrt, bias=EPS)
    P.op('dve', lambda e: e.reciprocal(rs, sd), reads=[sd], writes=[rs])
    P.ts('dve', xn, src, mv[:, 0:1], ALU.subtract, rs, ALU.mult)
    P.tt('dve', xn, xn, L.g, ALU.mult)
    P.tt('pool', xn, xn, L.b, ALU.add)
    P.dma('sp', dst_dram[t * 128:(t + 1) * 128, :], xn)
    P.copy('act', xb, xn)
    pv = pst.rearrange("p (c q) -> p c q", c=8)
    for c in range(8):
        P.tr(pv[:, c, :], xb[:, c * 128:(c + 1) * 128], C.ident)
    P.copy('dve', C.xT[:, :, t * 128:(t + 1) * 128], pv)


def stage_ln0(C, do_ln):
    P, A, dr = C.P, C.A, C.dr
    m = A.mark()
    L = ln_setup(C, dr['ln0_g'], dr['ln0_b'])
    xin = [A.alloc([D], F32) for _ in range(2)]
    for t in range(NT):
        xi = xin[t % 2]
        P.dma('sp', xi, dr['x'][t * 128:(t + 1) * 128, :])
        if do_ln:
            ln_tile(C, L, xi, t, dr['xs'], C.psb[t % 2])
        else:
            P.dma('sp', dr['xs'][t * 128:(t + 1) * 128, :], xi)
            xb = L.xb[t % 2]
            P.copy('act', xb, xi)
            pv = C.psb[t % 2].rearrange("p (c q) -> p c q", c=8)
            for c in range(8):
                P.tr(pv[:, c, :], xb[:, c * 128:(c + 1) * 128], C.ident)
            P.copy('dve', C.xT[:, :, t * 128:(t + 1) * 128], pv)
    A.release(m)


def stage_out(C):
    P, A, dr = C.P, C.A, C.dr
    m = A.mark()
    buf = [A.alloc([D], F32) for _ in range(2)]
    for t in range(NT):
        b = buf[t % 2]
        P.dma('sp', b, dr['xs'][t * 128:(t + 1) * 128, :])
        P.dma('sp', dr['out'][t * 128:(t + 1) * 128, :], b)
    A.release(m)


def stage_moe(C, li, last=False):
    dst_final = C.dr['out'] if last else C.dr['xs']
    P, A, dr = C.P, C.A, C.dr
    ps, psb = C.ps, C.psb
    xT = C.xT
    m0 = A.mark()
    wr = A.alloc([8, NE], BF16)
    P.dma('pool', wr, dr['w_router'][li].rearrange("(k p) n -> p k n", p=128))
    brow = A.alloc([NE], BF16)
    P.dma('pool', brow[0:1, :], dr['b_router'][li:li + 1, :])
    lg = A.alloc([NT, NE], F32)
    G = A.alloc([NT, NE], F32)
    m8 = A.alloc([NT, 8], F32)
    negmx = A.alloc([NT], F32)
    lgp = ps[0][:, :].rearrange("p (t e) -> p t e", t=NT)
    for t in range(NT):
        for k in range(8):
            P.mm(lgp[:, t, :], xT[:, k, t * 128:(t + 1) * 128], wr[:, k, :], start=(k == 0), stop=False)
        P.mm(lgp[:, t, :], C.ones[0:1, 0:128], brow[0:1, :], start=False, stop=True)
    P.copy('act', lg, lgp)
    tmpm = A.alloc([NE], F32)
    tmpe = A.alloc([NE], F32)
    ssum = A.alloc([1], F32)
    rsum = A.alloc([1], F32)
    for t in range(NT):
        P.op('dve', lambda e, t=t: e.max(m8[:, t, :], lg[:, t, :]), reads=[lg[:, t, :]], writes=[m8[:, t, :]])
    P.ts('dve', negmx, m8[:, :, 0], -1.0, ALU.mult)
    for t in range(NT):
        P.ts('dve', tmpm, lg[:, t, :], m8[:, t, 3:4], ALU.is_ge)
        P.act(tmpe, lg[:, t, :], AF.Exp, bias=negmx[:, t:t + 1])
        P.tt('dve', tmpe, tmpe, tmpm, ALU.mult)
        P.op('dve', lambda e: e.tensor_reduce(ssum, tmpe, AX.X, ALU.add), reads=[tmpe], writes=[ssum])
        P.op('dve', lambda e: e.reciprocal(rsum, ssum), reads=[ssum], writes=[rsum])
        P.ts('dve', G[:, t, :], tmpe, rsum, ALU.mult)
    bgu_raw = A.alloc([2048], F32)
    P.dma('sp', bgu_raw[0:NE, :], dr['b_gu'][li])
    bguT = A.alloc([16, NE], F32)
    bp = ps[1][:, :].rearrange("p (c e) -> p c e", c=16)
    for c in range(16):
        P.tr(bp[:, c, :], bgu_raw[0:NE, c * 128:(c + 1) * 128], C.identf[0:NE, 0:NE])
    P.copy('act', bguT, bp)
    bd = A.alloc([D], BF16)
    P.dma('pool', bd[0:NE, :], dr['b_down'][li])
    m1 = A.mark()
    NB = 8
    for half in range(2):
        A.release(m1)
        yacc = A.alloc([8, D], F32)
        m2 = A.mark()
        ring = [A.alloc([8, 512], BF16) for _ in range(NB)]
        actT = [A.alloc([8, 512], BF16) for _ in range(2)]
        tg = [A.alloc([512], F32) for _ in range(2)]
        tsg = [A.alloc([512], F32) for _ in range(2)]
        tu = [A.alloc([512], F32) for _ in range(2)]
        rr = [0]

        def getblk(src2d):
            b = ring[rr[0] % NB]
            rr[0] += 1
            P.dma('pool', b, src2d.rearrange("(k p) n -> p k n", p=128))
            return b

        pending = []
        cnt = 0
        fgc = 0
        dcn = 0

        def do_down(job):
            nonlocal dcn
            (e, tok0, aT, dblk) = job
            for sub in range(4):
                tile = (tok0 + sub * 128) // 128
                lt = tile - half * 8
                for hc in range(2):
                    pD = ps[4 + dcn % 4]
                    dcn += 1
                    for fk in range(8):
                        P.mm(pD, aT[:, fk, sub * 128:(sub + 1) * 128], dblk[hc][:, fk, :], start=(fk == 0), stop=(fk == 7))
                    ysl = yacc[:, lt, hc * 512:(hc + 1) * 512]
                    if e == 0:
                        P.ts('dve', ysl, pD, G[:, tile, e:e + 1], ALU.mult)
                    else:
                        P.stt(ysl, pD, G[:, tile, e:e + 1], ysl, ALU.mult, ALU.add)

        for e in range(NE):
            gu = [getblk(dr['w_gu'][li, e][:, b * 512:(b + 1) * 512]) for b in range(4)]
            dblk = [getblk(dr['w_down'][li, e][:, b * 512:(b + 1) * 512]) for b in range(2)]
            for qt in range(2):
                tok0 = half * 1024 + qt * 512
                aT = actT[cnt % 2]
                cnt += 1
                for fg in range(8):
                    pA = ps[(2 * fgc) % 4]
                    pB = ps[(2 * fgc + 1) % 4]
                    i2 = fgc % 2
                    fgc += 1
                    cols = (fg % 4) * 128
                    bg = gu[fg // 4]
                    bu = gu[2 + fg // 4]
                    for k in range(8):
                        P.mm(pA, bg[:, k, cols:cols + 128], xT[:, k, tok0:tok0 + 512], start=(k == 0), stop=(k == 7))
                    for k in range(8):
                        P.mm(pB, bu[:, k, cols:cols + 128], xT[:, k, tok0:tok0 + 512], start=(k == 0), stop=(k == 7))
                    g1, sg, ua = tg[i2], tsg[i2], tu[i2]
                    P.ts('dve', g1, pA, bguT[:, fg, e:e + 1], ALU.add, 7.0, ALU.min)
                    P.act(sg, g1, AF.Sigmoid, scale=1.702)
                    P.act(ua, pB, AF.Identity, bias=bguT[:, 8 + fg, e:e + 1])
                    P.ts('dve', ua, ua, 7.0, ALU.min, -7.0, ALU.max)
                    P.tt('dve', g1, g1, sg, ALU.mult)
                    P.stt(aT[:, fg, :], ua, 1.0, g1, ALU.add, ALU.mult)
                pending.append((e, tok0, aT, dblk))
                if len(pending) > 1:
                    do_down(pending.pop(0))
        while pending:
            do_down(pending.pop(0))
        A.release(m2)
        L = ln_setup(C, dr['ln3_g'][li:li + 1, :], dr['ln3_b'][li:li + 1, :])
        gTb = [A.alloc([128], BF16) for _ in range(2)]
        xo = [A.alloc([D], F32) for _ in range(2)]
        for lt in range(8):
            tile = half * 8 + lt
            i2 = lt % 2
            gp = ps[0][0:NE, 0:128]
            P.tr(gp, G[:, tile, :], C.identf)
            P.copy('act', gTb[i2][0:NE, :], gp)
            for hc in range(2):
                P.mm(ps[2 + hc], gTb[i2][0:NE, :], bd[0:NE, hc * 512:(hc + 1) * 512], start=True, stop=True)
            P.dma('sp', xo[i2], dr['xs'][tile * 128:(tile + 1) * 128, :])
            P.stt(xo[i2], xo[i2], ALPHA, yacc[:, lt, :], ALU.mult, ALU.add)
            for hc in range(2):
                P.tt('dve', xo[i2][:, hc * 512:(hc + 1) * 512], xo[i2][:, hc * 512:(hc + 1) * 512], ps[2 + hc], ALU.add)
            ln_tile(C, L, xo[i2], tile, dst_final, psb[1])
    A.release(m0)


def run_pipe(jobs, depth=1):
    q = []
    for (s1, s2) in jobs:
        ctx = s1()
        q.append((s2, ctx))
        if len(q) > depth:
            f, c = q.pop(0)
            f(c)
    while q:
        f, c = q.pop(0)
        f(c)


def load_w(C, dst, src2d, q='pool'):
    C.P.dma(q, dst, src2d.rearrange("(k p) n -> p k n", p=128))


def bias_col(C, dst, src_row):
    C.P.dma('sp', dst, src_row.rearrange("o (p i) -> (o p) i", i=1))


def stage_meminit(C):
    P, A, dr = C.P, C.A, C.dr
    m = A.mark()
    mf = A.alloc([2, D], F32)
    mb = A.alloc([2, D], BF16)
    P.dma('sp', mf, dr['mem'].rearrange("(b p) d -> p b d", p=128))
    P.copy('act', mb, mf)
    for b in range(2):
        pv = C.psb[b].rearrange("p (c q) -> p c q", c=8)
        for c in range(8):
            P.tr(pv[:, c, :], mb[:, b, c * 128:(c + 1) * 128], C.ident)
        P.copy('dve', C.memT[:, :, b * 128:(b + 1) * 128], pv)
    A.release(m)


def stage_xattn(C, li):
    P, A, dr = C.P, C.A, C.dr
    ps, psb, xT = C.ps, C.psb, C.xT
    m0 = A.mark()
    kTx = A.alloc([4, 2, MEM], BF16)
    Vx = A.alloc([2, 4, 258], BF16)
    wk = A.alloc([8, D], BF16)
    wv = A.alloc([8, D], BF16)
    wq = A.alloc([8, D], BF16)
    wo = A.alloc([8, D], BF16)
    load_w(C, wk, dr['w_xk'][li])
    load_w(C, wv, dr['w_xv'][li])
    load_w(C, wq, dr['w_xq'][li])
    load_w(C, wo, dr['w_xo'][li])
    P.memset('dve', Vx[:, :, :, 256:258], 1.0)
    n = 0
    for h in range(4):
        for c in range(2):
            pp = ps[n % 2][:, 0:MEM]
            n += 1
            col = h * 256 + c * 128
            for k in range(8):
                P.mm(pp, wk[:, k, col:col + 128], C.memT[:, k, :], start=(k == 0), stop=(k == 7))
            P.copy('act', kTx[:, h, c, :], pp)
    for mb in range(2):
        for hc in range(2):
            pp = ps[2 + hc]
            for k in range(8):
                P.mm(pp, C.memT[:, k, mb * 128:(mb + 1) * 128], wv[:, k, hc * 512:(hc + 1) * 512], start=(k == 0), stop=(k == 7))
            P.copy('act', Vx[:, mb, hc * 2:hc * 2 + 2, 0:256], pp.rearrange("p (h d) -> p h d", h=2))
    L = ln_setup(C, dr['ln2_g'][li:li + 1, :], dr['ln2_b'][li:li + 1, :])
    qTx = [A.alloc([8, 512], BF16) for _ in range(2)]
    ox = [A.alloc([4, D], BF16) for _ in range(2)]
    oT = [A.alloc([8, 128], BF16) for _ in range(2)]
    xo = [A.alloc([D], F32) for _ in range(2)]
    rec = [A.alloc([1], F32) for _ in range(4)]
    NPX = 8
    pT = [A.alloc([512], BF16) for _ in range(NPX)]
    cnt = {'npt': 0, 'nr': 0, 'no': 0}
    jobs = []
    for qt in range(4):
        for h in range(4):
            def s1(qt=qt, h=h):
                qx = qTx[qt % 2]
                if h == 0:
                    for j in range(8):
                        pp = ps[j % 2]
                        for k in range(8):
                            P.mm(pp, wq[:, k, j * 128:(j + 1) * 128], xT[:, k, qt * 512:(qt + 1) * 512], start=(k == 0), stop=(k == 7))
                        P.copy('act', qx[:, j, :], pp)
                pts = []
                for mb in range(2):
                    sp_ = ps[2 + mb]
                    for c in range(2):
                        P.mm(sp_, kTx[:, h, c, mb * 128:(mb + 1) * 128], qx[:, 2 * h + c, :], start=(c == 0), stop=(c == 1))
                    pt = pT[cnt['npt'] % NPX]
                    cnt['npt'] += 1
                    P.act(pt, sp_, AF.Exp, scale=1.0 / 16.0)
                    pts.append(pt)
                return pts

            def s2(pts, qt=qt, h=h):
                oxx = ox[qt % 2]
                for sub in range(4):
                    po = ps[4 + (cnt['no'] % 2)][:, 0:257]
                    cnt['no'] += 1
                    for mb in range(2):
                        P.mm(po, pts[mb][:, sub * 128:(sub + 1) * 128], Vx[:, mb, h, 0:257], start=(mb == 0), stop=(mb == 1))
                    rc = rec[cnt['nr'] % 4]
                    cnt['nr'] += 1
                    P.op('dve', lambda e, rc=rc, po=po: e.reciprocal(rc, po[:, 256:257]), reads=[po[:, 256:257]], writes=[rc])
                    P.ts('dve', oxx[:, sub, h * 256:(h + 1) * 256], po[:, 0:256], rc, ALU.mult)
                if h == 3:
                    for sub in range(4):
                        tile = qt * 4 + sub
                        i2 = sub % 2
                        pv = psb[6].rearrange("p (c q) -> p c q", c=8)
                        for c in range(8):
                            P.tr(pv[:, c, :], oxx[:, sub, c * 128:(c + 1) * 128], C.ident)
                        P.copy('act', oT[i2], pv)
                        P.dma('sp', xo[i2], dr['xs'][tile * 128:(tile + 1) * 128, :])
                        for hc in range(2):
                            pp = ps[hc]
                            for k in range(8):
                                P.mm(pp, oT[i2][:, k, :], wo[:, k, hc * 512:(hc + 1) * 512], start=(k == 0), stop=(k == 7))
                            P.stt(xo[i2][:, hc * 512:(hc + 1) * 512], xo[i2][:, hc * 512:(hc + 1) * 512], ALPHA, pp, ALU.mult, ALU.add)
                        ln_tile(C, L, xo[i2], tile, dr['xs'], psb[7])
            jobs.append((s1, s2))
    run_pipe(jobs, 2)
    A.release(m0)


def stage_mixA(C, li):
    P, A, dr = C.P, C.A, C.dr
    ps, psb, xT = C.ps, C.psb, C.xT
    m0 = A.mark()
    accA = A.alloc([4, S], F32)
    qT = A.alloc([2, S], BF16)
    kT = A.alloc([2, S], BF16)
    Va = A.alloc([16, 4, 128], BF16)
    WA = [dict(wq=A.alloc([8, 256], BF16), wk=A.alloc([8, 256], BF16), wv=A.alloc([8, 256], BF16),
               bq=A.alloc([2], F32), bk=A.alloc([2], F32), bvr=A.alloc([256], BF16)) for _ in range(2)]

    def load_group_a(g):
        W = WA[g % 2]
        load_w(C, W['wq'], dr['w_in'][li][:, O_AQ + g * 256:O_AQ + (g + 1) * 256])
        load_w(C, W['wk'], dr['w_in'][li][:, O_AK + g * 256:O_AK + (g + 1) * 256])
        load_w(C, W['wv'], dr['w_in'][li][:, O_AV + g * 256:O_AV + (g + 1) * 256])
        for j in range(2):
            bias_col(C, W['bq'][:, j:j + 1], dr['b_in'][li:li + 1, O_AQ + g * 256 + j * 128:O_AQ + g * 256 + (j + 1) * 128])
            bias_col(C, W['bk'][:, j:j + 1], dr['b_in'][li:li + 1, O_AK + g * 256 + j * 128:O_AK + g * 256 + (j + 1) * 128])
        P.dma('pool', W['bvr'][0:1, :], dr['b_in'][li:li + 1, O_AV + g * 256:O_AV + (g + 1) * 256])
    load_group_a(0)
    load_group_a(1)
    NPA = 8
    pT = [A.alloc([256], BF16) for _ in range(NPA)]
    P.memset('dve', Va[:, :, :, 64:128], 1.0)
    DIL = [1, 4, 16]
    npt = 0
    nsp = 0
    nob = 0
    for g in range(3):
        dil = DIL[g]
        nb = S // dil // 128
        W = WA[g % 2]
        wq, wk, wv, bq, bk, bvr = W['wq'], W['wk'], W['wv'], W['bq'], W['bk'], W['bvr']
        n = 0
        for (w_, b_, dst) in ((wq, bq, qT), (wk, bk, kT)):
            for j in range(2):
                for qt in range(4):
                    pp = ps[n % 2]
                    n += 1
                    for k in range(8):
                        P.mm(pp, w_[:, k, j * 128:(j + 1) * 128], xT[:, k, qt * 512:(qt + 1) * 512], start=(k == 0), stop=(k == 7))
                    P.act(dst[:, j, qt * 512:(qt + 1) * 512], pp, AF.Identity, bias=b_[:, j:j + 1])

        def toks(r, blk, cnt):
            st = blk * 128 * dil + r
            return slice(st, st + (cnt - 1) * dil + 1, dil)

        for r in range(dil):
            for blk in range(nb):
                bi = r * nb + blk
                pp = ps[2 + bi % 2][:, 0:256]
                for k in range(8):
                    P.mm(pp, xT[:, k, toks(r, blk, 128)], wv[:, k, :], start=(k == 0), stop=False)
                P.mm(pp, C.ones[0:1, 0:128], bvr[0:1, :], start=False, stop=True)
                P.copy('act', Va[:, bi, :, 0:64], pp.rearrange("p (h d) -> p h d", h=4))
        if g == 0:
            load_group_a(2)
        jobs = []
        for h in range(4):
            for r0 in range(0, dil, 4 if g == 2 else 1):
                rs_ = list(range(r0, min(dil, r0 + (4 if g == 2 else 1))))
                if g < 2:
                    batches = [[(rs_[0], qb) for qb in range(b0, b0 + 4)] for b0 in range(0, nb, 4)]
                else:
                    batches = [[(r, 0) for r in rs_]]
                prevp = {}
                for batch in batches:
                    bst = {'ob': None}
                    for si, (r, qb) in enumerate(batch):
                        def s1(h=h, r=r, qb=qb):
                            nonlocal nsp, npt
                            j = h // 2
                            pb = (h % 2) * 64
                            nq = 256 if qb < nb - 1 else 128
                            sp_ = ps[4 + nsp % 2][:, 0:nq]
                            nsp += 1
                            P.mm(sp_, kT[pb:pb + 64, j, toks(r, qb, 128)], qT[pb:pb + 64, j, toks(r, qb, nq)], start=True, stop=True)
                            pt = pT[npt % NPA]
                            npt += 1
                            P.act(pt[:, 0:nq], sp_, AF.Exp, scale=0.125)
                            P.tt('dve', pt[:, 0:nq], pt[:, 0:nq], C.maska[:, 0:nq], ALU.mult)
                            return pt

                        def s2(pt, h=h, r=r, qb=qb, si=si, batch=batch, bst=bst, prevp=prevp, rs_=rs_):
                            nonlocal nob
                            if bst['ob'] is None:
                                bst['ob'] = ps[6 + nob % 2]
                                nob += 1
                            ob = bst['ob']
                            oo = ob[:, si * 128:(si + 1) * 128]
                            first = True
                            if qb > 0:
                                pp_ = prevp[(r, qb - 1)]
                                P.mm(oo, Va[:, r * nb + qb - 1, h, :], pp_[:, 128:256], start=True, stop=False)
                                first = False
                            P.mm(oo, Va[:, r * nb + qb, h, :], pt[:, 0:128], start=first, stop=True)
                            prevp[(r, qb)] = pt
                            if si == len(batch) - 1:
                                nbk = len(batch)
                                if g < 2:
                                    r_, qb0 = batch[0]
                                    st = qb0 * 128 * dil + r_
                                    dst = accA[:, h, st:st + (nbk * 128 - 1) * dil + 1:dil]
                                    src = ob[:, 0:nbk * 128]
                                else:
                                    dst = accA[:, h, :].rearrange("p (i r) -> p i r", r=16)[:, :, rs_[0]:rs_[0] + nbk]
                                    src = ob[:, 0:nbk * 128].rearrange("p (r i) -> p i r", r=nbk)
                                if g == 0:
                                    P.copy('act', dst, src)
                                else:
                                    P.tt('dve', dst, dst, src, ALU.add)
                        jobs.append((s1, s2))
        run_pipe(jobs, 2)
    rt = A.alloc([S], F32)
    rsft = A.alloc([S], F32)
    yb_ = [A.alloc([S], BF16) for _ in range(2)]
    for h in range(4):
        P.op('dve', lambda e, h=h: e.reciprocal(rt[64:128, :], accA[64:128, h, :]), reads=[accA[64:128, h, :]], writes=[rt[64:128, :]])
        P.copy('act', rsft[0:64, :], rt[64:128, :])
        P.tt('dve', yb_[h % 2][0:64, :], accA[0:64, h, :], rsft[0:64, :], ALU.mult)
        P.dma('sp', dr['ysa'][h], yb_[h % 2][0:64, :])
    A.release(m0)


def stage_mixB(C, li):
    P, A, dr = C.P, C.A, C.dr
    ps, psb, xT = C.ps, C.psb, C.xT
    m0 = A.mark()
    d0 = A.alloc([S], BF16)
    P.memset('dve', d0, 1.0)
    P.memset('dve', d0.rearrange("p (t i) -> p t i", i=128)[:, :, 0:1], 0.0)
    wlr = A.alloc([8, 16], BF16)
    load_w(C, wlr, dr['w_in'][li][:, O_BLR:O_BLR + 16])
    blr = A.alloc([1], F32)
    bias_col(C, blr[0:16, :], dr['b_in'][li:li + 1, O_BLR:O_BLR + 16])
    lrT = A.alloc([S], BF16)
    for qt in range(4):
        pp = ps[qt % 2][0:16, :]
        for k in range(8):
            P.mm(pp, wlr[:, k, :], xT[:, k, qt * 512:(qt + 1) * 512], start=(k == 0), stop=(k == 7))
        P.act(lrT[0:16, qt * 512:(qt + 1) * 512], pp, AF.Identity, bias=blr[0:16, :])
    wa2 = A.alloc([512], BF16)
    P.dma('pool', wa2[0:16, :], dr['w_alpha2'][li])
    bal = A.alloc([4], F32)
    for h in range(4):
        bias_col(C, bal[:, h:h + 1], dr['b_alpha'][li:li + 1, h * 128:(h + 1) * 128])
    nbal = A.alloc([4], F32)
    P.ts('dve', nbal, bal, -1.0, ALU.mult)
    ngb = A.alloc([256], F32)
    P.dma('sp', ngb, dr['gla_norm_g'][li:li + 1, :].broadcast_to([128, 256]))
    cs = A.alloc([S], F32)
    WB = [dict(wq=A.alloc([8, 128], BF16), wk=A.alloc([8, 128], BF16), wv=A.alloc([8, 256], BF16), wr=A.alloc([8, 256], BF16),
               bq=A.alloc([1], F32), bk=A.alloc([1], F32), bvr=A.alloc([256], BF16), brr=A.alloc([256], BF16)) for _ in range(2)]

    def load_head_b(h, i):
        W = WB[i]
        load_w(C, W['wq'], dr['w_in'][li][:, O_BQ + h * 128:O_BQ + (h + 1) * 128])
        load_w(C, W['wk'], dr['w_in'][li][:, O_BK + h * 128:O_BK + (h + 1) * 128])
        load_w(C, W['wv'], dr['w_in'][li][:, O_BV + h * 256:O_BV + (h + 1) * 256])
        load_w(C, W['wr'], dr['w_in'][li][:, O_BR + h * 256:O_BR + (h + 1) * 256])
        bias_col(C, W['bq'], dr['b_in'][li:li + 1, O_BQ + h * 128:O_BQ + (h + 1) * 128])
        bias_col(C, W['bk'], dr['b_in'][li:li + 1, O_BK + h * 128:O_BK + (h + 1) * 128])
        P.dma('pool', W['bvr'][0:1, :], dr['b_in'][li:li + 1, O_BV + h * 256:O_BV + (h + 1) * 256])
        P.dma('pool', W['brr'][0:1, :], dr['b_in'][li:li + 1, O_BR + h * 256:O_BR + (h + 1) * 256])
    load_head_b(0, 0)
    load_head_b(1, 1)
    eqs = [A.alloc([S], F32) for _ in range(2)]
    enbs = [A.alloc([S], F32) for _ in range(2)]
    elasts = [A.alloc([NT], F32) for _ in range(2)]
    qtls = [A.alloc([S], BF16) for _ in range(2)]
    ktls = [A.alloc([S], BF16) for _ in range(2)]
    Vs_ = [A.alloc([NT, 256], BF16) for _ in range(2)]
    srs = [A.alloc([NT, 256], F32) for _ in range(2)]
    ybts = [A.alloc([2, 256], BF16) for _ in range(2)]
    Sts = [A.alloc([256], F32) for _ in range(2)]
    Sbs = [A.alloc([256], BF16) for _ in range(2)]
    aTms = [A.alloc([128], BF16) for _ in range(2)]
    kTts = [A.alloc([128], BF16) for _ in range(2)]
    ssqs = [A.alloc([1], F32) for _ in range(2)]
    sds = [A.alloc([1], F32) for _ in range(2)]
    rstds = [A.alloc([1], F32) for _ in range(2)]
    junks = [A.alloc([256], F32) for _ in range(2)]
    LNQ = float(np.log(128.0 ** -0.5))
    ysb_v = dr['ysb'].rearrange("(t p) c -> p t c", p=128)

    def setup(h, i):
        eq, enb, elast, qtl, ktl, V, sr = eqs[i], enbs[i], elasts[i], qtls[i], ktls[i], Vs_[i], srs[i]
        W = WB[i]
        wq, wk, wv, wr, bq, bk, bvr, brr = W['wq'], W['wk'], W['wv'], W['wr'], W['bq'], W['bk'], W['bvr'], W['brr']
        for qt in range(4):
            pp = ps[qt % 2]
            P.mm(pp, wa2[0:16, h * 128:(h + 1) * 128], lrT[0:16, qt * 512:(qt + 1) * 512], start=True, stop=True)
            P.act(eq[:, qt * 512:(qt + 1) * 512], pp, AF.Exp, bias=nbal[:, h:h + 1], scale=-1.0)
        P.act(enb, eq, AF.Ln, bias=1.0)
        P.op('dve', lambda e: e.tensor_tensor_scan(cs, d0, enb, 0.0, ALU.mult, ALU.add), reads=[d0, enb], writes=[cs])
        P.act(eq, cs, AF.Exp, scale=-1.0 / 16.0, bias=LNQ)
        P.act(enb, cs, AF.Exp, scale=1.0 / 16.0)
        P.act(elast, cs.rearrange("p (t i) -> p t i", i=128)[:, :, 127], AF.Exp, scale=-1.0 / 16.0)
        for qt in range(4):
            sl = slice(qt * 512, (qt + 1) * 512)
            pp = ps[qt % 2]
            for k in range(8):
                P.mm(pp, wq[:, k, :], xT[:, k, sl], start=(k == 0), stop=(k == 7))
            P.stt(qtl[:, sl], pp, bq, eq[:, sl], ALU.add, ALU.mult)
            pp2 = ps[2 + qt % 2]
            for k in range(8):
                P.mm(pp2, wk[:, k, :], xT[:, k, sl], start=(k == 0), stop=(k == 7))
            P.stt(ktl[:, sl], pp2, bk, enb[:, sl], ALU.add, ALU.mult)
        for t in range(NT):
            tsl = slice(t * 128, (t + 1) * 128)
            pp = ps[t % 2][:, 0:256]
            for k in range(8):
                P.mm(pp, xT[:, k, tsl], wv[:, k, :], start=(k == 0), stop=False)
            P.mm(pp, C.ones[0:1, 0:128], bvr[0:1, :], start=False, stop=True)
            P.copy('act', V[:, t, :], pp)
            pp2 = ps[2 + t % 2][:, 0:256]
            for k in range(8):
                P.mm(pp2, xT[:, k, tsl], wr[:, k, :], start=(k == 0), stop=False)
            P.mm(pp2, C.ones[0:1, 0:128], brr[0:1, :], start=False, stop=True)
            P.act(sr[:, t, :], pp2, AF.Silu)
            P.tt('pool', sr[:, t, :], sr[:, t, :], ngb, ALU.mult)

    def step(h, i, t):
        elast, qtl, ktl, V, sr, ybt = elasts[i], qtls[i], ktls[i], Vs_[i], srs[i], ybts[i]
        St, Sb = Sts[i], Sbs[i]
        tsl = slice(t * 128, (t + 1) * 128)
        pa = ps[4 + i][:, 0:128]
        P.mm(pa, ktl[:, tsl], qtl[:, tsl], start=True, stop=True)
        P.tt('dve', aTms[i], pa, C.maskd[:, 0, 0:128], ALU.mult)
        po = ps[6 + i][:, 0:256]
        P.mm(po, aTms[i], V[:, t, :], start=True, stop=(t == 0))
        if t > 0:
            P.mm(po, qtl[:, tsl], Sb, start=False, stop=True)
        P.act(junks[i], po, AF.Square, accum_out=ssqs[i])
        P.act(sds[i], ssqs[i], AF.Sqrt, scale=1.0 / 256.0, bias=EPS)
        P.op('dve', lambda e: e.reciprocal(rstds[i], sds[i]), reads=[sds[i]], writes=[rstds[i]])
        P.stt(ybt[:, t % 2, :], po, rstds[i], sr[:, t, :], ALU.mult, ALU.mult)
        P.dma('sp', dr['ysb'][t * 128:(t + 1) * 128, h * 256:(h + 1) * 256], ybt[:, t % 2, :])
        if t < NT - 1:
            pk = psb[4 + i][:, 256:384]
            P.tr(pk, ktl[:, tsl], C.ident)
            P.copy('act', kTts[i], pk)
            pm = ps[2 + i][:, 256:512]
            P.mm(pm, kTts[i], V[:, t, :], start=True, stop=True)
            if t == 0:
                P.ts('dve', St, pm, elast[:, 0:1], ALU.mult)
            else:
                P.tt('dve', St, St, pm, ALU.add)
                P.ts('dve', St, St, elast[:, t:t + 1], ALU.mult)
            P.copy('act', Sb, St)

    for hp in (0, 2):
        for i in range(2):
            setup(hp + i, i)
        if hp == 0:
            load_head_b(2, 0)
            load_head_b(3, 1)
        for t in range(NT):
            for i in range(2):
                step(hp + i, i, t)
    A.release(m0)


def stage_mixC(C, li):
    P, A, dr = C.P, C.A, C.dr
    ps, psb, xT = C.ps, C.psb, C.xT
    m0 = A.mark()
    cmpv = A.alloc([S], BF16)
    P.dma('pool', cmpv, dr['c_cmpvalid'])
    keep = A.alloc([NT, 32], F32)
    addb = A.alloc([NT, 32], F32)
    P.dma('sp', keep, dr['c_keep'].rearrange("(t p) j -> p t j", p=128))
    P.dma('sp', addb, dr['c_add'].rearrange("(t p) j -> p t j", p=128))
    wcg = A.alloc([8, 48], BF16)
    load_w(C, wcg, dr['w_in'][li][:, O_CG:O_CG + 48])
    bcg = A.alloc([48], BF16)
    P.dma('pool', bcg[0:1, :], dr['b_in'][li:li + 1, O_CG:O_CG + 48])
    gates = A.alloc([NT, 48], F32)
    for t in range(NT):
        pp = ps[t % 2][:, 0:48]
        for k in range(8):
            P.mm(pp, xT[:, k, t * 128:(t + 1) * 128], wcg[:, k, :], start=(k == 0), stop=False)
        P.mm(pp, C.ones[0:1, 0:128], bcg[0:1, :], start=False, stop=True)
        P.act(gates[:, t, :], pp, AF.Sigmoid)
    WC = [dict(wq=A.alloc([8, 256], BF16), wkc=A.alloc([8, 64], BF16), wvc=A.alloc([8, 64], BF16), wks=A.alloc([8, 64], BF16),
               wkw=A.alloc([8, 64], BF16), wvs=A.alloc([8, 64], BF16), wvw=A.alloc([8, 64], BF16), bq=A.alloc([4], F32),
               bkc=A.alloc([1], F32), bvc=A.alloc([1], F32), bks=A.alloc([1], F32), bkw=A.alloc([1], F32),
               bvs=A.alloc([64], BF16), bvw=A.alloc([64], BF16)) for _ in range(2)]

    def load_group_c(g):
        W = WC[g % 2]
        cq0 = O_CQ + g * 256
        load_w(C, W['wq'], dr['w_in'][li][:, cq0:cq0 + 256])
        for nm, off in (('wkc', O_CKC), ('wvc', O_CVC), ('wks', O_CKS), ('wkw', O_CKW), ('wvs', O_CVS), ('wvw', O_CVW)):
            load_w(C, W[nm], dr['w_in'][li][:, off + g * 64:off + (g + 1) * 64])
        for r in range(4):
            bias_col(C, W['bq'][0:64, r:r + 1], dr['b_in'][li:li + 1, cq0 + r * 64:cq0 + (r + 1) * 64])
        for nm, off in (('bkc', O_CKC), ('bvc', O_CVC), ('bks', O_CKS), ('bkw', O_CKW)):
            bias_col(C, W[nm][0:64, :], dr['b_in'][li:li + 1, off + g * 64:off + (g + 1) * 64])
        P.dma('pool', W['bvs'][0:1, :], dr['b_in'][li:li + 1, O_CVS + g * 64:O_CVS + (g + 1) * 64])
        P.dma('pool', W['bvw'][0:1, :], dr['b_in'][li:li + 1, O_CVW + g * 64:O_CVW + (g + 1) * 64])
    load_group_c(0)
    load_group_c(1)
    qT = A.alloc([4, S], BF16)
    negst = A.alloc([S], BF16)
    kcin = A.alloc([S], BF16)
    vcin = A.alloc([S], BF16)
    ksd = A.alloc([S], BF16)
    kwd = A.alloc([S], BF16)
    VS = A.alloc([NT, 66], BF16)
    VW = A.alloc([NT, 66], BF16)
    P.memset('dve', qT[64:128, :, :], 0.0)
    P.memset('dve', ksd[64:128, :], 0.0)
    P.memset('dve', kwd[64:128, :], 0.0)
    P.dma('pool', ksd[64:96, :], dr['c_eexp'])
    P.memset('dve', VS[:, :, 64:66], 1.0)
    P.memset('dve', VW[:, :, 64:66], 1.0)
    w1k = A.alloc([32, 128], BF16)
    w1v = A.alloc([32, 128], BF16)
    w2kd = A.alloc([128], BF16)
    w2v = A.alloc([64], BF16)
    pe_raw = A.alloc([64], F32)
    peT = A.alloc([2, 32], BF16)
    hb = A.alloc([2], F32)
    gh = A.alloc([2, 128], BF16)
    kcTd = A.alloc([128], BF16)
    VC = A.alloc([98], BF16)
    ovl = A.alloc([32], BF16)
    P.dma('pool', ovl, dr['c_overlap'])
    yc = A.alloc([NT, 256], F32)
    impacc = A.alloc([NT, 32], F32)
    NPT = 6
    pT = [A.alloc([512], BF16) for _ in range(NPT)]
    rec = [A.alloc([4], F32) for _ in range(4)]
    gsc = [A.alloc([4], F32) for _ in range(4)]
    imadj = [A.alloc([32], F32) for _ in range(2)]
    imw = [A.alloc([32], F32) for _ in range(2)]
    m8a = [A.alloc([8], F32) for _ in range(2)]
    m8b = [A.alloc([8], F32) for _ in range(2)]
    selm = [A.alloc([32], F32) for _ in range(2)]
    selb = [A.alloc([32], BF16) for _ in range(2)]
    P.dma('pool', w1k[0:64, :, :], dr['cmp_w1_k'][li].rearrange("(l d) h -> d l h", d=64))
    P.dma('pool', w1v[0:64, :, :], dr['cmp_w1_v'][li].rearrange("(l d) h -> d l h", d=64))
    P.dma('pool', w2kd[:, 0:64], dr['cmp_w2_k'][li])
    P.dma('pool', w2kd[:, 64:128], dr['cmp_w2_k'][li])
    P.dma('pool', w2v, dr['cmp_w2_v'][li])
    for wi, (pn, w1) in enumerate((('cmp_pe_k', w1k), ('cmp_pe_v', w1v))):
        P.dma('sp', pe_raw[0:32, :], dr[pn][li])
        pp = ps[wi][0:64, 0:32]
        P.tr(pp, pe_raw[0:32, :], C.identf[0:32, 0:32])
        P.copy('act', peT[0:64, wi, :], pp)
        pb_ = ps[2 + wi][:, 0:1]
        for l in range(32):
            P.mm(pb_, w1[0:64, l, :], peT[0:64, wi, l:l + 1], start=(l == 0), stop=(l == 31))
        P.copy('act', hb[:, wi:wi + 1], pb_)
    ysc_v = dr['ysc'].rearrange("(t p) c -> p t c", p=128)
    nsp = 0
    npt = 0
    nob = 0
    nrc = 0

    def evac(po, W, qt, gcol, r, first_y, imp_mode):
        nonlocal nrc
        rc = rec[nrc % 4]
        gs = gsc[nrc % 4]
        nrc += 1
        if imp_mode is not None:
            P.ts('dve', rc, po[:, :, 64], 1e-30, ALU.max)
            P.op('dve', lambda e: e.reciprocal(rc, rc), reads=[rc], writes=[rc])
        else:
            P.op('dve', lambda e: e.reciprocal(rc, po[:, :, 64]), reads=[po[:, :, 64]], writes=[rc])
        P.tt('dve', gs, rc, gates[:, qt * 4:(qt + 1) * 4, gcol], ALU.mult)
        for sub in range(4):
            t = qt * 4 + sub
            ysl = yc[:, t, r * 64:(r + 1) * 64]
            if first_y:
                P.ts('dve', ysl, po[:, sub, 0:64], gs[:, sub:sub + 1], ALU.mult)
            else:
                P.stt(ysl, po[:, sub, 0:64], gs[:, sub:sub + 1], ysl, ALU.mult, ALU.add)
            if imp_mode == 'first':
                P.ts('dve', impacc[:, t, :], po[:, sub, 65:97], rc[:, sub:sub + 1], ALU.mult)
            elif imp_mode == 'add':
                P.stt(impacc[:, t, :], po[:, sub, 65:97], rc[:, sub:sub + 1], impacc[:, t, :], ALU.mult, ALU.add)

    for g in range(4):
        W = WC[g % 2]
        wq, wkc, wvc, wks, wkw, wvs, wvw = W['wq'], W['wkc'], W['wvc'], W['wks'], W['wkw'], W['wvs'], W['wvw']
        bq, bkc, bvc, bks, bkw, bvs, bvw = W['bq'], W['bkc'], W['bvc'], W['bks'], W['bkw'], W['bvs'], W['bvw']
        n = 0
        jobs = [(wq[:, :, r * 64:(r + 1) * 64], bq[0:64, r:r + 1], qT[0:64, r, :], 64) for r in range(4)]
        jobs += [(wkc, bkc[0:64, :], kcin[0:64, :], 64), (wvc, bvc[0:64, :], vcin[0:64, :], 64),
                 (wks, bks[0:64, :], ksd[0:64, :], 64), (wkw, bkw[0:64, :], kwd[0:64, :], 64)]
        for (w_, b_, dst, m_) in jobs:
            for qt in range(4):
                pp = ps[n % 2][0:m_, :]
                n += 1
                for k in range(8):
                    P.mm(pp, w_[:, k, :], xT[:, k, qt * 512:(qt + 1) * 512], start=(k == 0), stop=(k == 7))
                P.act(dst[:, qt * 512:(qt + 1) * 512], pp, AF.Identity, bias=b_)
        for (w_, br_, dst) in ((wvs, bvs, VS), (wvw, bvw, VW)):
            for t in range(NT):
                pp = ps[2 + t % 2][:, 0:64]
                for k in range(8):
                    P.mm(pp, xT[:, k, t * 128:(t + 1) * 128], w_[:, k, :], start=(k == 0), stop=False)
                P.mm(pp, C.ones[0:1, 0:128], br_[0:1, :], start=False, stop=True)
                P.copy('act', dst[:, t, 0:64], pp)
        if 1 <= g <= 2:
            load_group_c(g + 1)
        for wi, (src, w1) in enumerate(((kcin, w1k), (vcin, w1v))):
            ph = ps[4 + wi][:, 0:127]
            for l in range(32):
                P.mm(ph, w1[0:64, l, :], src[0:64, l:l + 16 * 126 + 1:16], start=(l == 0), stop=(l == 31))
            P.act(gh[:, wi, 0:127], ph, AF.Gelu_apprx_tanh, bias=hb[:, wi:wi + 1])
        pk = ps[6][:, 0:127]
        P.mm(pk, w2kd, gh[:, 0, 0:127], start=True, stop=True)
        P.memset('dve', kcTd, 0.0)
        P.copy('act', kcTd[0:64, 0:127], pk[0:64, :])
        pv_ = ps[7][0:127, 0:64]
        P.mm(pv_, gh[:, 1, 0:127], w2v, start=True, stop=True)
        P.memset('dve', VC, 0.0)
        P.copy('act', VC[0:127, 0:64], pv_)
        P.memset('dve', VC[0:127, 64:65], 1.0)
        P.copy('dve', VC[0:127, 65:97], ovl[0:127, :])
        jobs = []
        for r in range(4):
            for qt in range(4):
                def s1(r=r, qt=qt):
                    nonlocal nsp, npt
                    qs = slice(qt * 512, (qt + 1) * 512)
                    sp_ = ps[nsp % 4][0:127, :]
                    nsp += 1
                    P.mm(sp_, kcTd[:, 0:127], qT[:, r, qs], start=True, stop=True)
                    pt = pT[npt % NPT]
                    npt += 1
                    P.act(pt[0:127, :], sp_, AF.Exp, scale=0.125)
                    P.tt('dve', pt[0:127, :], pt[0:127, :], cmpv[0:127, qs], ALU.mult)
                    return pt

                def s2(pt, r=r, qt=qt):
                    nonlocal nob
                    gcol0 = (g * 4 + r) * 3
                    po = ps[4 + nob % 4].rearrange("p (s w) -> p s w", s=4)
                    nob += 1
                    for sub in range(4):
                        P.mm(po[:, sub, 0:97], pt[0:127, sub * 128:(sub + 1) * 128], VC[0:127, 0:97], start=True, stop=True)
                    evac(po, 97, qt, gcol0 + 0, r, True, 'first' if r == 0 else 'add')
                jobs.append((s1, s2))
        run_pipe(jobs, 2)
        for t in range(NT):
            i2 = t % 2
            P.tt('dve', imadj[i2], impacc[:, t, :], keep[:, t, :], ALU.mult)
            P.tt('dve', imadj[i2], imadj[i2], addb[:, t, :], ALU.add)
            P.op('dve', lambda e, i2=i2: e.max(m8a[i2], imadj[i2]), reads=[imadj[i2]], writes=[m8a[i2]])
            P.op('dve', lambda e, i2=i2: e.match_replace(imw[i2], m8a[i2], imadj[i2], -1e9),
                 reads=[m8a[i2], imadj[i2]], writes=[imw[i2]])
            P.op('dve', lambda e, i2=i2: e.max(m8b[i2], imw[i2]), reads=[imw[i2]], writes=[m8b[i2]])
            P.ts('dve', selm[i2], imadj[i2], m8b[i2][:, 7:8], ALU.is_ge)
            P.ts('dve', selb[i2], selm[i2], -1.0, ALU.add, -NEGBIG, ALU.mult)
            pn = psb[t % 2][0:32, 0:128]
            P.tr(pn, selb[i2], C.ident)
            P.copy('act', negst[64:96, t * 128:(t + 1) * 128], pn)
        for r in range(4):
            P.copy('pool' if r % 2 else 'dve', qT[64:96, r, :], negst[64:96, :])
        jobs = []
        for r in range(4):
            for qt in range(4):
                for br in (1, 2):
                    st_ = {'po': None, 'first': True}
                    kb_lo = 0 if br == 1 else max(0, 4 * qt - 4)
                    kb_hi = 4 * qt + 3
                    for kb in range(kb_lo, kb_hi + 1):
                        def s1(r=r, qt=qt, br=br, kb=kb):
                            nonlocal nsp, npt
                            qs = slice(qt * 512, (qt + 1) * 512)
                            ksrc = ksd if br == 1 else kwd
                            ks_ = slice(kb * 128, (kb + 1) * 128)
                            sp_ = ps[nsp % 4]
                            nsp += 1
                            P.mm(sp_, ksrc[:, ks_], qT[:, r, qs], start=True, stop=True)
                            pt = pT[npt % NPT]
                            npt += 1
                            P.act(pt, sp_, AF.Exp, scale=0.125)
                            d = kb - 4 * qt
                            if d >= 0:
                                P.tt('dve', pt, pt, C.maskd[:, d, :], ALU.mult)
                            elif br == 2:
                                P.tt('dve', pt, pt, C.maskd[:, 8 + d, :], ALU.mult)
                            return pt

                        def s2(pt, r=r, qt=qt, br=br, kb=kb, st_=st_, kb_lo=kb_lo, kb_hi=kb_hi):
                            nonlocal nob
                            if st_['po'] is None:
                                st_['po'] = ps[4 + nob % 4].rearrange("p (s w) -> p s w", s=4)
                                nob += 1
                            po = st_['po']
                            vsrc = VS if br == 1 else VW
                            for sub in range(4):
                                lo = kb_lo if br == 1 else max(0, 4 * qt + sub - 4)
                                hi = 4 * qt + sub
                                if kb < lo or kb > hi:
                                    continue
                                P.mm(po[:, sub, 0:65], pt[:, sub * 128:(sub + 1) * 128], vsrc[:, kb, 0:65], start=st_['first'], stop=(kb == kb_hi and sub == 3))
                                st_['first'] = False
                            if kb == kb_hi:
                                evac(po, 65, qt, (g * 4 + r) * 3 + br, r, False, None)
                        jobs.append((s1, s2))
        run_pipe(jobs, 2)
        P.dma('pool', ysc_v[:, :, g * 256:(g + 1) * 256], yc)
    A.release(m0)


def stage_merge(C, li):
    P, A, dr = C.P, C.A, C.dr
    ps, psb, xT = C.ps, C.psb, C.xT
    m0 = A.mark()
    mgT = A.alloc([8, S], BF16)
    m1 = A.mark()
    wbas = [A.alloc([4, 512], BF16) for _ in range(2)]
    wbbs = [A.alloc([8, 512], BF16) for _ in range(2)]
    wbcs = [A.alloc([8, 512], BF16) for _ in range(2)]
    wmgs = [A.alloc([3, 8, 512], BF16) for _ in range(2)]
    bmgs = [A.alloc([3, 512], BF16) for _ in range(2)]

    def load_half(hc):
        cs_ = slice(hc * 512, (hc + 1) * 512)
        P.dma('pool', wbas[hc][0:64, :, :], dr['w_br_a'][li][:, cs_].rearrange("(h d) n -> d h n", d=64))
        load_w(C, wbbs[hc], dr['w_br_b'][li][:, cs_])
        load_w(C, wbcs[hc], dr['w_br_c'][li][:, cs_])
        for b in range(3):
            load_w(C, wmgs[hc][:, b, :, :], dr['w_in'][li][:, O_MG + b * D + hc * 512:O_MG + b * D + (hc + 1) * 512])
            P.dma('pool', bmgs[hc][0:1, b, :], dr['b_in'][li:li + 1, O_MG + b * D + hc * 512:O_MG + b * D + (hc + 1) * 512])
    load_half(0)
    load_half(1)
    yaT = [A.alloc([4, 128], BF16) for _ in range(2)]
    ybl = [A.alloc([D], BF16) for _ in range(2)]
    ycl = [A.alloc([D], BF16) for _ in range(2)]
    ybT = [A.alloc([8, 128], BF16) for _ in range(2)]
    ycT = [A.alloc([8, 128], BF16) for _ in range(2)]
    mg = [A.alloc([512], F32) for _ in range(2)]
    sg = [A.alloc([512], F32) for _ in range(2)]
    mgb = [A.alloc([512], BF16) for _ in range(2)]
    nsg = 0
    for hc in range(2):
        wba, wbb, wbc, wmg, bmg = wbas[hc], wbbs[hc], wbcs[hc], wmgs[hc], bmgs[hc]
        for t in range(NT):
            i2 = t % 2
            tsl = slice(t * 128, (t + 1) * 128)
            P.dma('sp', yaT[i2][0:64, :, :], dr['ysa'][:, :, tsl].rearrange("h d t -> d h t"))
            P.dma('sp', ybl[i2], dr['ysb'][tsl, :])
            P.dma('sp', ycl[i2], dr['ysc'][tsl, :])
            for (src, dst, bank) in ((ybl[i2], ybT[i2], 6), (ycl[i2], ycT[i2], 7)):
                pv = psb[bank].rearrange("p (c q) -> p c q", c=8)
                for c in range(8):
                    P.tr(pv[:, c, :], src[:, c * 128:(c + 1) * 128], C.ident)
                P.copy('act', dst, pv)
            for b in range(3):
                pg = ps[b % 2]
                for k in range(8):
                    P.mm(pg, xT[:, k, tsl], wmg[:, b, k, :], start=(k == 0), stop=False)
                P.mm(pg, C.ones[0:1, 0:128], bmg[0:1, b, :], start=False, stop=True)
                s_ = sg[nsg % 2]
                nsg += 1
                P.act(s_, pg, AF.Sigmoid)
                pp = ps[2 + b % 2]
                if b == 0:
                    for h in range(4):
                        P.mm(pp, yaT[i2][0:64, h, :], wba[0:64, h, :], start=(h == 0), stop=(h == 3))
                else:
                    yT_ = ybT[i2] if b == 1 else ycT[i2]
                    w_ = wbb if b == 1 else wbc
                    for k in range(8):
                        P.mm(pp, yT_[:, k, :], w_[:, k, :], start=(k == 0), stop=(k == 7))
                if b == 0:
                    P.tt('dve', mg[i2], s_, pp, ALU.mult)
                else:
                    P.tt('dve', s_, s_, pp, ALU.mult)
                    P.tt('pool', mg[i2], mg[i2], s_, ALU.add)
            P.copy('act', mgb[i2], mg[i2])
            pv = psb[4 + i2][:, 0:512].rearrange("p (c q) -> p c q", c=4)
            for c in range(4):
                P.tr(pv[:, c, :], mgb[i2][:, c * 128:(c + 1) * 128], C.ident)
            P.copy('act', mgT[:, hc * 4:(hc + 1) * 4, tsl], pv)
    A.release(m1)
    wom = A.alloc([8, D], BF16)
    load_w(C, wom, dr['w_o_mix'][li])
    L = ln_setup(C, dr['ln1_g'][li:li + 1, :], dr['ln1_b'][li:li + 1, :])
    xo = [A.alloc([D], F32) for _ in range(2)]
    for t in range(NT):
        i2 = t % 2
        tsl = slice(t * 128, (t + 1) * 128)
        P.dma('sp', xo[i2], dr['xs'][tsl, :])
        for hc in range(2):
            cs_ = slice(hc * 512, (hc + 1) * 512)
            pp = ps[hc]
            for k in range(8):
                P.mm(pp, mgT[:, k, tsl], wom[:, k, cs_], start=(k == 0), stop=(k == 7))
            P.stt(xo[i2][:, cs_], xo[i2][:, cs_], ALPHA, pp, ALU.mult, ALU.add)
        ln_tile(C, L, xo[i2], t, dr['xs'], psb[4 + i2])
    A.release(m0)


_CACHE = {}


def _get_nc(stages, dbg=False):
    key = (tuple(stages), dbg)
    if key not in _CACHE:
        _CACHE[key] = build_nc(stages, dbg)
    return _CACHE[key]


def make_in_maps(inputs, ncores=8):
    consts = host_consts()
    shared = {}
    for n in WNAMES:
        shared[n] = np.ascontiguousarray(inputs[n], dtype=np.float32)
    shared['ln0_g'] = np.ascontiguousarray(inputs['ln0_g'], dtype=np.float32).reshape(1, D)
    shared['ln0_b'] = np.ascontiguousarray(inputs['ln0_b'], dtype=np.float32).reshape(1, D)
    shared.update(consts)
    maps = []
    for b in range(ncores):
        m = dict(shared)
        m['x'] = np.ascontiguousarray(inputs['x'][b], dtype=np.float32)
        m['mem'] = np.ascontiguousarray(inputs['mem'][b], dtype=np.float32)
        maps.append(m)
    return maps


FULL_STAGES = ['ln0', 'meminit']
for _li in range(DEPTH):
    FULL_STAGES += ['mixA%d' % _li, 'mixB%d' % _li, 'mixC%d' % _li, 'merge%d' % _li, 'xattn%d' % _li, 'moe%d' % _li]
FULL_STAGES[-1] += 'L'


def kernel(**inputs):
    nc, C = _get_nc(FULL_STAGES)
    maps = make_in_maps(inputs, 8)
    res = run_bass_kernel_spmd(nc, maps, core_ids=list(range(8)))
    out = np.stack([np.asarray(r['out']) for r in res.results], axis=0)
    return out.astype(np.float32)
```
